# Optimizing a Trainium2 kernel written in Bass

```python
import math
import jax, jax.numpy as jnp
from jax import lax
import numpy as np

D_MODEL = 1024
BATCH = 32
SEQ = 2048
DEPTH = 4

D_MIX = 2 * D_MODEL
M_WIDTH = 3 * D_MIX // 8
M_HEADS = 4
M_HEAD_DIM = M_WIDTH // M_HEADS
M_QKV_BLOCK = 4
M_CONV = 5
M_CHUNK = 64
H_WIDTH = 3 * D_MIX // 8
H_EXPAND = 128
H_HEADS = H_WIDTH // H_EXPAND
H_CHUNK = 16
A_WIDTH = D_MIX - M_WIDTH - H_WIDTH
A_HEADS = 4
A_VDIM = A_WIDTH // A_HEADS
A_QKDIM = A_VDIM // 2
ROPE_DIM = A_QKDIM // 4
ROPE_THETA = 500000.0
Q_BLOCK = 128
IN_COLS = 3 * M_WIDTH + 5 * H_WIDTH + 4 * A_WIDTH
EPS = 1e-6

kernel_name = "hybrid_mlstm_hgrn2_diffattn_encoder"


def _split_points():
    sizes = [M_WIDTH] * 3 + [H_WIDTH] * 5 + [A_WIDTH] * 4
    return [int(c) for c in np.cumsum(sizes)[:-1]]


def rms_norm(x, g):
    xf = x.astype(jnp.float32)
    y = xf * lax.rsqrt(jnp.mean(xf * xf, axis=-1, keepdims=True) + EPS)
    return (y * g.astype(jnp.float32)).astype(x.dtype)


def head_rms_norm(x, g, n_heads):
    B, S, W = x.shape
    xf = x.astype(jnp.float32).reshape(B, S, n_heads, W // n_heads)
    y = xf * lax.rsqrt(jnp.mean(xf * xf, axis=-1, keepdims=True) + EPS)
    return (y.reshape(B, S, W) * g.astype(jnp.float32)).astype(x.dtype)


def centred_conv(x, w, b):
    K = w.shape[0]
    pad = K // 2
    y = lax.conv_general_dilated(x, w[:, None, :], window_strides=(1,), padding=[(pad, pad)],
                                 dimension_numbers=("NWC", "WIO", "NWC"),
                                 feature_group_count=x.shape[-1])
    return y + b


def block_diag_proj(x, w):
    B, S, W = x.shape
    nb, blk, _ = w.shape
    return jnp.einsum("bsgi,gio->bsgo", x.reshape(B, S, nb, blk), w).reshape(B, S, W)


def to_heads(t, n):
    B, S, W = t.shape
    return t.reshape(B, S, n, W // n).transpose(0, 2, 1, 3)


def from_heads(t):
    B, H, S, D = t.shape
    return t.transpose(0, 2, 1, 3).reshape(B, S, H * D)


def _chunk(t, L):
    B, H, S = t.shape[:3]
    return jnp.moveaxis(t.reshape((B, H, S // L, L) + t.shape[3:]), 2, 0)


def _unchunk(t):
    t = jnp.moveaxis(t, 0, 2)
    return t.reshape(t.shape[:2] + (t.shape[2] * t.shape[3],) + t.shape[4:])


def _flip_seq(t):
    return jnp.flip(t, axis=2)


def mlstm_chunkwise(q, k, v, i_pre, f_pre):
    B, H, S, D = q.shape
    L = M_CHUNK
    qf = q.astype(jnp.float32) * (D ** -0.5)
    kf, vf = k.astype(jnp.float32), v.astype(jnp.float32)
    log_f = jax.nn.log_sigmoid(f_pre.astype(jnp.float32))
    log_i = i_pre.astype(jnp.float32)
    xs = (_chunk(qf, L), _chunk(kf, L), _chunk(vf, L), _chunk(log_i, L), _chunk(log_f, L))
    mask = jnp.tril(jnp.ones((L, L), dtype=bool))

    def step(carry, inp):
        C, n, m = carry
        qb, kb, vb, ib, fb = inp
        b = jnp.cumsum(fb, axis=-1)
        bL = b[..., -1]
        d_ts = jnp.where(mask, b[..., :, None] - b[..., None, :] + ib[..., None, :], -jnp.inf)
        inter = b + m[..., None]
        m_t = jnp.maximum(jnp.max(d_ts, axis=-1), inter)
        w = jnp.exp(d_ts - m_t[..., None]) * jnp.einsum("bhtd,bhsd->bhts", qb, kb)
        g_inter = jnp.exp(inter - m_t)
        num = jnp.einsum("bhts,bhsd->bhtd", w, vb) + g_inter[..., None] * jnp.einsum("bhtd,bhde->bhte", qb, C)
        den = jnp.sum(w, axis=-1) + g_inter * jnp.einsum("bhtd,bhd->bht", qb, n)
        h = num / jnp.maximum(jnp.abs(den), jnp.exp(-m_t))[..., None]
        dec_s = bL[..., None] - b + ib
        m_new = jnp.maximum(bL + m, jnp.max(dec_s, axis=-1))
        a = jnp.exp(bL + m - m_new)
        ws = jnp.exp(dec_s - m_new[..., None])
        C_new = a[..., None, None] * C + jnp.einsum("bhs,bhsd,bhse->bhde", ws, kb, vb)
        n_new = a[..., None] * n + jnp.einsum("bhs,bhsd->bhd", ws, kb)
        return (C_new, n_new, m_new), h

    init = (jnp.zeros((B, H, D, D), jnp.float32), jnp.zeros((B, H, D), jnp.float32),
            jnp.zeros((B, H), jnp.float32))
    _, hs = lax.scan(step, init, xs)
    return _unchunk(hs).astype(q.dtype)


def mlstm_bidirectional(q, k, v, i_fwd, f_fwd, i_bwd, f_bwd):
    fwd = mlstm_chunkwise(q, k, v, i_fwd, f_fwd)
    bwd = _flip_seq(mlstm_chunkwise(_flip_seq(q), _flip_seq(k), _flip_seq(v),
                                    _flip_seq(i_bwd), _flip_seq(f_bwd)))
    return fwd + bwd


def hgrn2_chunkwise(q, f, v):
    B, H, S, Dk = q.shape
    Dv = v.shape[-1]
    L = H_CHUNK
    ff = f.astype(jnp.float32)
    xs = (_chunk(q.astype(jnp.float32), L), _chunk(1.0 - ff, L),
          _chunk(v.astype(jnp.float32), L), _chunk(jnp.log(ff), L))
    mask = jnp.tril(jnp.ones((L, L), dtype=bool))[:, :, None]

    def step(S_state, inp):
        qb, kb, vb, gb = inp
        A = jnp.cumsum(gb, axis=-2)
        diff = A[..., :, None, :] - A[..., None, :, :]
        decay = jnp.exp(jnp.where(mask, diff, -jnp.inf))
        scores = jnp.einsum("bhtc,bhtsc,bhsc->bhts", qb, decay, kb)
        o = jnp.einsum("bhts,bhsv->bhtv", scores, vb) + jnp.einsum("bhtc,bhcv->bhtv", qb * jnp.exp(A), S_state)
        AL = A[..., -1, :]
        S_new = jnp.exp(AL)[..., None] * S_state + jnp.einsum("bhsc,bhsv->bhcv", kb * jnp.exp(AL[..., None, :] - A), vb)
        return S_new, o

    _, os_ = lax.scan(step, jnp.zeros((B, H, Dk, Dv), jnp.float32), xs)
    return _unchunk(os_).astype(q.dtype)


def hgrn2_bidirectional(q, f_fwd, f_bwd, v):
    fwd = hgrn2_chunkwise(q, f_fwd, v)
    bwd = _flip_seq(hgrn2_chunkwise(_flip_seq(q), _flip_seq(f_bwd), _flip_seq(v)))
    return fwd + bwd


def hgrn2_lower_bounds(lb_logits):
    p = jax.nn.softmax(lb_logits.astype(jnp.float32), axis=0)
    c = jnp.cumsum(p, axis=0)
    return c - c[:1]


def rope_partial(x, positions):
    half = ROPE_DIM // 2
    inv = ROPE_THETA ** (-jnp.arange(half, dtype=jnp.float32) / half)
    ang = positions.astype(jnp.float32)[..., None] * inv
    cos, sin = jnp.cos(ang)[:, :, None, :], jnp.sin(ang)[:, :, None, :]
    xr = x[..., :ROPE_DIM].astype(jnp.float32)
    x1, x2 = xr[..., :half], xr[..., half:]
    rot = jnp.concatenate([x1 * cos - x2 * sin, x2 * cos + x1 * sin], axis=-1).astype(x.dtype)
    return jnp.concatenate([rot, x[..., ROPE_DIM:]], axis=-1)


def differential_attention(q, k, v, lam, positions):
    B, S, N2, Dq = q.shape
    Hh, Dv = v.shape[2], v.shape[3]
    q = rope_partial(q, positions) * (Dq ** -0.5)
    k = rope_partial(k, positions)
    nb = S // Q_BLOCK
    qb = q.reshape(B, nb, Q_BLOCK, N2, Dq).transpose(1, 0, 2, 3, 4)

    def block(qi):
        s = jnp.einsum("bqnd,bknd->bnqk", qi, k).astype(jnp.float32)
        p = jax.nn.softmax(s, axis=-1).reshape(B, Hh, 2, Q_BLOCK, S)
        w = p[:, :, 0] - lam * p[:, :, 1]
        return jnp.einsum("bhqk,bkhv->bqhv", w.astype(v.dtype), v)

    o = lax.map(block, qb)
    return o.transpose(1, 0, 2, 3, 4).reshape(B, S, Hh, Dv)


def setup_inputs(seed: int = 0) -> dict:
    key = jax.random.key(seed)
    ks = jax.random.split(key, 20)
    f32 = jnp.float32

    def nrm(k, shape, scale):
        return jax.random.normal(k, shape, f32) * scale

    nblk = M_WIDTH // M_QKV_BLOCK
    f_bias = jnp.linspace(3.0, 6.0, M_HEADS, dtype=f32)
    zeros_h = jnp.zeros((M_HEADS,), f32)
    gate_base = jnp.concatenate([zeros_h, f_bias, zeros_h, f_bias])
    return {
        "x": nrm(ks[0], (BATCH, SEQ, D_MODEL), 1.0),
        "positions": jnp.broadcast_to(jnp.arange(SEQ, dtype=jnp.int32), (BATCH, SEQ)),
        "norm_g": 1.0 + nrm(ks[1], (DEPTH, D_MODEL), 0.02),
        "w_in": nrm(ks[2], (DEPTH, D_MODEL, IN_COLS), D_MODEL ** -0.5),
        "m_conv_w": nrm(ks[3], (DEPTH, M_CONV, M_WIDTH), M_CONV ** -0.5),
        "m_conv_b": nrm(ks[4], (DEPTH, M_WIDTH), 0.02),
        "m_wq": nrm(ks[5], (DEPTH, nblk, M_QKV_BLOCK, M_QKV_BLOCK), M_QKV_BLOCK ** -0.5),
        "m_wk": nrm(ks[6], (DEPTH, nblk, M_QKV_BLOCK, M_QKV_BLOCK), M_QKV_BLOCK ** -0.5),
        "m_wv": nrm(ks[7], (DEPTH, nblk, M_QKV_BLOCK, M_QKV_BLOCK), M_QKV_BLOCK ** -0.5),
        "m_w_gates": nrm(ks[8], (DEPTH, 3 * M_WIDTH, 4 * M_HEADS), (3 * M_WIDTH) ** -0.5),
        "m_b_gates": gate_base + nrm(ks[9], (DEPTH, 4 * M_HEADS), 0.1),
        "m_skip": 1.0 + nrm(ks[10], (DEPTH, M_WIDTH), 0.02),
        "m_norm_g": 1.0 + nrm(ks[11], (DEPTH, M_WIDTH), 0.02),
        "h_lb_logits": nrm(ks[12], (DEPTH, 2, H_WIDTH), 0.5),
        "h_norm_g": 1.0 + nrm(ks[13], (DEPTH, H_WIDTH), 0.02),
        "a_lambda": nrm(ks[14], (DEPTH, 4, A_QKDIM), 0.1),
        "a_norm_g": 1.0 + nrm(ks[15], (DEPTH, A_WIDTH), 0.02),
        "w_out": nrm(ks[16], (DEPTH, D_MIX, D_MODEL), D_MIX ** -0.5),
        "final_g": 1.0 + nrm(ks[17], (D_MODEL,), 0.02),
    }


def reference(x, positions, norm_g, w_in, m_conv_w, m_conv_b, m_wq, m_wk, m_wv, m_w_gates,
              m_b_gates, m_skip, m_norm_g, h_lb_logits, h_norm_g, a_lambda, a_norm_g, w_out,
              final_g):
    split_pts = _split_points()
    lower_bounds = hgrn2_lower_bounds(h_lb_logits)
    for l in range(DEPTH):
        h = rms_norm(x, norm_g[l])
        proj = jnp.einsum("bsd,de->bse", h, w_in[l])
        (xm, om, zm, hq, hff, hfb, hi, hz, aq, ak, av, az) = jnp.split(proj, split_pts, axis=-1)

        xc = jax.nn.silu(centred_conv(xm, m_conv_w[l], m_conv_b[l]))
        mq = block_diag_proj(xc, m_wq[l])
        mk = block_diag_proj(xc, m_wk[l])
        mv = block_diag_proj(xm, m_wv[l])
        gates = jnp.einsum("bsc,cg->bsg", jnp.concatenate([mq, mk, mv], axis=-1), m_w_gates[l]) + m_b_gates[l]
        gates = gates.transpose(0, 2, 1)
        i_f, f_f, i_b, f_b = jnp.split(gates, 4, axis=1)
        hm = mlstm_bidirectional(to_heads(mq, M_HEADS), to_heads(mk, M_HEADS), to_heads(mv, M_HEADS),
                                 i_f, f_f, i_b, f_b)
        hm = jax.nn.sigmoid(om) * from_heads(hm)
        hm = head_rms_norm(hm, m_norm_g[l], M_HEADS) + m_skip[l] * xc
        y_m = hm * jax.nn.silu(zm)

        lb = lower_bounds[l]
        f_fwd = lb[0] + (1.0 - lb[0]) * jax.nn.sigmoid(hff.astype(jnp.float32))
        f_bwd = lb[1] + (1.0 - lb[1]) * jax.nn.sigmoid(hfb.astype(jnp.float32))
        ho = hgrn2_bidirectional(to_heads(hq, H_HEADS), to_heads(f_fwd, H_HEADS),
                                 to_heads(f_bwd, H_HEADS), to_heads(hi, H_HEADS))
        y_h = head_rms_norm(from_heads(ho), h_norm_g[l], H_HEADS) * jax.nn.silu(hz)

        lam_init = 0.8 - 0.6 * math.exp(-0.3 * l)
        lp = a_lambda[l].astype(jnp.float32)
        lam = jnp.exp(jnp.sum(lp[0] * lp[1])) - jnp.exp(jnp.sum(lp[2] * lp[3])) + lam_init
        B_, S_ = x.shape[0], x.shape[1]
        ao = differential_attention(aq.reshape(B_, S_, 2 * A_HEADS, A_QKDIM),
                                    ak.reshape(B_, S_, 2 * A_HEADS, A_QKDIM),
                                    av.reshape(B_, S_, A_HEADS, A_VDIM), lam, positions)
        ao = head_rms_norm(ao.reshape(B_, S_, A_WIDTH), a_norm_g[l], A_HEADS) * (1.0 - lam_init)
        y_a = ao * jax.nn.silu(az)

        y = jnp.concatenate([y_m, y_h, y_a], axis=-1)
        x = x + jnp.einsum("bse,ed->bsd", y, w_out[l])
    return rms_norm(x, final_g)
```

```python
import math
from contextlib import ExitStack
import numpy as np
import concourse.bass as bass
import concourse.mybir as mybir
from concourse.bass_utils import run_bass_kernel_spmd

F32 = mybir.dt.float32
BF16 = mybir.dt.bfloat16
I32 = mybir.dt.int32
ALU = mybir.AluOpType
AF = mybir.ActivationFunctionType
AX = mybir.AxisListType

ENGS = ("pe", "act", "dve", "pool", "sp")
SEM_WRAP = 30000
EPS = 1e-6


class Buf:
    __slots__ = ("last_w", "readers", "id")
    _n = 0

    def __init__(self):
        self.last_w = None
        self.readers = []
        Buf._n += 1
        self.id = Buf._n


class T:
    __slots__ = ("ap", "buf")

    def __init__(self, ap, buf=None):
        self.ap = ap
        self.buf = buf if buf is not None else Buf()

    def __getitem__(self, idx):
        return T(self.ap[idx], self.buf)

    def sub(self, idx):
        return T(self.ap[idx], Buf())


class Op:
    __slots__ = ("eng", "fn", "deps", "idx", "is_dma", "sig", "dkey")


class Prog:
    def __init__(self):
        self.ops = []

    def op(self, eng, fn, reads=(), writes=(), is_dma=False, dkey=None):
        o = Op()
        o.eng = eng
        o.fn = fn
        o.is_dma = is_dma
        o.dkey = dkey
        o.sig = None
        o.idx = len(self.ops)
        deps = set()
        for t in reads:
            b = t.buf
            if b.last_w is not None:
                deps.add(b.last_w)
        for t in writes:
            b = t.buf
            if b.last_w is not None:
                deps.add(b.last_w)
            deps.update(b.readers)
        for t in reads:
            t.buf.readers.append(o.idx)
        for t in writes:
            t.buf.last_w = o.idx
            t.buf.readers = []
        deps.discard(o.idx)
        o.deps = deps
        self.ops.append(o)
        return o

    def emit(self, nc, ctx):
        ops = self.ops
        needed = set()
        for o in ops:
            best = {}
            for d in o.deps:
                p = ops[d]
                if p.eng == "pe" and o.eng == "pe" and not p.is_dma and not o.is_dma:
                    continue
                key = ("dma", p.dkey) if p.is_dma else ("eng", p.eng)
                if key not in best or best[key] < d:
                    best[key] = d
            o.deps = sorted(best.values())
            needed.update(o.deps)
        counters = {}
        sems = {}

        def getsem(name):
            if name not in sems:
                sems[name] = ctx.enter_context(nc.semaphore(name))
            return sems[name]

        for o in ops:
            if o.idx not in needed and not (o.is_dma and o.fn is not None):
                continue
            if o.is_dma:
                base = "d%s" % (o.dkey,)
                inc = 16
            else:
                base = "e" + o.eng
                inc = 1
            cnt, epoch = counters.get(base, (0, 0))
            if cnt + inc > SEM_WRAP:
                epoch += 1
                cnt = 0
            cnt += inc
            counters[base] = (cnt, epoch)
            o.sig = ("%s_%d" % (base, epoch), cnt, inc)
        for o in ops:
            if o.sig:
                getsem(o.sig[0])
        self.n_sems = len(sems)
        per_eng = {e: [] for e in ENGS}
        for o in ops:
            per_eng[o.eng].append(o)
        block = ctx.enter_context(nc.Block())

        def run(eng_obj, lst):
            last_wait = {}
            for o in lst:
                for d in o.deps:
                    sname, val, _ = ops[d].sig
                    if last_wait.get(sname, 0) >= val:
                        continue
                    last_wait[sname] = val
                    eng_obj.wait_ge(sems[sname], val)
                if o.fn is None:
                    continue
                ins = o.fn(eng_obj)
                if o.sig is not None:
                    ins.then_inc(sems[o.sig[0]], o.sig[2])

        @block.tensor
        def _(e):
            run(e, per_eng["pe"])

        @block.scalar
        def _(e):
            run(e, per_eng["act"])

        @block.vector
        def _(e):
            run(e, per_eng["dve"])

        @block.gpsimd
        def _(e):
            run(e, per_eng["pool"])

        @block.sync
        def _(e):
            run(e, per_eng["sp"])


D_MODEL = 1024
D_MIX = 2048
M_W = 768
H_W = 768
A_W = 512
IN_COLS = 8192
OFF = dict(xm=0, om=768, zm=1536, hq=2304, hff=3072, hfb=3840, hi=4608, hz=5376,
           aq=6144, ak=6656, av=7168, az=7680)
Y_OFF = dict(m=0, h=768, a=1536)
ROPE_THETA = 500000.0
NEG = -30000.0

C_IDENT = 0
C_U = 128
C_L = 256
C_PSW = 384
C_ONES = 512
C_VEC = 640
NCONST = 648


def make_consts():
    c = np.zeros((128, NCONST), np.float32)
    r = np.arange(128)
    c[:, C_IDENT:C_IDENT + 128] = np.eye(128, dtype=np.float32)
    c[:, C_U:C_U + 128] = (r[:, None] <= r[None, :]).astype(np.float32)
    c[:, C_L:C_L + 128] = (r[:, None] >= r[None, :]).astype(np.float32)
    psw = np.zeros((128, 128), np.float32)
    for m in range(128):
        d = m % 64
        if d < 8:
            psw[m + 8, m] = 1.0
        elif d < 16:
            psw[m - 8, m] = 1.0
    c[:, C_PSW:C_PSW + 128] = psw
    c[:, C_ONES:C_ONES + 128] = 1.0
    half = 8
    inv = ROPE_THETA ** (-np.arange(half, dtype=np.float32) / half)
    for p in range(128):
        d = p % 64
        if d < 16:
            c[p, C_VEC + 0] = inv[d % 8]
            c[p, C_VEC + 1] = -1.0 if d < 8 else 1.0
        c[p, C_VEC + 2] = 1.0 if p < 64 else 0.0
        c[p, C_VEC + 3] = 0.0 if p < 64 else 1.0
    c[:, C_VEC + 4] = 1024 * EPS
    c[:, C_VEC + 5] = 128 * EPS
    c[:, C_VEC + 6] = 192 * EPS
    c[:, C_VEC + 7] = 1.0
    return c


def build(S, NSEQ, NL, DEPTH_ALL, lam_inits, final_norm=True, groups=("a", "h", "m"), dbg=()):
    NT = S // 128
    NG = S // 512
    nc = bass.Bass("TRN2", target_bir_lowering=False)
    P = Prog()
    ctx = ExitStack()

    def dram(name, shape, dt=F32, kind="ExternalInput"):
        return nc.dram_tensor(name, shape, dt, kind=kind).ap()

    d_x = dram("x", [NSEQ, S, D_MODEL])
    d_pos = dram("pos", [NSEQ, S], I32)
    d_win = dram("w_in", [NL, D_MODEL, IN_COLS])
    d_wout = dram("w_out", [NL, D_MIX, D_MODEL])
    d_consts = dram("consts", [128, NCONST])
    d_ng = dram("ng", [128, NL * 8])
    d_fgrow = dram("fgrow", [1, 1024])
    d_alam = dram("alam", [1, NL * 256])
    d_ang = dram("ang", [128, NL * 4])
    d_lb = dram("lb", [128, 2 * 6 * DEPTH_ALL])
    d_hng = dram("hng", [128, NL * 6])
    d_cwA = dram("cwA", [128, NL * 4 * 5])
    d_cwB = dram("cwB", [64, NL * 4 * 5])
    d_mvA = dram("mvA", [128, NL * 4 * 3])
    d_mvB = dram("mvB", [64, NL * 4 * 3])
    d_bdA = dram("bdA", [NL, 4, 128, 6 * 128])
    d_bdB = dram("bdB", [NL, 4, 64, 6 * 64])
    d_wgA = dram("wgA", [NL, 128, 4 * 3 * 16])
    d_wgB = dram("wgB", [NL, 64, 4 * 3 * 16])
    d_bg = dram("bg", [1, NL * 16])
    d_out = dram("out", [NSEQ, S, D_MODEL], kind="ExternalOutput")
    dbg_outs = {}

    def sb(shape, dt=F32, name=None):
        return T(ctx.enter_context(nc.sbuf_tensor("s_" + name, shape, dt))[:])

    def dma_in(dst, src_ap, eng="sp"):
        P.op(eng, lambda e: e.dma_start(out=dst.ap, in_=src_ap), writes=[dst], is_dma=True, dkey=dst.buf.id)

    def dma_out(dst_ap, src, eng="sp"):
        tok = T(None)
        P.op(eng, lambda e: e.dma_start(out=dst_ap, in_=src.ap), reads=[src], writes=[tok], is_dma=True,
             dkey="o%d" % src.buf.id)
        return tok

    def mm(out, lhsT, rhs, start=True, stop=True, extra_reads=()):
        P.op("pe", lambda e: e.matmul(out.ap, lhsT=lhsT.ap, rhs=rhs.ap, start=start, stop=stop),
             reads=[lhsT, rhs] + list(extra_reads), writes=[out])

    def transp(out, in_, ident):
        P.op("pe", lambda e: e.transpose(out=out.ap, in_=in_.ap, identity=ident.ap), reads=[in_, ident], writes=[out])

    def act(out, in_, func, bias=None, scale=None, eng="act", extra_reads=()):
        kw = {}
        rd = [in_] + list(extra_reads)
        if bias is not None:
            if isinstance(bias, T):
                kw["bias"] = bias.ap
                rd.append(bias)
            else:
                kw["bias"] = bias
        if scale is not None:
            if isinstance(scale, T):
                kw["scale"] = scale.ap
                rd.append(scale)
            else:
                kw["scale"] = scale
        P.op(eng, lambda e: e.activation(out=out.ap, in_=in_.ap, func=func, **kw), reads=rd, writes=[out])

    def tt(out, a, b, op, eng="dve"):
        P.op(eng, lambda e: e.tensor_tensor(out=out.ap, in0=a.ap, in1=b.ap, op=op), reads=[a, b], writes=[out])

    def ts(out, a, s1, op0, s2=None, op1=None, eng="dve"):
        rd = [a]
        v1 = s1
        v2 = s2
        if isinstance(s1, T):
            rd.append(s1)
            v1 = s1.ap
        if isinstance(s2, T):
            rd.append(s2)
            v2 = s2.ap
        if op1 is None:
            P.op(eng, lambda e: e.tensor_scalar(out=out.ap, in0=a.ap, scalar1=v1, scalar2=None, op0=op0),
                 reads=rd, writes=[out])
        else:
            P.op(eng, lambda e: e.tensor_scalar(out=out.ap, in0=a.ap, scalar1=v1, scalar2=v2, op0=op0, op1=op1),
                 reads=rd, writes=[out])

    def stt(out, a, s, b, op0, op1, eng="dve"):
        rd = [a, b]
        v = s
        if isinstance(s, T):
            rd.append(s)
            v = s.ap
        P.op(eng, lambda e: e.scalar_tensor_tensor(out=out.ap, in0=a.ap, scalar=v, in1=b.ap, op0=op0, op1=op1),
             reads=rd, writes=[out])

    def cp(out, in_, eng="dve"):
        if eng == "act":
            P.op("act", lambda e: e.copy(out=out.ap, in_=in_.ap), reads=[in_], writes=[out])
        else:
            P.op(eng, lambda e: e.tensor_copy(out=out.ap, in_=in_.ap), reads=[in_], writes=[out])

    def memset(out, val, eng="pool"):
        P.op(eng, lambda e: e.memset(out.ap, val), writes=[out])

    def recip(out, in_):
        P.op("dve", lambda e: e.reciprocal(out=out.ap, in_=in_.ap), reads=[in_], writes=[out])

    def scan_cumsum(out, ones, in_):
        P.op("dve", lambda e: e.tensor_tensor_scan(out=out.ap, data0=ones.ap, data1=in_.ap, initial=0.0,
                                                   op0=ALU.mult, op1=ALU.add), reads=[ones, in_], writes=[out])

    def dbg_out(name, src, shape, dt=BF16):
        if name not in dbg:
            return
        d = dram("dbg_" + name, shape, dt, kind="ExternalOutput")
        dbg_outs[name] = d
        fin.append(dma_out(d, src))

    fin = []

    banks = [T(ctx.enter_context(nc.psum_tensor("pb%d" % i, [128, 512], F32))[:]) for i in range(8)]
    rr = {"b": 0}

    def pbank():
        rr["b"] = (rr["b"] + 1) % 8
        return banks[rr["b"]]

    def pquart():
        return pbank()[:, 0:128]

    consts_f = sb([128, NCONST], F32, "consts_f")
    consts_b = sb([128, 640], BF16, "consts_b")
    dma_in(consts_f, d_consts)
    cp(consts_b, consts_f[:, 0:640])
    ident_f = consts_f[:, C_IDENT:C_IDENT + 128]
    U_f = consts_f[:, C_U:C_U + 128]
    L_f = consts_f[:, C_L:C_L + 128]
    psw_b = consts_b[:, C_PSW:C_PSW + 128]
    ones_b = consts_b[:, C_ONES:C_ONES + 128]
    ones_f = consts_f[:, C_ONES:C_ONES + 128]
    U_b = consts_b[:, C_U:C_U + 128]
    L_b = consts_b[:, C_L:C_L + 128]
    invf = consts_f[:, C_VEC + 0:C_VEC + 1]
    sgn = consts_f[:, C_VEC + 1:C_VEC + 2]
    m1 = consts_f[:, C_VEC + 2:C_VEC + 3]
    m2 = consts_f[:, C_VEC + 3:C_VEC + 4]
    epsc = {1024: consts_f[:, C_VEC + 4:C_VEC + 5], 128: consts_f[:, C_VEC + 5:C_VEC + 6], 192: consts_f[:, C_VEC + 6:C_VEC + 7]}
    one_c = consts_f[:, C_VEC + 7:C_VEC + 8]

    def rstd_from(out, v, n):
        act(out, v, AF.Ln, bias=epsc[n][0:out.ap.shape[0], :])
        act(out, out, AF.Exp, scale=-0.5)
    mnegF = sb([128, 128], F32, "mnegF")
    mnegB = sb([128, 128], F32, "mnegB")
    ts(mnegF, U_f, 1.0, ALU.subtract, -NEG, ALU.mult)
    ts(mnegB, L_f, 1.0, ALU.subtract, -NEG, ALU.mult)

    ng = sb([128, NL * 8], F32, "ng")
    dma_in(ng, d_ng)
    ng32 = sb([128, NL * 8], F32, "ng32")
    ts(ng32, ng, 32.0, ALU.mult)
    ang = sb([128, NL * 4], F32, "angs")
    dma_in(ang, d_ang)
    lbl = sb([128, 2 * 6 * DEPTH_ALL], F32, "lbl")
    dma_in(lbl, d_lb)
    hng = sb([128, NL * 6], F32, "hngs")
    dma_in(hng, d_hng)
    cwA = sb([128, NL * 20], F32, "cwA")
    dma_in(cwA, d_cwA)
    cwB = sb([64, NL * 20], F32, "cwB")
    dma_in(cwB, d_cwB)
    mvA = sb([128, NL * 12], F32, "mvA")
    dma_in(mvA, d_mvA)
    mvB = sb([64, NL * 12], F32, "mvB")
    dma_in(mvB, d_mvB)
    bgs = sb([128, NL * 16], F32, "bgs")
    dma_in(bgs, d_bg[0:1, :].to_broadcast([128, NL * 16]))

    neglam = sb([128, NL], F32, "neglam")
    gsa = sb([128, NL * 4], F32, "gsa")
    lamtmp = sb([128, 64], F32, "lamtmp")
    lam2 = sb([128, 4], F32, "lam2")
    alam_t = sb([128, 256], F32, "alam")
    for l in range(NL):
        dma_in(alam_t, d_alam[0:1, l * 256:(l + 1) * 256].to_broadcast([128, 256]))
        for j in range(2):
            tt(lamtmp, alam_t[:, j * 128:j * 128 + 64],
               alam_t[:, j * 128 + 64:j * 128 + 128], ALU.mult)
            P.op("dve", (lambda o, i: (lambda e: e.reduce_sum(out=o.ap, in_=i.ap, axis=AX.X)))(lam2[:, j:j + 1], lamtmp),
                 reads=[lamtmp], writes=[lam2])
        act(lam2[:, 2:4], lam2[:, 0:2], AF.Exp)
        tt(lam2[:, 0:1], lam2[:, 3:4], lam2[:, 2:3], ALU.subtract)
        ts(neglam[:, l:l + 1], lam2[:, 0:1], -float(lam_inits[l]), ALU.add)
        ts(gsa[:, l * 4:(l + 1) * 4], ang[:, l * 4:(l + 1) * 4], float((1.0 - lam_inits[l]) * math.sqrt(128.0)), ALU.mult)
    NLB = 12 * DEPTH_ALL
    lbe = sb([128, NLB], F32, "lbe")
    act(lbe, lbl, AF.Exp)
    lbs = sb([128, 12], F32, "lbs")
    P.op("dve", lambda e: e.reduce_sum(out=lbs.ap, in_=lbe.ap.rearrange("p (a l) -> p a l", l=DEPTH_ALL), axis=AX.X),
         reads=[lbe], writes=[lbs])
    lbr = sb([128, 12], F32, "lbr")
    recip(lbr, lbs)
    lbv = sb([128, NL * 12], F32, "lbv")
    omlb = sb([128, NL * 12], F32, "omlb")
    lbt = sb([128, 12], F32, "lbt")

    def lb_for_layer(l, lglob):
        dst = lbv[:, l * 12:(l + 1) * 12]
        if lglob == 0:
            memset(dst, 0.0, eng="dve")
        else:
            P.op("dve", lambda e: e.reduce_sum(
                out=lbt.ap, in_=lbe.ap.rearrange("p (a l) -> p a l", l=DEPTH_ALL)[:, :, 1:lglob + 1], axis=AX.X),
                reads=[lbe], writes=[lbt])
            tt(dst, lbt, lbr, ALU.mult)
        ts(omlb[:, l * 12:(l + 1) * 12], dst, -1.0, ALU.mult, 1.0, ALU.add)

    hgs = sb([128, NL * 6], F32, "hgs")
    ts(hgs, hng, float(math.sqrt(128.0)), ALU.mult)
    mgsA = sb([128, NL * 4], F32, "mgsA")
    mgsB = sb([64, NL * 4], F32, "mgsB")

    xdr = [[T(None) for t in range(NT)] for b in range(NSEQ)]
    xsrc = {}
    hT = [sb([128, S], BF16, "hT%d" % k) for k in range(8)]
    hTg = [[hT[k].sub((slice(None), slice(g * 512, (g + 1) * 512))) for g in range(NG)] for k in range(8)]
    W_bf = [sb([128, S], BF16, "wbf%d" % i) for i in range(10)]
    W_f32 = [sb([128, max(S + 4, 1024)], F32, "wf%d" % i) for i in range(3)]
    xin = [W_f32[0][:, 0:1024], W_f32[1][:, 0:1024]]
    fgb = W_f32[2][:, 0:1024]
    xt_tiles = [sb([128, 1024], F32, "xtile%d" % i) for i in range(2)]
    rrx = {"i": 0}
    ssq = sb([128, 4], F32, "ssq")
    tmpf = [sb([128, 512], F32, "tmpf%d" % i) for i in range(6)]
    tmpb = [sb([128, 512], BF16, "tmpb%d" % i) for i in range(6)]
    rrt = {"f": 0, "b": 0}

    def tf():
        rrt["f"] = (rrt["f"] + 1) % len(tmpf)
        return tmpf[rrt["f"]]

    def tb():
        rrt["b"] = (rrt["b"] + 1) % len(tmpb)
        return tmpb[rrt["b"]]

    NWB = 3
    wbf = [sb([128, 8, 128], BF16, "wbf16_%d" % i) for i in range(NWB)]
    rrw = {"s": 0, "b": 0, "os": 0, "ob": 0}
    wo_bf = [sb([128, 1024], BF16, "wobf%d" % i) for i in range(4)]

    def load_win(l, col0, ncols):
        rrw["b"] = (rrw["b"] + 1) % NWB
        wb = wbf[rrw["b"]]
        src_ = d_win[l, :, col0:col0 + ncols].rearrange("(k p) c -> p k c", p=128)
        dma_in(wb[:, :, 0:ncols], src_, eng="pool")
        return wb

    def load_wout(l, row0, nrows):
        rrw["ob"] = (rrw["ob"] + 1) % 4
        wb = wo_bf[rrw["ob"]]
        dma_in(wb[0:nrows, :], d_wout[l, row0:row0 + nrows, :], eng="pool")
        return wb

    def proj_fm(ps, wb, c0, ncols, g):
        for k in range(8):
            mm(ps[0:ncols, :], wb[:, k, c0:c0 + ncols], hTg[k][g], start=(k == 0), stop=(k == 7))

    def proj_tm(ps_view, wb, c0, ncols, tt_):
        g = tt_ // 4
        o = (tt_ % 4) * 128
        for k in range(8):
            mm(ps_view, hTg[k][g][:, o:o + 128], wb[:, k, c0:c0 + ncols], start=(k == 0), stop=(k == 7))

    def sumsq_rstd(rstd_out, srcs, n, g_cols):
        ps = pbank()
        for i, (s, kk) in enumerate(srcs):
            sq = tb()
            act(sq[0:kk, 0:g_cols], s, AF.Square)
            mm(ps[:, 0:g_cols], ones_b[0:kk, :], sq[0:kk, 0:g_cols], start=(i == 0), stop=(i == len(srcs) - 1))
        rstd_from(rstd_out, ps[:, 0:g_cols], n)

    def out_proj(l, ysrcs):
        b = cur["b"]
        wts = [load_wout(l, r0, kk) for (_, kk, r0) in ysrcs]
        for t in range(NT):
            g = t // 4
            o = (t % 4) * 128
            rrx["i"] = (rrx["i"] + 1) % 2
            xt = xt_tiles[rrx["i"]]
            P.op("sp", (lambda dst, s_: (lambda e: e.dma_start(out=dst.ap, in_=s_)))(xt, xsrc[b][t]),
                 reads=[xdr[b][t]], writes=[xt], is_dma=True, dkey=xt.buf.id)
            for hf in range(2):
                ps = pbank()
                for i, (yf, kk, r0) in enumerate(ysrcs):
                    mm(ps, yf(g)[:, o:o + 128], wts[i][0:kk, hf * 512:(hf + 1) * 512], start=(i == 0), stop=(i == len(ysrcs) - 1))
                tt(xt[:, hf * 512:(hf + 1) * 512], xt[:, hf * 512:(hf + 1) * 512], ps, ALU.add)
            P.op("sp", (lambda src_, d_: (lambda e: e.dma_start(out=d_, in_=src_.ap)))(xt, d_out[b, t * 128:(t + 1) * 128, :]),
                 reads=[xt], writes=[xdr[b][t]], is_dma=True, dkey="o%d" % xt.buf.id)
            xsrc[b][t] = d_out[b, t * 128:(t + 1) * 128, :]

    def attention_group(l, ctab, stab):
        qt = W_bf[0]
        k1 = W_bf[1]
        k2 = W_bf[2]
        vtok = W_bf[3]
        sz = W_bf[4]
        ys = [W_bf[5], W_bf[6], W_bf[7], W_bf[8]]
        vt3 = T(vtok.ap.rearrange("p (t c) -> p t c", c=128), vtok.buf)
        for hd in range(4):
            wq = load_win(l, OFF["aq"] + hd * 128, 128)
            for g in range(NG):
                gs = slice(g * 512, (g + 1) * 512)
                ps = pbank()
                proj_fm(ps, wq, 0, 128, g)
                a_bf = tb()
                cp(a_bf, ps, eng="act")
                ps2 = pbank()
                mm(ps2, psw_b, a_bf)
                t1 = tf()
                tt(t1, ps2, stab[:, gs], ALU.mult)
                t2 = tf()
                tt(t2, ps, ctab[:, gs], ALU.mult)
                tt(qt[:, gs], t1, t2, ALU.add)
            wk = load_win(l, OFF["ak"] + hd * 128, 128)
            for g in range(NG):
                gs = slice(g * 512, (g + 1) * 512)
                ps = pbank()
                proj_fm(ps, wk, 0, 128, g)
                a_bf = tb()
                cp(a_bf, ps, eng="act")
                ps2 = pbank()
                mm(ps2, psw_b, a_bf)
                t1 = tf()
                tt(t1, ps2, stab[:, gs], ALU.mult)
                t2 = tf()
                tt(t2, ps, ctab[:, gs], ALU.mult)
                ktmp = tb()
                tt(ktmp, t1, t2, ALU.add)
                ts(k1[:, gs], ktmp, m1, ALU.mult, eng="pool")
                ts(k2[:, gs], ktmp, m2, ALU.mult, eng="pool")
            wv = load_win(l, OFF["av"] + hd * 128, 128)
            for g in range(NG):
                ps = pbank()
                for j in range(4):
                    proj_tm(ps[:, j * 128:(j + 1) * 128], wv, 0, 128, g * 4 + j)
                cp(T(vtok.ap[:, g * 512:(g + 1) * 512], vtok.buf), ps, eng="act")
            wz = load_win(l, OFF["az"] + hd * 128, 128)
            for g in range(NG):
                ps = pbank()
                proj_fm(ps, wz, 0, 128, g)
                act(sz[:, g * 512:(g + 1) * 512], ps, AF.Silu)
            kk = [k1, k2]
            for g in range(NG):
                gs = slice(g * 512, (g + 1) * 512)
                num = [banks[0], banks[1]]
                den = [banks[2], banks[3]]
                sc_banks = [banks[4], banks[5]]
                i_sc = 0
                for kt in range(NT):
                    for c in range(2):
                        pss = sc_banks[i_sc % 2]
                        i_sc += 1
                        mm(pss, kk[c][:, kt * 128:(kt + 1) * 128], qt[:, gs])
                        pt = tb()
                        act(pt, pss, AF.Exp, scale=0.125)
                        mm(num[c], vt3[:, kt, :], pt, start=(kt == 0), stop=(kt == NT - 1))
                        mm(den[c], ones_b, pt, start=(kt == 0), stop=(kt == NT - 1))
                r1 = tf()
                recip(r1, den[0])
                r2 = tf()
                recip(r2, den[1])
                o1 = tf()
                tt(o1, num[0], r1, ALU.mult)
                o2 = tf()
                tt(o2, num[1], r2, ALU.mult)
                o = tf()
                stt(o, o2, neglam[:, l:l + 1], o1, ALU.mult, ALU.add)
                rstd = tf()
                sumsq_rstd(rstd, [(o, 128)], 128, 512)
                y1 = o1
                tt(y1, o, rstd, ALU.mult)
                stt(ys[hd][:, gs], y1, gsa[:, l * 4 + hd:l * 4 + hd + 1], sz[:, gs], ALU.mult, ALU.mult)
        dbg_out("ya", ys[0], [128, S])
        out_proj(l, [((lambda g, hd=hd: ys[hd][:, g * 512:(g + 1) * 512]), 128, Y_OFF["a"] + hd * 128) for hd in range(4)])

    def hgrn_group(l):
        qT_ = W_bf[0]
        kT_ = W_bf[1]
        vtok = W_bf[2]
        vt3 = T(vtok.ap.rearrange("p (t c) -> p t c", c=128), vtok.buf)
        sz = W_bf[3]
        ys = [W_bf[4], W_bf[5], W_bf[6]]
        a_pad = W_f32[0]
        na_pad = W_f32[1]
        gtmp = W_f32[1]
        oT = W_f32[2]
        ek = [sb([128, 128], BF16, "hek%d" % i) for i in range(4)] if not hasattr(hgrn_group, "_t") else hgrn_group._t["ek"]
        if not hasattr(hgrn_group, "_t"):
            hgrn_group._t = dict(ek=ek)
            for i in range(4):
                memset(ek[i], 0.0)
            hgrn_group._t["eq"] = [sb([128, 128], F32, "heq%d" % i) for i in range(2)]
            hgrn_group._t["ekf"] = [sb([128, 128], F32, "hekf%d" % i) for i in range(2)]
            hgrn_group._t["qtl"] = [sb([128, 128], BF16, "hqtl%d" % i) for i in range(2)]
            hgrn_group._t["qc"] = [sb([128, 128], BF16, "hqc%d" % i) for i in range(2)]
            hgrn_group._t["kend"] = [sb([128, 128], F32, "hkend%d" % i) for i in range(2)]
            hgrn_group._t["kendT"] = [sb([128, 128], BF16, "hkendT%d" % i) for i in range(2)]
            hgrn_group._t["wT"] = [sb([128, 128], BF16, "hwT%d" % i) for i in range(2)]
            hgrn_group._t["Sst"] = sb([128, 128], F32, "hSst")
            hgrn_group._t["Sbf"] = sb([128, 128], BF16, "hSbf")
        tl = hgrn_group._t
        cnt = {"u": 0}
        for hd in range(6):
            wq = load_win(l, OFF["hq"] + hd * 128, 128)
            for g in range(NG):
                ps = pbank()
                proj_fm(ps, wq, 0, 128, g)
                cp(qT_[:, g * 512:(g + 1) * 512], ps, eng="act")
            wv = load_win(l, OFF["hi"] + hd * 128, 128)
            for g in range(NG):
                ps = pbank()
                for j in range(4):
                    proj_tm(ps[:, j * 128:(j + 1) * 128], wv, 0, 128, g * 4 + j)
                cp(T(vtok.ap[:, g * 512:(g + 1) * 512], vtok.buf), ps, eng="act")
            wz = load_win(l, OFF["hz"] + hd * 128, 128)
            for g in range(NG):
                ps = pbank()
                proj_fm(ps, wz, 0, 128, g)
                act(sz[:, g * 512:(g + 1) * 512], ps, AF.Silu)
            for dr in range(2):
                wf = load_win(l, OFF["hff" if dr == 0 else "hfb"] + hd * 128, 128)
                lbc = lbv[:, l * 12 + dr * 6 + hd:l * 12 + dr * 6 + hd + 1]
                olbc = omlb[:, l * 12 + dr * 6 + hd:l * 12 + dr * 6 + hd + 1]
                for g in range(NG):
                    gs = slice(g * 512, (g + 1) * 512)
                    ps = pbank()
                    proj_fm(ps, wf, 0, 128, g)
                    sg = tf()
                    act(sg, ps, AF.Sigmoid)
                    ff = tf()
                    ts(ff, sg, olbc, ALU.mult, lbc, ALU.add)
                    act(gtmp[:, gs], ff, AF.Ln)
                    ts(kT_[:, gs], ff, -1.0, ALU.mult, 1.0, ALU.add)
                memset(a_pad[:, 0:1], 0.0, eng="dve")
                scan_cumsum(a_pad[:, 1:S + 1], T(ones_f.ap[:, 0:1].to_broadcast([128, S]), ones_f.buf), gtmp[:, 0:S])
                ts(na_pad[:, 0:S + 1], a_pad[:, 0:S + 1], -1.0, ALU.mult)
                order = range(NT) if dr == 0 else range(NT - 1, -1, -1)
                first = True
                for c in order:
                    u = cnt["u"]
                    cnt["u"] += 1
                    c0 = c * 128
                    eq = tl["eq"][u % 2]
                    qtl = tl["qtl"][u % 2]
                    qc = tl["qc"][u % 2]
                    kend = tl["kend"][u % 2]
                    kendT = tl["kendT"][u % 2]
                    wT = tl["wT"][u % 2]
                    ekf = tl["ekf"][u % 2]
                    if dr == 0:
                        for I in range(4):
                            act(eq[:, 32 * I:32 * I + 32], a_pad[:, 1 + c0 + 32 * I:1 + c0 + 32 * I + 32], AF.Exp,
                                bias=na_pad[:, c0 + 32 * I:c0 + 32 * I + 1])
                        tt(qtl, qT_[:, c0:c0 + 128], eq, ALU.mult)
                        for I in range(4):
                            w_ = 32 * (I + 1)
                            act(ekf[:, 0:w_], na_pad[:, 1 + c0:1 + c0 + w_], AF.Exp, bias=a_pad[:, c0 + 32 * I:c0 + 32 * I + 1])
                            tt(ek[I][:, 0:w_], kT_[:, c0:c0 + w_], ekf[:, 0:w_], ALU.mult)
                        act(eq, a_pad[:, 1 + c0:1 + c0 + 128], AF.Exp, bias=na_pad[:, c0:c0 + 1])
                        tt(qc, qT_[:, c0:c0 + 128], eq, ALU.mult)
                        dec = eq[:, 127:128]
                        act(ekf, na_pad[:, 1 + c0:1 + c0 + 128], AF.Exp, bias=a_pad[:, c0 + 128:c0 + 129])
                        tt(kend, kT_[:, c0:c0 + 128], ekf, ALU.mult)
                        mask = U_f
                    else:
                        for I in range(4):
                            act(eq[:, 32 * I:32 * I + 32], na_pad[:, c0 + 32 * I:c0 + 32 * I + 32], AF.Exp,
                                bias=a_pad[:, c0 + 32 * (I + 1):c0 + 32 * (I + 1) + 1])
                        tt(qtl, qT_[:, c0:c0 + 128], eq, ALU.mult)
                        for I in range(4):
                            lo = 32 * I
                            act(ekf[:, lo:128], a_pad[:, c0 + lo:c0 + 128], AF.Exp,
                                bias=na_pad[:, c0 + 32 * (I + 1):c0 + 32 * (I + 1) + 1])
                            tt(ek[I][:, lo:128], kT_[:, c0 + lo:c0 + 128], ekf[:, lo:128], ALU.mult)
                        act(eq, na_pad[:, c0:c0 + 128], AF.Exp, bias=a_pad[:, c0 + 128:c0 + 129])
                        tt(qc, qT_[:, c0:c0 + 128], eq, ALU.mult)
                        dec = eq[:, 0:1]
                        act(ekf, a_pad[:, c0:c0 + 128], AF.Exp, bias=na_pad[:, c0:c0 + 1])
                        tt(kend, kT_[:, c0:c0 + 128], ekf, ALU.mult)
                        mask = L_f
                    pss = pquart()
                    for I in range(4):
                        mm(pss[:, 32 * I:32 * I + 32], ek[I], qtl[:, 32 * I:32 * I + 32])
                    tt(wT, pss, mask, ALU.mult)
                    pso = pquart()
                    mm(pso, vt3[:, c, :], wT, start=True, stop=first)
                    if not first:
                        mm(pso, tl["Sbf"], qc, start=False, stop=True)
                    if dr == 0:
                        cp(oT[:, c0:c0 + 128], pso, eng="act")
                    else:
                        tt(oT[:, c0:c0 + 128], oT[:, c0:c0 + 128], pso, ALU.add)
                    pst = pquart()
                    transp(pst, kend, ident_f)
                    cp(kendT, pst, eng="act")
                    psd = pquart()
                    mm(psd, kendT, vt3[:, c, :])
                    if first:
                        cp(tl["Sst"], psd)
                    else:
                        stt(tl["Sst"], tl["Sst"], dec, psd, ALU.mult, ALU.add)
                    cp(tl["Sbf"], tl["Sst"], eng="pool")
                    first = False
            yh = ys[hd % 3]
            for g in range(NG):
                gs = slice(g * 512, (g + 1) * 512)
                rstd = tf()
                sumsq_rstd(rstd, [(oT[:, gs], 128)], 128, 512)
                y1 = tf()
                tt(y1, oT[:, gs], rstd, ALU.mult)
                stt(yh[:, gs], y1, hgs[:, l * 6 + hd:l * 6 + hd + 1], sz[:, gs], ALU.mult, ALU.mult)
            if hd == 0:
                dbg_out("yh", yh, [128, S])
            if hd % 3 == 2:
                h0 = hd - 2
                out_proj(l, [((lambda g, j=j: ys[j][:, g * 512:(g + 1) * 512]), 128, Y_OFF["h"] + (h0 + j) * 128) for j in range(3)])

    def mlstm_group(l):
        xmA = W_f32[0]
        xmB = W_f32[1]
        cacc = W_f32[2]
        xcA = W_bf[0]
        xcB = W_bf[1]
        mqA, mqB, mkA, mkB = W_bf[2], W_bf[3], W_bf[4], W_bf[5]
        hfA, hfB = W_bf[6], W_bf[7]
        yA, yB = W_bf[8], W_bf[9]
        xmbA = sb([128, S], BF16, "xmbA") if not hasattr(mlstm_group, "_t") else mlstm_group._t["xmbA"]
        if not hasattr(mlstm_group, "_t"):
            t_ = dict(xmbA=xmbA)
            t_["xmbB"] = sb([128, S], BF16, "xmbB")
            t_["vtok"] = sb([128, NT, 200], BF16, "mvtok")
            t_["ktok"] = sb([128, NT, 192], BF16, "mktok")
            t_["gates"] = sb([128, NT, 16], F32, "mgates")
            t_["lf"] = sb([128, NT, 8], F32, "mlf")
            t_["eib"] = sb([128, NT, 8], F32, "meib")
            t_["bd"] = sb([128, 6 * 128], F32, "mbd")
            t_["bdB"] = sb([64, 6 * 64], F32, "mbdB")
            t_["bdb"] = sb([128, 3 * 128], BF16, "mbdb")
            t_["bdbB"] = sb([64, 3 * 64], BF16, "mbdbB")
            t_["wg"] = sb([128, 4 * 3 * 16], F32, "mwg")
            t_["wgB"] = sb([64, 4 * 3 * 16], F32, "mwgB")
            t_["G"] = sb([128, 4 * 2 * 16], BF16, "mG")
            t_["GB"] = sb([64, 4 * 2 * 16], BF16, "mGB")
            t_["lfrep"] = [sb([128, 128], F32, "mlfrep%d" % i) for i in range(2)]
            t_["Eb"] = [sb([128, 128], F32, "mEb%d" % i) for i in range(2)]
            t_["EbM"] = [sb([128, 128], F32, "mEbM%d" % i) for i in range(2)]
            t_["wT"] = [sb([128, 128], BF16, "mwT%d" % i) for i in range(2)]
            t_["qsA"] = [sb([128, 128], BF16, "mqsA%d" % i) for i in range(2)]
            t_["qsB"] = [sb([64, 128], BF16, "mqsB%d" % i) for i in range(2)]
            t_["dm"] = [sb([128, 128], F32, "mdm%d" % i) for i in range(2)]
            t_["rd"] = [sb([128, 128], F32, "mrd%d" % i) for i in range(2)]
            t_["CA"] = sb([128, 200], F32, "mCA")
            t_["CB"] = sb([64, 200], F32, "mCB")
            t_["CtA"] = sb([128, 200], F32, "mCtA")
            t_["CtB"] = sb([64, 200], F32, "mCtB")
            t_["CbA"] = sb([128, 200], BF16, "mCbA")
            t_["CbB"] = sb([64, 200], BF16, "mCbB")
            t_["nrA"] = sb([128, 128], BF16, "mnrA")
            t_["nrB"] = sb([64, 128], BF16, "mnrB")
            t_["eibrep"] = [sb([128, 128], BF16, "meibrep%d" % i) for i in range(2)]
            t_["hbA"] = sb([128, 512], F32, "mhbA")
            t_["hbB"] = sb([64, 512], F32, "mhbB")
            t_["so"] = [sb([128, 512], BF16, "mso%d" % i) for i in range(4)]
            mlstm_group._t = t_
        t_ = mlstm_group._t
        xmbB = t_["xmbB"]
        vtok, ktok, gates, lf, eib = t_["vtok"], t_["ktok"], t_["gates"], t_["lf"], t_["eib"]
        ts(mgsA[:, l * 4:(l + 1) * 4], mvA[:, l * 12 + 8:l * 12 + 12], float(math.sqrt(192.0)), ALU.mult)
        ts(mgsB[:, l * 4:(l + 1) * 4], mvB[:, l * 12 + 8:l * 12 + 12], float(math.sqrt(192.0)), ALU.mult)
        QSCALE = float(192.0 ** -0.5)

        def compute_xm_xc(j):
            for (xm_, xmb_, xc_, kk, coff, cw_, mv_) in ((xmA, xmbA, xcA, 128, 0, cwA, mvA), (xmB, xmbB, xcB, 64, 128, cwB, mvB)):
                wx = load_win(l, OFF["xm"] + j * 192 + coff, kk)
                memset(xm_[0:kk, 0:2], 0.0, eng="dve")
                memset(xm_[0:kk, S + 2:S + 4], 0.0, eng="dve")
                for g in range(NG):
                    ps = pbank()
                    proj_fm(ps, wx, 0, kk, g)
                    cp(xm_[0:kk, 2 + g * 512:2 + (g + 1) * 512], ps[0:kk, :], eng="act")
                    cp(xmb_[0:kk, g * 512:(g + 1) * 512], ps[0:kk, :], eng="act")
                cb = l * 20 + j * 5
                ts(cacc[0:kk, 0:S], xm_[0:kk, 0:S], cw_[0:kk, cb:cb + 1], ALU.mult, eng="pool")
                for k in range(1, 5):
                    stt(cacc[0:kk, 0:S], xm_[0:kk, k:k + S], cw_[0:kk, cb + k:cb + k + 1], cacc[0:kk, 0:S],
                        ALU.mult, ALU.add)
                act(xc_[0:kk, 0:S], cacc[0:kk, 0:S], AF.Silu, bias=mv_[0:kk, l * 12 + j:l * 12 + j + 1])

        dma_in(t_["wg"], d_wgA[l])
        dma_in(t_["wgB"], d_wgB[l])
        psg = banks[0]
        psg3 = T(psg.ap[:, 0:NT * 16].rearrange("p (t c) -> p t c", c=16), psg.buf)
        for j in range(4):
            dma_in(t_["bd"], d_bdA[l, j])
            dma_in(t_["bdB"], d_bdB[l, j])
            for (bd_, wg_, G_, kk) in ((t_["bd"], t_["wg"], t_["G"], 128), (t_["bdB"], t_["wgB"], t_["GB"], 64)):
                pq = pquart()
                mm(pq[0:kk, 0:16], bd_[0:kk, 3 * kk:4 * kk], wg_[0:kk, (j * 3 + 0) * 16:(j * 3 + 1) * 16], start=True, stop=False)
                mm(pq[0:kk, 0:16], bd_[0:kk, 4 * kk:5 * kk], wg_[0:kk, (j * 3 + 1) * 16:(j * 3 + 2) * 16], start=False, stop=True)
                mm(pq[0:kk, 16:32], bd_[0:kk, 5 * kk:6 * kk], wg_[0:kk, (j * 3 + 2) * 16:(j * 3 + 3) * 16], start=True, stop=True)
                cp(G_[0:kk, j * 32:(j + 1) * 32], pq[0:kk, 0:32])
            compute_xm_xc(j)
            for t in range(NT):
                tsl = slice(t * 128, (t + 1) * 128)
                mm(psg3[:, t, :], xcA[:, tsl], t_["G"][:, j * 32:j * 32 + 16], start=True, stop=False)
                mm(psg3[:, t, :], xmbA[:, tsl], t_["G"][:, j * 32 + 16:j * 32 + 32], start=False, stop=False)
                mm(psg3[:, t, :], xcB[0:64, tsl], t_["GB"][0:64, j * 32:j * 32 + 16], start=False, stop=False)
                mm(psg3[:, t, :], xmbB[0:64, tsl], t_["GB"][0:64, j * 32 + 16:j * 32 + 32], start=False, stop=True)
            if j == 0:
                for t in range(NT):
                    tt(gates[:, t, :], psg3[:, t, :], bgs[:, l * 16:(l + 1) * 16], ALU.add)
            else:
                tt(gates, gates, psg3, ALU.add)
        etmp = sb([128, NT, 8], F32, "metmp") if "etmp" not in t_ else t_["etmp"]
        t_["etmp"] = etmp
        act(etmp[:, :, 0:4], gates[:, :, 4:8], AF.Exp, scale=-1.0)
        act(etmp[:, :, 4:8], gates[:, :, 12:16], AF.Exp, scale=-1.0)
        act(lf, etmp, AF.Ln, bias=one_c)
        ts(lf, lf, -1.0, ALU.mult)
        bcs = sb([128, NT, 8], F32, "mbcs") if "bcs" not in t_ else t_["bcs"]
        t_["bcs"] = bcs
        psb = banks[1]
        psb3 = T(psb.ap[:, 0:NT * 8].rearrange("p (t c) -> p t c", c=8), psb.buf)
        for t in range(NT):
            mm(psb3[:, t, 0:4], U_f, lf[:, t, 0:4])
            mm(psb3[:, t, 4:8], L_f, lf[:, t, 4:8])
        cp(bcs, psb3)
        tt(etmp[:, :, 0:4], gates[:, :, 0:4], bcs[:, :, 0:4], ALU.subtract)
        tt(etmp[:, :, 4:8], gates[:, :, 8:12], bcs[:, :, 4:8], ALU.subtract)
        act(eib, etmp, AF.Exp)
        dbg_out("gates", T(gates.ap.rearrange("p t c -> p (t c)"), gates.buf), [128, NT * 16], F32)

        cntu = {"u": 0}
        for j in range(4):
            dma_in(t_["bd"], d_bdA[l, j])
            dma_in(t_["bdB"], d_bdB[l, j])
            cp(t_["bdb"], t_["bd"][:, 0:384], eng="pool")
            cp(t_["bdbB"], t_["bdB"][:, 0:192], eng="pool")
            bdb, bdbB = t_["bdb"], t_["bdbB"]
            compute_xm_xc(j)
            for g in range(NG):
                gs = slice(g * 512, (g + 1) * 512)
                for (dst, src, w_, kk, sc) in ((mqA, xcA, bdb[:, 0:128], 128, QSCALE), (mqB, xcB, bdbB[:, 0:64], 64, QSCALE),
                                               (mkA, xcA, bdb[:, 128:256], 128, 1.0), (mkB, xcB, bdbB[:, 64:128], 64, 1.0)):
                    ps = pbank()
                    mm(ps[0:kk, :], w_[0:kk, :], src[0:kk, gs])
                    act(dst[0:kk, gs], ps[0:kk, :], AF.Copy, scale=sc)
            for t in range(NT):
                tsl = slice(t * 128, (t + 1) * 128)
                ps = pbank()
                mm(ps[:, 0:128], xmbA[:, tsl], bdb[:, 256:384])
                mm(ps[:, 128:192], xmbB[0:64, tsl], bdbB[0:64, 128:192])
                mm(ps[:, 192:320], xcA[:, tsl], bdb[:, 128:256])
                mm(ps[:, 320:384], xcB[0:64, tsl], bdbB[0:64, 64:128])
                cp(vtok[:, t, 0:192], ps[:, 0:192], eng="act")
                cp(ktok[:, t, :], ps[:, 192:384], eng="act")
            memset(vtok[:, :, 192:193], 1.0, eng="dve")
            vp = sb([128, 200], BF16, "mvp0") if "vp" not in t_ else t_["vp"][0]
            if "vp" not in t_:
                t_["vp"] = [vp, sb([128, 200], BF16, "mvp1")]
            for dr in range(2):
                order = range(NT) if dr == 0 else range(NT - 1, -1, -1)
                gcol = dr * 4 + j
                tri = U_f if dr == 0 else L_f
                mneg = mnegF if dr == 0 else mnegB
                first = True
                for c in order:
                    u = cntu["u"]
                    cntu["u"] += 1
                    csl = slice(c * 128, (c + 1) * 128)
                    Eb, EbM, wT = t_["Eb"][u % 2], t_["EbM"][u % 2], t_["wT"][u % 2]
                    qsA, qsB = t_["qsA"][u % 2], t_["qsB"][u % 2]
                    dm, rd = t_["dm"][u % 2], t_["rd"][u % 2]
                    lfrep = t_["lfrep"][u % 2]
                    vpc = t_["vp"][u % 2]
                    eibrep = t_["eibrep"][u % 2]
                    ts(vpc[:, 0:193], vtok[:, c, 0:193], eib[:, c, gcol:gcol + 1], ALU.mult, eng="pool")
                    ts(eibrep, ones_f, eib[:, c, gcol:gcol + 1], ALU.mult, eng="pool")
                    bk1 = pbank()
                    pss = bk1[:, 0:128]
                    psl = bk1[:, 128:256]
                    pslm = bk1[:, 256:384]
                    mm(pss, mkA[:, csl], mqA[:, csl], start=True, stop=False)
                    mm(pss, mkB[0:64, csl], mqB[0:64, csl], start=False, stop=True)
                    ts(lfrep, ones_f, lf[:, c, gcol:gcol + 1], ALU.mult, eng="pool")
                    mm(psl, lfrep, tri)
                    mm(pslm, lfrep, tri, start=True, stop=False)
                    mm(pslm, ident_f, mneg, start=False, stop=True)
                    act(Eb, psl, AF.Exp)
                    act(EbM, pslm, AF.Exp)
                    tt(wT, pss, EbM, ALU.mult)
                    bk2 = pbank()
                    pnA = bk2[:, 0:128]
                    pnB = bk2[:, 128:256]
                    pdn = bk2[:, 256:384]
                    if not first:
                        tt(qsA, mqA[:, csl], Eb, ALU.mult)
                        tt(qsB, mqB[0:64, csl], Eb[0:64, :], ALU.mult)
                    mm(pnA, vpc[:, 0:128], wT, start=True, stop=first)
                    if not first:
                        mm(pnA, t_["CbA"][:, 0:128], qsA, start=False, stop=False)
                        mm(pnA, t_["CbB"][:, 0:128], qsB, start=False, stop=True)
                    mm(pnB[0:64, :], vpc[:, 128:192], wT, start=True, stop=first)
                    if not first:
                        mm(pnB[0:64, :], t_["CbA"][:, 128:192], qsA, start=False, stop=False)
                        mm(pnB[0:64, :], t_["CbB"][:, 128:192], qsB, start=False, stop=True)
                    mm(pdn, eibrep, wT, start=True, stop=first)
                    if not first:
                        mm(pdn, t_["nrA"], qsA, start=False, stop=False)
                        mm(pdn, t_["nrB"], qsB, start=False, stop=True)
                    ts(rd, pdn, -1.0, ALU.mult, 1.0, ALU.max)
                    stt(dm, pdn, 1.0, rd, ALU.max, ALU.max)
                    recip(rd, dm)
                    if dr == 0:
                        tt(hfA[:, csl], pnA, rd, ALU.mult)
                        tt(hfB[0:64, csl], pnB[0:64, :], rd[0:64, :], ALU.mult)
                    else:
                        o4 = (c % 4) * 128
                        tt(t_["hbA"][:, o4:o4 + 128], pnA, rd, ALU.mult)
                        tt(t_["hbB"][0:64, o4:o4 + 128], pnB[0:64, :], rd[0:64, :], ALU.mult)
                    ebl = Eb[:, 127:128] if dr == 0 else Eb[:, 0:1]
                    bk3 = pbank()
                    pcA = bk3[:, 0:256]
                    pcB = bk3[:, 256:512]
                    mm(pcA[:, 0:193], ktok[:, c, 0:128], vpc[:, 0:193])
                    mm(pcB[0:64, 0:193], ktok[:, c, 128:192], vpc[:, 0:193])
                    if first:
                        act(t_["CA"][:, 0:193], pcA[:, 0:193], AF.Copy, scale=ebl)
                        act(t_["CB"][:, 0:193], pcB[0:64, 0:193], AF.Copy, scale=ebl[0:64, :])
                    else:
                        tt(t_["CtA"][:, 0:193], pcA[:, 0:193], t_["CA"][:, 0:193], ALU.add)
                        tt(t_["CtB"][:, 0:193], pcB[0:64, 0:193], t_["CB"][:, 0:193], ALU.add)
                        act(t_["CA"][:, 0:193], t_["CtA"][:, 0:193], AF.Copy, scale=ebl)
                        act(t_["CB"][:, 0:193], t_["CtB"][:, 0:193], AF.Copy, scale=ebl[0:64, :])
                    cp(t_["CbA"][:, 0:193], t_["CA"][:, 0:193], eng="pool")
                    cp(t_["CbB"][:, 0:193], t_["CB"][:, 0:193], eng="pool")
                    ts(t_["nrA"], ones_f, t_["CA"][:, 192:193], ALU.mult, eng="pool")
                    ts(t_["nrB"], ones_f[0:64, :], t_["CB"][:, 192:193], ALU.mult, eng="pool")
                    first = False
                    if dr == 1 and c % 4 == 0:
                        g = c // 4
                        gs = slice(g * 512, (g + 1) * 512)
                        hbA, hbB = t_["hbA"], t_["hbB"]
                        tt(hbA, hbA, hfA[:, gs], ALU.add)
                        tt(hbB, hbB, hfB[0:64, gs], ALU.add)
                        so = t_["so"]
                        for (idx, nm, coff, kk, fn) in ((0, "om", 0, 128, AF.Sigmoid), (1, "om", 128, 64, AF.Sigmoid),
                                                        (2, "zm", 0, 128, AF.Silu), (3, "zm", 128, 64, AF.Silu)):
                            wz = load_win(l, OFF[nm] + j * 192 + coff, kk)
                            ps = pbank()
                            proj_fm(ps, wz, 0, kk, g)
                            act(so[idx][0:kk, :], ps[0:kk, :], fn)
                        tt(hbA, hbA, so[0], ALU.mult)
                        tt(hbB, hbB, so[1][0:64, :], ALU.mult)
                        rstd = tf()
                        sumsq_rstd(rstd, [(hbA, 128), (hbB, 64)], 192, 512)
                        tt(hbA, hbA, rstd, ALU.mult)
                        tt(hbB, hbB, rstd[0:64, :], ALU.mult)
                        skA = tf()
                        skB = tf()
                        ts(skA, xcA[:, gs], mvA[:, l * 12 + 4 + j:l * 12 + 5 + j], ALU.mult, eng="pool")
                        ts(skB[0:64, :], xcB[0:64, gs], mvB[:, l * 12 + 4 + j:l * 12 + 5 + j], ALU.mult, eng="pool")
                        stt(hbA, hbA, mgsA[:, l * 4 + j:l * 4 + j + 1], skA, ALU.mult, ALU.add)
                        stt(hbB, hbB, mgsB[:, l * 4 + j:l * 4 + j + 1], skB[0:64, :], ALU.mult, ALU.add)
                        tt(yA[:, gs], hbA, so[2], ALU.mult)
                        tt(yB[0:64, gs], hbB, so[3][0:64, :], ALU.mult)
            if j == 0:
                dbg_out("ym", yA, [128, S])
            out_proj(l, [((lambda g: yA[:, g * 512:(g + 1) * 512]), 128, Y_OFF["m"] + j * 192),
                         ((lambda g: yB[0:64, g * 512:(g + 1) * 512]), 64, Y_OFF["m"] + j * 192 + 128)])

    cur = {"b": 0}
    for l in range(NL):
        lb_for_layer(l, l)

    for b in range(NSEQ):
        cur["b"] = b
        xsrc[b] = [d_x[b, t * 128:(t + 1) * 128, :] for t in range(NT)]
        for l in range(NL):
            for t in range(NT):
                g = t // 4
                o = (t % 4) * 128
                rrx["i"] = (rrx["i"] + 1) % 2
                xt = xt_tiles[rrx["i"]]
                P.op("sp", (lambda dst, s_: (lambda e: e.dma_start(out=dst.ap, in_=s_)))(xt, xsrc[b][t]),
                     reads=[xdr[b][t]], writes=[xt], is_dma=True, dkey=xt.buf.id)
                xi = xin[t % 2]
                P.op("act", (lambda o_, i_, a_: (lambda e: e.activation(out=o_.ap, in_=i_.ap, func=AF.Square, accum_out=a_.ap)))(xi, xt, ssq[:, 0:1]),
                     reads=[xt], writes=[xi, ssq])
                rstd_from(ssq[:, 1:2], ssq[:, 0:1], 1024)
                ts(xi, xt, ssq[:, 1:2], ALU.mult)
                for kq in range(2):
                    ps = pbank()
                    for k4 in range(4):
                        k = kq * 4 + k4
                        transp(ps[:, k4 * 128:(k4 + 1) * 128], xi[:, k * 128:(k + 1) * 128], ident_f)
                    for k4 in range(4):
                        k = kq * 4 + k4
                        ts(hTg[k][g][:, o:o + 128], ps[:, k4 * 128:(k4 + 1) * 128], ng32[:, l * 8 + k:l * 8 + k + 1], ALU.mult)
            if "a" in groups:
                ctab = W_f32[0]
                stab = W_f32[1]
                angt = W_f32[2]
                posi = T(angt.ap[:, 0:S].bitcast(I32), angt.buf)
                dma_in(posi, d_pos[b:b + 1, :].to_broadcast([128, S]))
                cp(angt[:, 0:S], posi)
                ts(angt[:, 0:S], angt[:, 0:S], invf, ALU.mult)
                ts(ctab[:, 0:S], angt[:, 0:S], float(1.0 / (2 * math.pi)), ALU.mult)
                ki = T(stab.ap[:, 0:S].bitcast(I32), stab.buf)
                cp(ki, ctab[:, 0:S])
                cp(ctab[:, 0:S], ki)
                stt(angt[:, 0:S], ctab[:, 0:S], float(-2 * math.pi), angt[:, 0:S], ALU.mult, ALU.add)
                act(stab[:, 0:S], angt[:, 0:S], AF.Sin, scale=0.25)
                tt(stab[:, 0:S], stab[:, 0:S], stab[:, 0:S], ALU.mult)
                ts(stab[:, 0:S], stab[:, 0:S], -2.0, ALU.mult, 1.0, ALU.add)
                act(angt[:, 0:S], angt[:, 0:S], AF.Sin, scale=0.5)
                stt(stab[:, 0:S], angt[:, 0:S], 2.0, stab[:, 0:S], ALU.mult, ALU.mult)
                ts(stab[:, 0:S], stab[:, 0:S], sgn, ALU.mult)
                tt(ctab[:, 0:S], angt[:, 0:S], angt[:, 0:S], ALU.mult)
                ts(ctab[:, 0:S], ctab[:, 0:S], -2.0, ALU.mult, 1.0, ALU.add)
                attention_group(l, ctab, stab)
            if "h" in groups:
                hgrn_group(l)
            if "m" in groups:
                mlstm_group(l)
        if final_norm:
            dma_in(fgb, d_fgrow[0:1, :].to_broadcast([128, 1024]))
            ts(fgb, fgb, 32.0, ALU.mult)
        for t in range(NT):
            rrx["i"] = (rrx["i"] + 1) % 2
            xt = xt_tiles[rrx["i"]]
            P.op("sp", (lambda dst, s_: (lambda e: e.dma_start(out=dst.ap, in_=s_)))(xt, xsrc[b][t]),
                 reads=[xdr[b][t]], writes=[xt], is_dma=True, dkey=xt.buf.id)
            if final_norm:
                xi = xin[t % 2]
                P.op("act", (lambda o_, i_, a_: (lambda e: e.activation(out=o_.ap, in_=i_.ap, func=AF.Square, accum_out=a_.ap)))(xi, xt, ssq[:, 2:3]),
                     reads=[xt], writes=[xi, ssq])
                rstd_from(ssq[:, 3:4], ssq[:, 2:3], 1024)
                stt(xt, xt, ssq[:, 3:4], fgb, ALU.mult, ALU.mult)
            tok = T(None)
            P.op("sp", (lambda src_, d_: (lambda e: e.dma_start(out=d_, in_=src_.ap)))(xt, d_out[b, t * 128:(t + 1) * 128, :]),
                 reads=[xt], writes=[xdr[b][t], tok], is_dma=True, dkey="o%d" % xt.buf.id)
            fin.append(tok)
    P.op("sp", None, reads=fin)
    P.emit(nc, ctx)
    ctx.close()
    return nc, P, dbg_outs


def prep_params(inp, layers, depth_all):
    NL = len(layers)
    f = np.float32
    out = {}
    out["w_in"] = np.ascontiguousarray(inp["w_in"][layers], dtype=f)
    out["w_out"] = np.ascontiguousarray(inp["w_out"][layers], dtype=f)
    out["consts"] = make_consts()
    ng = inp["norm_g"][layers].reshape(NL, 8, 128)
    out["ng"] = np.ascontiguousarray(ng.transpose(2, 0, 1).reshape(128, NL * 8), dtype=f)
    out["fgrow"] = np.ascontiguousarray(inp["final_g"].reshape(1, 1024), dtype=f)
    out["alam"] = np.ascontiguousarray(inp["a_lambda"][layers].reshape(1, NL * 256), dtype=f)
    out["ang"] = np.ascontiguousarray(inp["a_norm_g"][layers].reshape(NL, 4, 128).transpose(2, 0, 1).reshape(128, NL * 4), dtype=f)
    lb = inp["h_lb_logits"].reshape(depth_all, 2, 6, 128)
    out["lb"] = np.ascontiguousarray(lb.transpose(3, 1, 2, 0).reshape(128, 12 * depth_all), dtype=f)
    out["hng"] = np.ascontiguousarray(inp["h_norm_g"][layers].reshape(NL, 6, 128).transpose(2, 0, 1).reshape(128, NL * 6), dtype=f)
    cw = inp["m_conv_w"][layers].reshape(NL, 5, 4, 192)
    out["cwA"] = np.ascontiguousarray(cw[:, :, :, 0:128].transpose(3, 0, 2, 1).reshape(128, NL * 20), dtype=f)
    out["cwB"] = np.ascontiguousarray(cw[:, :, :, 128:192].transpose(3, 0, 2, 1).reshape(64, NL * 20), dtype=f)
    mv = np.stack([inp["m_conv_b"][layers], inp["m_skip"][layers], inp["m_norm_g"][layers]], axis=1)
    mv = mv.reshape(NL, 3, 4, 192)
    out["mvA"] = np.ascontiguousarray(mv[:, :, :, 0:128].transpose(3, 0, 1, 2).reshape(128, NL * 12), dtype=f)
    out["mvB"] = np.ascontiguousarray(mv[:, :, :, 128:192].transpose(3, 0, 1, 2).reshape(64, NL * 12), dtype=f)
    bdA = np.zeros((NL, 4, 128, 6, 128), f)
    bdB = np.zeros((NL, 4, 64, 6, 64), f)
    for wi, nm in enumerate(("m_wq", "m_wk", "m_wv")):
        w = inp[nm][layers].reshape(NL, 4, 48, 4, 4)
        for g_ in range(32):
            bdA[:, :, 4 * g_:4 * g_ + 4, wi, 4 * g_:4 * g_ + 4] = w[:, :, g_]
            bdA[:, :, 4 * g_:4 * g_ + 4, 3 + wi, 4 * g_:4 * g_ + 4] = w[:, :, g_].transpose(0, 1, 3, 2)
        for g_ in range(16):
            bdB[:, :, 4 * g_:4 * g_ + 4, wi, 4 * g_:4 * g_ + 4] = w[:, :, 32 + g_]
            bdB[:, :, 4 * g_:4 * g_ + 4, 3 + wi, 4 * g_:4 * g_ + 4] = w[:, :, 32 + g_].transpose(0, 1, 3, 2)
    out["bdA"] = bdA.reshape(NL, 4, 128, 6 * 128)
    out["bdB"] = bdB.reshape(NL, 4, 64, 6 * 64)
    wg = inp["m_w_gates"][layers].reshape(NL, 3, 4, 192, 16)
    out["wgA"] = np.ascontiguousarray(wg[:, :, :, 0:128].transpose(0, 3, 2, 1, 4).reshape(NL, 128, 4 * 3 * 16), dtype=f)
    out["wgB"] = np.ascontiguousarray(wg[:, :, :, 128:192].transpose(0, 3, 2, 1, 4).reshape(NL, 64, 4 * 3 * 16), dtype=f)
    out["bg"] = np.ascontiguousarray(inp["m_b_gates"][layers].reshape(1, NL * 16), dtype=f)
    return out


_CACHE = {}


def _get_prog(S, NSEQ, NL, depth_all, lam_inits, final_norm):
    key = (S, NSEQ, NL, depth_all, tuple(lam_inits), final_norm)
    if key not in _CACHE:
        _CACHE[key] = build(S, NSEQ, NL, depth_all, lam_inits, final_norm=final_norm)[0]
    return _CACHE[key]


def kernel(**inputs):
    inp = {k: np.asarray(v) for k, v in inputs.items()}
    x = np.ascontiguousarray(inp["x"], dtype=np.float32)
    pos = np.ascontiguousarray(inp["positions"], dtype=np.int32)
    B, S, D = x.shape
    DEPTH = inp["w_in"].shape[0]
    NCORE = 8
    per = B // NCORE
    lam_all = [0.8 - 0.6 * math.exp(-0.3 * l) for l in range(DEPTH)]
    params = prep_params(inp, list(range(DEPTH)), DEPTH)
    nc = _get_prog(S, per, DEPTH, DEPTH, lam_all, True)
    in_maps = []
    for c in range(NCORE):
        m = dict(params)
        m["x"] = x[c * per:(c + 1) * per]
        m["pos"] = pos[c * per:(c + 1) * per]
        in_maps.append(m)
    res = run_bass_kernel_spmd(nc, in_maps, core_ids=list(range(NCORE)))
    out = np.concatenate([np.asarray(r["out"]) for r in res.results], axis=0)
    return out.astype(np.float32)
```

```python
import math
from contextlib import ExitStack
import numpy as np
import concourse.bass as bass
import concourse.mybir as mybir
from concourse.bass_utils import run_bass_kernel_spmd

F32 = mybir.dt.float32
BF16 = mybir.dt.bfloat16
I32 = mybir.dt.int32
ALU = mybir.AluOpType
AF = mybir.ActivationFunctionType
AX = mybir.AxisListType

ENGS = ("pe", "act", "dve", "pool", "sp")
SEM_WRAP = 30000
EPS = 1e-6


class Buf:
    __slots__ = ("last_w", "readers", "id")
    _n = 0

    def __init__(self):
        self.last_w = None
        self.readers = []
        Buf._n += 1
        self.id = Buf._n


class T:
    __slots__ = ("ap", "buf")

    def __init__(self, ap, buf=None):
        self.ap = ap
        self.buf = buf if buf is not None else Buf()

    def __getitem__(self, idx):
        return T(self.ap[idx], self.buf)

    def sub(self, idx):
        return T(self.ap[idx], Buf())


class Op:
    __slots__ = ("eng", "fn", "deps", "idx", "is_dma", "sig", "dkey")


class Prog:
    def __init__(self):
        self.ops = []

    def op(self, eng, fn, reads=(), writes=(), is_dma=False, dkey=None):
        o = Op()
        o.eng = eng
        o.fn = fn
        o.is_dma = is_dma
        o.dkey = dkey
        o.sig = None
        o.idx = len(self.ops)
        deps = set()
        for t in reads:
            b = t.buf
            if b.last_w is not None:
                deps.add(b.last_w)
        for t in writes:
            b = t.buf
            if b.last_w is not None:
                deps.add(b.last_w)
            deps.update(b.readers)
        for t in reads:
            t.buf.readers.append(o.idx)
        for t in writes:
            t.buf.last_w = o.idx
            t.buf.readers = []
        deps.discard(o.idx)
        o.deps = deps
        self.ops.append(o)
        return o

    def emit(self, nc, ctx):
        ops = self.ops
        needed = set()
        for o in ops:
            best = {}
            for d in o.deps:
                p = ops[d]
                if p.eng == "pe" and o.eng == "pe" and not p.is_dma and not o.is_dma:
                    continue
                key = ("dma", p.dkey) if p.is_dma else ("eng", p.eng)
                if key not in best or best[key] < d:
                    best[key] = d
            o.deps = sorted(best.values())
            needed.update(o.deps)
        counters = {}
        sems = {}

        def getsem(name):
            if name not in sems:
                sems[name] = ctx.enter_context(nc.semaphore(name))
            return sems[name]

        for o in ops:
            if o.idx not in needed and not (o.is_dma and o.fn is not None):
                continue
            if o.is_dma:
                base = "d%s" % (o.dkey,)
                inc = 16
            else:
                base = "e" + o.eng
                inc = 1
            cnt, epoch = counters.get(base, (0, 0))
            if cnt + inc > SEM_WRAP:
                epoch += 1
                cnt = 0
            cnt += inc
            counters[base] = (cnt, epoch)
            o.sig = ("%s_%d" % (base, epoch), cnt, inc)
        for o in ops:
            if o.sig:
                getsem(o.sig[0])
        self.n_sems = len(sems)
        per_eng = {e: [] for e in ENGS}
        for o in ops:
            per_eng[o.eng].append(o)
        block = ctx.enter_context(nc.Block())

        def run(eng_obj, lst):
            last_wait = {}
            for o in lst:
                for d in o.deps:
                    sname, val, _ = ops[d].sig
                    if last_wait.get(sname, 0) >= val:
                        continue
                    last_wait[sname] = val
                    eng_obj.wait_ge(sems[sname], val)
                if o.fn is None:
                    continue
                ins = o.fn(eng_obj)
                if o.sig is not None:
                    ins.then_inc(sems[o.sig[0]], o.sig[2])

        @block.tensor
        def _(e):
            run(e, per_eng["pe"])

        @block.scalar
        def _(e):
            run(e, per_eng["act"])

        @block.vector
        def _(e):
            run(e, per_eng["dve"])

        @block.gpsimd
        def _(e):
            run(e, per_eng["pool"])

        @block.sync
        def _(e):
            run(e, per_eng["sp"])


D_MODEL = 1024
D_MIX = 2048
M_W = 768
H_W = 768
A_W = 512
IN_COLS = 8192
OFF = dict(xm=0, om=768, zm=1536, hq=2304, hff=3072, hfb=3840, hi=4608, hz=5376,
           aq=6144, ak=6656, av=7168, az=7680)
Y_OFF = dict(m=0, h=768, a=1536)
ROPE_THETA = 500000.0
NEG = -30000.0

C_IDENT = 0
C_U = 128
C_L = 256
C_PSW = 384
C_ONES = 512
C_VEC = 640
NCONST = 648


def make_consts():
    c = np.zeros((128, NCONST), np.float32)
    r = np.arange(128)
    c[:, C_IDENT:C_IDENT + 128] = np.eye(128, dtype=np.float32)
    c[:, C_U:C_U + 128] = (r[:, None] <= r[None, :]).astype(np.float32)
    c[:, C_L:C_L + 128] = (r[:, None] >= r[None, :]).astype(np.float32)
    psw = np.zeros((128, 128), np.float32)
    for m in range(128):
        d = m % 64
        if d < 8:
            psw[m + 8, m] = 1.0
        elif d < 16:
            psw[m - 8, m] = 1.0
    c[:, C_PSW:C_PSW + 128] = psw
    c[:, C_ONES:C_ONES + 128] = 1.0
    half = 8
    inv = ROPE_THETA ** (-np.arange(half, dtype=np.float32) / half)
    for p in range(128):
        d = p % 64
        if d < 16:
            c[p, C_VEC + 0] = inv[d % 8]
            c[p, C_VEC + 1] = -1.0 if d < 8 else 1.0
        c[p, C_VEC + 2] = 1.0 if p < 64 else 0.0
        c[p, C_VEC + 3] = 0.0 if p < 64 else 1.0
    c[:, C_VEC + 4] = 1024 * EPS
    c[:, C_VEC + 5] = 128 * EPS
    c[:, C_VEC + 6] = 192 * EPS
    c[:, C_VEC + 7] = 1.0
    return c


def build(S, NSEQ, NL, DEPTH_ALL, lam_inits, final_norm=True, groups=("a", "h", "m"), dbg=()):
    NT = S // 128
    NG = S // 512
    nc = bass.Bass("TRN2", target_bir_lowering=False)
    P = Prog()
    ctx = ExitStack()

    def dram(name, shape, dt=F32, kind="ExternalInput"):
        return nc.dram_tensor(name, shape, dt, kind=kind).ap()

    d_x = dram("x", [NSEQ, S, D_MODEL])
    d_pos = dram("pos", [NSEQ, S], I32)
    d_win = dram("w_in", [NL, D_MODEL, IN_COLS])
    d_wout = dram("w_out", [NL, D_MIX, D_MODEL])
    d_consts = dram("consts", [128, NCONST])
    d_ng = dram("ng", [128, NL * 8])
    d_fgrow = dram("fgrow", [1, 1024])
    d_alam = dram("alam", [1, NL * 256])
    d_ang = dram("ang", [128, NL * 4])
    d_lb = dram("lb", [128, 2 * 6 * DEPTH_ALL])
    d_hng = dram("hng", [128, NL * 6])
    d_cwA = dram("cwA", [128, NL * 4 * 5])
    d_cwB = dram("cwB", [64, NL * 4 * 5])
    d_mvA = dram("mvA", [128, NL * 4 * 3])
    d_mvB = dram("mvB", [64, NL * 4 * 3])
    d_bdA = dram("bdA", [NL, 4, 128, 6 * 128])
    d_bdB = dram("bdB", [NL, 4, 64, 6 * 64])
    d_wgA = dram("wgA", [NL, 128, 4 * 3 * 16])
    d_wgB = dram("wgB", [NL, 64, 4 * 3 * 16])
    d_bg = dram("bg", [1, NL * 16])
    d_out = dram("out", [NSEQ, S, D_MODEL], kind="ExternalOutput")
    dbg_outs = {}

    def sb(shape, dt=F32, name=None):
        return T(ctx.enter_context(nc.sbuf_tensor("s_" + name, shape, dt))[:])

    def dma_in(dst, src_ap, eng="sp"):
        P.op(eng, lambda e: e.dma_start(out=dst.ap, in_=src_ap), writes=[dst], is_dma=True, dkey=dst.buf.id)

    def dma_out(dst_ap, src, eng="sp"):
        tok = T(None)
        P.op(eng, lambda e: e.dma_start(out=dst_ap, in_=src.ap), reads=[src], writes=[tok], is_dma=True,
             dkey="o%d" % src.buf.id)
        return tok

    def mm(out, lhsT, rhs, start=True, stop=True, extra_reads=()):
        P.op("pe", lambda e: e.matmul(out.ap, lhsT=lhsT.ap, rhs=rhs.ap, start=start, stop=stop),
             reads=[lhsT, rhs] + list(extra_reads), writes=[out])

    def transp(out, in_, ident):
        P.op("pe", lambda e: e.transpose(out=out.ap, in_=in_.ap, identity=ident.ap), reads=[in_, ident], writes=[out])

    def act(out, in_, func, bias=None, scale=None, eng="act", extra_reads=()):
        kw = {}
        rd = [in_] + list(extra_reads)
        if bias is not None:
            if isinstance(bias, T):
                kw["bias"] = bias.ap
                rd.append(bias)
            else:
                kw["bias"] = bias
        if scale is not None:
            if isinstance(scale, T):
                kw["scale"] = scale.ap
                rd.append(scale)
            else:
                kw["scale"] = scale
        P.op(eng, lambda e: e.activation(out=out.ap, in_=in_.ap, func=func, **kw), reads=rd, writes=[out])

    def tt(out, a, b, op, eng="dve"):
        P.op(eng, lambda e: e.tensor_tensor(out=out.ap, in0=a.ap, in1=b.ap, op=op), reads=[a, b], writes=[out])

    def ts(out, a, s1, op0, s2=None, op1=None, eng="dve"):
        rd = [a]
        v1 = s1
        v2 = s2
        if isinstance(s1, T):
            rd.append(s1)
            v1 = s1.ap
        if isinstance(s2, T):
            rd.append(s2)
            v2 = s2.ap
        if op1 is None:
            P.op(eng, lambda e: e.tensor_scalar(out=out.ap, in0=a.ap, scalar1=v1, scalar2=None, op0=op0),
                 reads=rd, writes=[out])
        else:
            P.op(eng, lambda e: e.tensor_scalar(out=out.ap, in0=a.ap, scalar1=v1, scalar2=v2, op0=op0, op1=op1),
                 reads=rd, writes=[out])

    def stt(out, a, s, b, op0, op1, eng="dve"):
        rd = [a, b]
        v = s
        if isinstance(s, T):
            rd.append(s)
            v = s.ap
        P.op(eng, lambda e: e.scalar_tensor_tensor(out=out.ap, in0=a.ap, scalar=v, in1=b.ap, op0=op0, op1=op1),
             reads=rd, writes=[out])

    def cp(out, in_, eng="dve"):
        if eng == "act":
            P.op("act", lambda e: e.copy(out=out.ap, in_=in_.ap), reads=[in_], writes=[out])
        else:
            P.op(eng, lambda e: e.tensor_copy(out=out.ap, in_=in_.ap), reads=[in_], writes=[out])

    def memset(out, val, eng="pool"):
        P.op(eng, lambda e: e.memset(out.ap, val), writes=[out])

    def recip(out, in_):
        P.op("dve", lambda e: e.reciprocal(out=out.ap, in_=in_.ap), reads=[in_], writes=[out])

    def scan_cumsum(out, ones, in_):
        P.op("dve", lambda e: e.tensor_tensor_scan(out=out.ap, data0=ones.ap, data1=in_.ap, initial=0.0,
                                                   op0=ALU.mult, op1=ALU.add), reads=[ones, in_], writes=[out])

    def dbg_out(name, src, shape, dt=BF16):
        if name not in dbg:
            return
        d = dram("dbg_" + name, shape, dt, kind="ExternalOutput")
        dbg_outs[name] = d
        fin.append(dma_out(d, src))

    fin = []

    banks = [T(ctx.enter_context(nc.psum_tensor("pb%d" % i, [128, 512], F32))[:]) for i in range(8)]
    rr = {"b": 0, "x": 0}
    XB = (5, 6, 7)
    excl = {"on": False}
    SKEW = 2

    def pbank():
        while True:
            rr["b"] = (rr["b"] + 1) % 8
            if excl["on"] and rr["b"] in XB:
                continue
            return banks[rr["b"]]

    def xbank():
        rr["x"] = (rr["x"] + 1) % len(XB)
        return banks[XB[rr["x"]]]

    def pquart():
        return pbank()[:, 0:128]

    def run_pipeline(units, stages, skew):
        n = len(units)
        K = len(stages)
        for step in range(n + (K - 1) * skew):
            for s in reversed(range(K)):
                ui = step - s * skew
                if 0 <= ui < n:
                    stages[s](units[ui])

    consts_f = sb([128, NCONST], F32, "consts_f")
    consts_b = sb([128, 640], BF16, "consts_b")
    dma_in(consts_f, d_consts)
    cp(consts_b, consts_f[:, 0:640])
    ident_f = consts_f[:, C_IDENT:C_IDENT + 128]
    U_f = consts_f[:, C_U:C_U + 128]
    L_f = consts_f[:, C_L:C_L + 128]
    psw_b = consts_b[:, C_PSW:C_PSW + 128]
    ones_b = consts_b[:, C_ONES:C_ONES + 128]
    ones_f = consts_f[:, C_ONES:C_ONES + 128]
    U_b = consts_b[:, C_U:C_U + 128]
    L_b = consts_b[:, C_L:C_L + 128]
    invf = consts_f[:, C_VEC + 0:C_VEC + 1]
    sgn = consts_f[:, C_VEC + 1:C_VEC + 2]
    m1 = consts_f[:, C_VEC + 2:C_VEC + 3]
    m2 = consts_f[:, C_VEC + 3:C_VEC + 4]
    epsc = {1024: consts_f[:, C_VEC + 4:C_VEC + 5], 128: consts_f[:, C_VEC + 5:C_VEC + 6], 192: consts_f[:, C_VEC + 6:C_VEC + 7]}
    one_c = consts_f[:, C_VEC + 7:C_VEC + 8]

    def rstd_from(out, v, n):
        act(out, v, AF.Ln, bias=epsc[n][0:out.ap.shape[0], :])
        act(out, out, AF.Exp, scale=-0.5)
    mnegF = sb([128, 128], F32, "mnegF")
    mnegB = sb([128, 128], F32, "mnegB")
    ts(mnegF, U_f, 1.0, ALU.subtract, -NEG, ALU.mult)
    ts(mnegB, L_f, 1.0, ALU.subtract, -NEG, ALU.mult)

    ng = sb([128, NL * 8], F32, "ng")
    dma_in(ng, d_ng)
    ng32 = sb([128, NL * 8], F32, "ng32")
    ts(ng32, ng, 32.0, ALU.mult)
    ang = sb([128, NL * 4], F32, "angs")
    dma_in(ang, d_ang)
    lbl = sb([128, 2 * 6 * DEPTH_ALL], F32, "lbl")
    dma_in(lbl, d_lb)
    hng = sb([128, NL * 6], F32, "hngs")
    dma_in(hng, d_hng)
    cwA = sb([128, NL * 20], F32, "cwA")
    dma_in(cwA, d_cwA)
    cwB = sb([64, NL * 20], F32, "cwB")
    dma_in(cwB, d_cwB)
    mvA = sb([128, NL * 12], F32, "mvA")
    dma_in(mvA, d_mvA)
    mvB = sb([64, NL * 12], F32, "mvB")
    dma_in(mvB, d_mvB)
    bgs = sb([128, NL * 16], F32, "bgs")
    dma_in(bgs, d_bg[0:1, :].to_broadcast([128, NL * 16]))

    neglam = sb([128, NL], F32, "neglam")
    gsa = sb([128, NL * 4], F32, "gsa")
    lamtmp = sb([128, 64], F32, "lamtmp")
    lam2 = sb([128, 4], F32, "lam2")
    alam_t = sb([128, 256], F32, "alam")
    for l in range(NL):
        dma_in(alam_t, d_alam[0:1, l * 256:(l + 1) * 256].to_broadcast([128, 256]))
        for j in range(2):
            tt(lamtmp, alam_t[:, j * 128:j * 128 + 64],
               alam_t[:, j * 128 + 64:j * 128 + 128], ALU.mult)
            P.op("dve", (lambda o, i: (lambda e: e.reduce_sum(out=o.ap, in_=i.ap, axis=AX.X)))(lam2[:, j:j + 1], lamtmp),
                 reads=[lamtmp], writes=[lam2])
        act(lam2[:, 2:4], lam2[:, 0:2], AF.Exp)
        tt(lam2[:, 0:1], lam2[:, 3:4], lam2[:, 2:3], ALU.subtract)
        ts(neglam[:, l:l + 1], lam2[:, 0:1], -float(lam_inits[l]), ALU.add)
        ts(gsa[:, l * 4:(l + 1) * 4], ang[:, l * 4:(l + 1) * 4], float((1.0 - lam_inits[l]) * math.sqrt(128.0)), ALU.mult)
    NLB = 12 * DEPTH_ALL
    lbe = sb([128, NLB], F32, "lbe")
    act(lbe, lbl, AF.Exp)
    lbs = sb([128, 12], F32, "lbs")
    P.op("dve", lambda e: e.reduce_sum(out=lbs.ap, in_=lbe.ap.rearrange("p (a l) -> p a l", l=DEPTH_ALL), axis=AX.X),
         reads=[lbe], writes=[lbs])
    lbr = sb([128, 12], F32, "lbr")
    recip(lbr, lbs)
    lbv = sb([128, NL * 12], F32, "lbv")
    omlb = sb([128, NL * 12], F32, "omlb")
    lbt = sb([128, 12], F32, "lbt")

    def lb_for_layer(l, lglob):
        dst = lbv[:, l * 12:(l + 1) * 12]
        if lglob == 0:
            memset(dst, 0.0, eng="dve")
        else:
            P.op("dve", lambda e: e.reduce_sum(
                out=lbt.ap, in_=lbe.ap.rearrange("p (a l) -> p a l", l=DEPTH_ALL)[:, :, 1:lglob + 1], axis=AX.X),
                reads=[lbe], writes=[lbt])
            tt(dst, lbt, lbr, ALU.mult)
        ts(omlb[:, l * 12:(l + 1) * 12], dst, -1.0, ALU.mult, 1.0, ALU.add)

    hgs = sb([128, NL * 6], F32, "hgs")
    ts(hgs, hng, float(math.sqrt(128.0)), ALU.mult)
    mgsA = sb([128, NL * 4], F32, "mgsA")
    mgsB = sb([64, NL * 4], F32, "mgsB")

    xdr = [[T(None) for t in range(NT)] for b in range(NSEQ)]
    xsrc = {}
    hT = [sb([128, S], BF16, "hT%d" % k) for k in range(8)]
    hTg = [[hT[k].sub((slice(None), slice(g * 512, (g + 1) * 512))) for g in range(NG)] for k in range(8)]
    W_bf = [sb([128, S], BF16, "wbf%d" % i) for i in range(10)]
    W_f32 = [sb([128, max(S + 4, 1024)], F32, "wf%d" % i) for i in range(3)]
    xin = [W_f32[0][:, 0:1024], W_f32[1][:, 0:1024]]
    fgb = W_f32[2][:, 0:1024]
    xt_tiles = [sb([128, 1024], F32, "xtile%d" % i) for i in range(2)]
    rrx = {"i": 0}
    ssq = sb([128, 4], F32, "ssq")
    tmpf = [sb([128, 512], F32, "tmpf%d" % i) for i in range(5)]
    tmpb = [sb([128, 512], BF16, "tmpb%d" % i) for i in range(5)]
    rrt = {"f": 0, "b": 0}

    def tf():
        rrt["f"] = (rrt["f"] + 1) % len(tmpf)
        return tmpf[rrt["f"]]

    def tb():
        rrt["b"] = (rrt["b"] + 1) % len(tmpb)
        return tmpb[rrt["b"]]

    NWB = 3
    wbf = [sb([128, 8, 128], BF16, "wbf16_%d" % i) for i in range(NWB)]
    rrw = {"s": 0, "b": 0, "os": 0, "ob": 0}
    wo_bf = [sb([128, 1024], BF16, "wobf%d" % i) for i in range(4)]

    def load_win(l, col0, ncols):
        rrw["b"] = (rrw["b"] + 1) % NWB
        wb = wbf[rrw["b"]]
        src_ = d_win[l, :, col0:col0 + ncols].rearrange("(k p) c -> p k c", p=128)
        dma_in(wb[:, :, 0:ncols], src_, eng="pool")
        return wb

    def load_wout(l, row0, nrows):
        rrw["ob"] = (rrw["ob"] + 1) % 4
        wb = wo_bf[rrw["ob"]]
        dma_in(wb[0:nrows, :], d_wout[l, row0:row0 + nrows, :], eng="pool")
        return wb

    def proj_fm(ps, wb, c0, ncols, g):
        for k in range(8):
            mm(ps[0:ncols, :], wb[:, k, c0:c0 + ncols], hTg[k][g], start=(k == 0), stop=(k == 7))

    def proj_tm(ps_view, wb, c0, ncols, tt_):
        g = tt_ // 4
        o = (tt_ % 4) * 128
        for k in range(8):
            mm(ps_view, hTg[k][g][:, o:o + 128], wb[:, k, c0:c0 + ncols], start=(k == 0), stop=(k == 7))

    def sumsq_rstd(rstd_out, srcs, n, g_cols):
        ps = pbank()
        for i, (s, kk) in enumerate(srcs):
            sq = tb()
            act(sq[0:kk, 0:g_cols], s, AF.Square)
            mm(ps[:, 0:g_cols], ones_b[0:kk, :], sq[0:kk, 0:g_cols], start=(i == 0), stop=(i == len(srcs) - 1))
        rstd_from(rstd_out, ps[:, 0:g_cols], n)

    def out_proj(l, ysrcs):
        b = cur["b"]
        wts = [load_wout(l, r0, kk) for (_, kk, r0) in ysrcs]
        for t in range(NT):
            g = t // 4
            o = (t % 4) * 128
            rrx["i"] = (rrx["i"] + 1) % 2
            xt = xt_tiles[rrx["i"]]
            P.op("sp", (lambda dst, s_: (lambda e: e.dma_start(out=dst.ap, in_=s_)))(xt, xsrc[b][t]),
                 reads=[xdr[b][t]], writes=[xt], is_dma=True, dkey=xt.buf.id)
            for hf in range(2):
                ps = pbank()
                for i, (yf, kk, r0) in enumerate(ysrcs):
                    mm(ps, yf(g)[:, o:o + 128], wts[i][0:kk, hf * 512:(hf + 1) * 512], start=(i == 0), stop=(i == len(ysrcs) - 1))
                tt(xt[:, hf * 512:(hf + 1) * 512], xt[:, hf * 512:(hf + 1) * 512], ps, ALU.add)
            P.op("sp", (lambda src_, d_: (lambda e: e.dma_start(out=d_, in_=src_.ap)))(xt, d_out[b, t * 128:(t + 1) * 128, :]),
                 reads=[xt], writes=[xdr[b][t]], is_dma=True, dkey="o%d" % xt.buf.id)
            xsrc[b][t] = d_out[b, t * 128:(t + 1) * 128, :]

    def attention_group(l, ctab, stab):
        qt = W_bf[0]
        k1 = W_bf[1]
        k2 = W_bf[2]
        vtok = W_bf[3]
        sz = W_bf[4]
        ys = [W_bf[5], W_bf[6], W_bf[7], W_bf[8]]
        vt3 = T(vtok.ap.rearrange("p (t c) -> p t c", c=128), vtok.buf)
        for hd in range(4):
            wq = load_win(l, OFF["aq"] + hd * 128, 128)
            for g in range(NG):
                gs = slice(g * 512, (g + 1) * 512)
                ps = pbank()
                proj_fm(ps, wq, 0, 128, g)
                a_bf = tb()
                cp(a_bf, ps, eng="act")
                ps2 = pbank()
                mm(ps2, psw_b, a_bf)
                t1 = tf()
                tt(t1, ps2, stab[:, gs], ALU.mult)
                t2 = tf()
                tt(t2, ps, ctab[:, gs], ALU.mult)
                tt(qt[:, gs], t1, t2, ALU.add)
            wk = load_win(l, OFF["ak"] + hd * 128, 128)
            for g in range(NG):
                gs = slice(g * 512, (g + 1) * 512)
                ps = pbank()
                proj_fm(ps, wk, 0, 128, g)
                a_bf = tb()
                cp(a_bf, ps, eng="act")
                ps2 = pbank()
                mm(ps2, psw_b, a_bf)
                t1 = tf()
                tt(t1, ps2, stab[:, gs], ALU.mult)
                t2 = tf()
                tt(t2, ps, ctab[:, gs], ALU.mult)
                ktmp = tb()
                tt(ktmp, t1, t2, ALU.add)
                ts(k1[:, gs], ktmp, m1, ALU.mult, eng="pool")
                ts(k2[:, gs], ktmp, m2, ALU.mult, eng="pool")
            wv = load_win(l, OFF["av"] + hd * 128, 128)
            for g in range(NG):
                ps = pbank()
                for j in range(4):
                    proj_tm(ps[:, j * 128:(j + 1) * 128], wv, 0, 128, g * 4 + j)
                cp(T(vtok.ap[:, g * 512:(g + 1) * 512], vtok.buf), ps, eng="act")
            wz = load_win(l, OFF["az"] + hd * 128, 128)
            for g in range(NG):
                ps = pbank()
                proj_fm(ps, wz, 0, 128, g)
                act(sz[:, g * 512:(g + 1) * 512], ps, AF.Silu)
            kk = [k1, k2]
            for g in range(NG):
                gs = slice(g * 512, (g + 1) * 512)
                num = [banks[0], banks[1]]
                den = [banks[2], banks[3]]
                sc_banks = [banks[4], banks[5]]
                i_sc = 0
                for kt in range(NT):
                    for c in range(2):
                        pss = sc_banks[i_sc % 2]
                        i_sc += 1
                        mm(pss, kk[c][:, kt * 128:(kt + 1) * 128], qt[:, gs])
                        pt = tb()
                        act(pt, pss, AF.Exp, scale=0.125)
                        mm(num[c], vt3[:, kt, :], pt, start=(kt == 0), stop=(kt == NT - 1))
                        mm(den[c], ones_b, pt, start=(kt == 0), stop=(kt == NT - 1))
                r1 = tf()
                recip(r1, den[0])
                r2 = tf()
                recip(r2, den[1])
                o1 = tf()
                tt(o1, num[0], r1, ALU.mult)
                o2 = tf()
                tt(o2, num[1], r2, ALU.mult)
                o = tf()
                stt(o, o2, neglam[:, l:l + 1], o1, ALU.mult, ALU.add)
                rstd = tf()
                sumsq_rstd(rstd, [(o, 128)], 128, 512)
                y1 = o1
                tt(y1, o, rstd, ALU.mult)
                stt(ys[hd][:, gs], y1, gsa[:, l * 4 + hd:l * 4 + hd + 1], sz[:, gs], ALU.mult, ALU.mult)
        dbg_out("ya", ys[0], [128, S])
        out_proj(l, [((lambda g, hd=hd: ys[hd][:, g * 512:(g + 1) * 512]), 128, Y_OFF["a"] + hd * 128) for hd in range(4)])

    def hgrn_group(l):
        qT_ = W_bf[0]
        kT_ = W_bf[1]
        vtok = W_bf[2]
        vt3 = T(vtok.ap.rearrange("p (t c) -> p t c", c=128), vtok.buf)
        sz = W_bf[3]
        ys = [W_bf[4], W_bf[5], W_bf[6]]
        a_pad = W_f32[0]
        na_pad = W_f32[1]
        gtmp = W_f32[1]
        oT = W_f32[2]
        NS0 = 2
        NSXH = 4
        if not hasattr(hgrn_group, "_t"):
            tl_ = dict()
            tl_["ek"] = [[sb([128, 128], BF16, "hek%d_%d" % (s_, i)) for i in range(4)] for s_ in range(NS0)]
            for s_ in range(NS0):
                for i in range(4):
                    memset(tl_["ek"][s_][i], 0.0)
            tl_["eq"] = [sb([128, 128], F32, "heq%d" % i) for i in range(NS0)]
            tl_["ekf"] = [sb([128, 128], F32, "hekf%d" % i) for i in range(NS0)]
            tl_["qtl"] = [sb([128, 128], BF16, "hqtl%d" % i) for i in range(NS0)]
            tl_["kend"] = [sb([128, 128], F32, "hkend%d" % i) for i in range(NS0)]
            tl_["kendT"] = [sb([128, 128], BF16, "hkendT%d" % i) for i in range(NS0)]
            tl_["qc"] = [sb([128, 128], BF16, "hqc%d" % i) for i in range(NSXH)]
            tl_["wT"] = [sb([128, 128], BF16, "hwT%d" % i) for i in range(NSXH)]
            tl_["dec"] = sb([128, NSXH], F32, "hdec")
            tl_["Sst"] = sb([128, 128], F32, "hSst")
            tl_["Sbf"] = sb([128, 128], BF16, "hSbf")
            hgrn_group._t = tl_
        tl = hgrn_group._t
        cnt = {"u": 0}
        for hd in range(6):
            wq = load_win(l, OFF["hq"] + hd * 128, 128)
            for g in range(NG):
                ps = pbank()
                proj_fm(ps, wq, 0, 128, g)
                cp(qT_[:, g * 512:(g + 1) * 512], ps, eng="act")
            wv = load_win(l, OFF["hi"] + hd * 128, 128)
            for g in range(NG):
                ps = pbank()
                for j in range(4):
                    proj_tm(ps[:, j * 128:(j + 1) * 128], wv, 0, 128, g * 4 + j)
                cp(T(vtok.ap[:, g * 512:(g + 1) * 512], vtok.buf), ps, eng="act")
            wz = load_win(l, OFF["hz"] + hd * 128, 128)
            for g in range(NG):
                ps = pbank()
                proj_fm(ps, wz, 0, 128, g)
                act(sz[:, g * 512:(g + 1) * 512], ps, AF.Silu)
            for dr in range(2):
                wf = load_win(l, OFF["hff" if dr == 0 else "hfb"] + hd * 128, 128)
                lbc = lbv[:, l * 12 + dr * 6 + hd:l * 12 + dr * 6 + hd + 1]
                olbc = omlb[:, l * 12 + dr * 6 + hd:l * 12 + dr * 6 + hd + 1]
                for g in range(NG):
                    gs = slice(g * 512, (g + 1) * 512)
                    ps = pbank()
                    proj_fm(ps, wf, 0, 128, g)
                    sg = tf()
                    act(sg, ps, AF.Sigmoid)
                    ff = tf()
                    ts(ff, sg, olbc, ALU.mult, lbc, ALU.add)
                    act(gtmp[:, gs], ff, AF.Ln)
                    ts(kT_[:, gs], ff, -1.0, ALU.mult, 1.0, ALU.add)
                memset(a_pad[:, 0:1], 0.0, eng="dve")
                scan_cumsum(a_pad[:, 1:S + 1], T(ones_f.ap[:, 0:1].to_broadcast([128, S]), ones_f.buf), gtmp[:, 0:S])
                ts(na_pad[:, 0:S + 1], a_pad[:, 0:S + 1], -1.0, ALU.mult)
                order = list(range(NT)) if dr == 0 else list(range(NT - 1, -1, -1))
                units = []
                for i_, c in enumerate(order):
                    units.append(dict(c=c, first=(i_ == 0), u=cnt["u"]))
                    cnt["u"] += 1

                def st0(un, dr=dr):
                    c, u = un["c"], un["u"]
                    c0 = c * 128
                    s0 = u % NS0
                    sx = u % NSXH
                    eq, ekf, qtl, kend, kendT = tl["eq"][s0], tl["ekf"][s0], tl["qtl"][s0], tl["kend"][s0], tl["kendT"][s0]
                    ek = tl["ek"][s0]
                    qc, wT = tl["qc"][sx], tl["wT"][sx]
                    dcol = tl["dec"][:, sx:sx + 1]
                    if dr == 0:
                        for I in range(4):
                            act(eq[:, 32 * I:32 * I + 32], a_pad[:, 1 + c0 + 32 * I:1 + c0 + 32 * I + 32], AF.Exp,
                                bias=na_pad[:, c0 + 32 * I:c0 + 32 * I + 1])
                        tt(qtl, qT_[:, c0:c0 + 128], eq, ALU.mult)
                        for I in range(4):
                            w_ = 32 * (I + 1)
                            act(ekf[:, 0:w_], na_pad[:, 1 + c0:1 + c0 + w_], AF.Exp, bias=a_pad[:, c0 + 32 * I:c0 + 32 * I + 1])
                            tt(ek[I][:, 0:w_], kT_[:, c0:c0 + w_], ekf[:, 0:w_], ALU.mult)
                        act(eq, a_pad[:, 1 + c0:1 + c0 + 128], AF.Exp, bias=na_pad[:, c0:c0 + 1])
                        tt(qc, qT_[:, c0:c0 + 128], eq, ALU.mult)
                        act(ekf, na_pad[:, 1 + c0:1 + c0 + 128], AF.Exp, bias=a_pad[:, c0 + 128:c0 + 129])
                        tt(kend, kT_[:, c0:c0 + 128], ekf, ALU.mult)
                        mask = U_f
                    else:
                        for I in range(4):
                            act(eq[:, 32 * I:32 * I + 32], na_pad[:, c0 + 32 * I:c0 + 32 * I + 32], AF.Exp,
                                bias=a_pad[:, c0 + 32 * (I + 1):c0 + 32 * (I + 1) + 1])
                        tt(qtl, qT_[:, c0:c0 + 128], eq, ALU.mult)
                        for I in range(4):
                            lo = 32 * I
                            act(ekf[:, lo:128], a_pad[:, c0 + lo:c0 + 128], AF.Exp,
                                bias=na_pad[:, c0 + 32 * (I + 1):c0 + 32 * (I + 1) + 1])
                            tt(ek[I][:, lo:128], kT_[:, c0 + lo:c0 + 128], ekf[:, lo:128], ALU.mult)
                        act(eq, na_pad[:, c0:c0 + 128], AF.Exp, bias=a_pad[:, c0 + 128:c0 + 129])
                        tt(qc, qT_[:, c0:c0 + 128], eq, ALU.mult)
                        act(ekf, a_pad[:, c0:c0 + 128], AF.Exp, bias=na_pad[:, c0:c0 + 1])
                        tt(kend, kT_[:, c0:c0 + 128], ekf, ALU.mult)
                        mask = L_f
                    act(dcol, a_pad[:, c0 + 128:c0 + 129], AF.Exp, bias=na_pad[:, c0:c0 + 1])
                    pb = pbank()
                    pss = pb[:, 0:128]
                    pst = pb[:, 128:256]
                    for I in range(4):
                        mm(pss[:, 32 * I:32 * I + 32], ek[I], qtl[:, 32 * I:32 * I + 32])
                    tt(wT, pss, mask, ALU.mult)
                    transp(pst, kend, ident_f)
                    cp(kendT, pst, eng="act")
                    psd = xbank()[:, 0:128]
                    mm(psd, kendT, vt3[:, c, :])
                    un.update(qc=qc, wT=wT, dcol=dcol, psd=psd)

                def st1(un, dr=dr):
                    c, first = un["c"], un["first"]
                    c0 = c * 128
                    qc, wT, dcol, psd = un["qc"], un["wT"], un["dcol"], un["psd"]
                    pso = pquart()
                    mm(pso, vt3[:, c, :], wT, start=True, stop=first)
                    if not first:
                        mm(pso, tl["Sbf"], qc, start=False, stop=True)
                    if first:
                        cp(tl["Sst"], psd)
                    else:
                        stt(tl["Sst"], tl["Sst"], dcol, psd, ALU.mult, ALU.add)
                    cp(tl["Sbf"], tl["Sst"], eng="act")
                    if dr == 0:
                        cp(oT[:, c0:c0 + 128], pso, eng="act")
                    else:
                        tt(oT[:, c0:c0 + 128], oT[:, c0:c0 + 128], pso, ALU.add)

                run_pipeline(units, [st0, st1], SKEW)
            yh = ys[hd % 3]
            for g in range(NG):
                gs = slice(g * 512, (g + 1) * 512)
                rstd = tf()
                sumsq_rstd(rstd, [(oT[:, gs], 128)], 128, 512)
                y1 = tf()
                tt(y1, oT[:, gs], rstd, ALU.mult)
                stt(yh[:, gs], y1, hgs[:, l * 6 + hd:l * 6 + hd + 1], sz[:, gs], ALU.mult, ALU.mult)
            if hd == 0:
                dbg_out("yh", yh, [128, S])
            if hd % 3 == 2:
                h0 = hd - 2
                out_proj(l, [((lambda g, j=j: ys[j][:, g * 512:(g + 1) * 512]), 128, Y_OFF["h"] + (h0 + j) * 128) for j in range(3)])

    def mlstm_group(l):
        xmA = W_f32[0]
        xmB = W_f32[1]
        cacc = W_f32[2]
        xcA = W_bf[0]
        xcB = W_bf[1]
        mqA, mqB, mkA, mkB = W_bf[2], W_bf[3], W_bf[4], W_bf[5]
        hfA, hfB = W_bf[6], W_bf[7]
        yA, yB = W_bf[8], W_bf[9]
        xmbA = sb([128, S], BF16, "xmbA") if not hasattr(mlstm_group, "_t") else mlstm_group._t["xmbA"]
        if not hasattr(mlstm_group, "_t"):
            t_ = dict(xmbA=xmbA)
            t_["xmbB"] = sb([128, S], BF16, "xmbB")
            t_["vtok"] = sb([128, NT, 200], BF16, "mvtok")
            t_["ktok"] = sb([128, NT, 192], BF16, "mktok")
            t_["gates"] = sb([128, NT, 16], F32, "mgates")
            t_["lf"] = sb([128, NT, 8], F32, "mlf")
            t_["eib"] = sb([128, NT, 8], F32, "meib")
            t_["bd"] = sb([128, 6 * 128], F32, "mbd")
            t_["bdB"] = sb([64, 6 * 64], F32, "mbdB")
            t_["bdb"] = sb([128, 3 * 128], BF16, "mbdb")
            t_["bdbB"] = sb([64, 3 * 64], BF16, "mbdbB")
            t_["wg"] = sb([128, 4 * 3 * 16], F32, "mwg")
            t_["wgB"] = sb([64, 4 * 3 * 16], F32, "mwgB")
            t_["G"] = sb([128, 4 * 2 * 16], BF16, "mG")
            t_["GB"] = sb([64, 4 * 2 * 16], BF16, "mGB")
            t_["lfrep"] = [sb([128, 128], F32, "mlfrep%d" % i) for i in range(2)]
            t_["Eb"] = [sb([128, 128], F32, "mEb%d" % i) for i in range(2)]
            t_["EbM"] = [sb([128, 128], F32, "mEbM%d" % i) for i in range(2)]
            t_["wT"] = [sb([128, 128], BF16, "mwT%d" % i) for i in range(2)]
            t_["qsA"] = [sb([128, 128], BF16, "mqsA%d" % i) for i in range(2)]
            t_["qsB"] = [sb([64, 128], BF16, "mqsB%d" % i) for i in range(2)]
            t_["dm"] = [sb([128, 128], F32, "mdm%d" % i) for i in range(2)]
            t_["rd"] = [sb([128, 128], F32, "mrd%d" % i) for i in range(2)]
            t_["CA"] = sb([128, 200], F32, "mCA")
            t_["CB"] = sb([64, 200], F32, "mCB")
            t_["CtA"] = sb([128, 200], F32, "mCtA")
            t_["CtB"] = sb([64, 200], F32, "mCtB")
            t_["CbA"] = sb([128, 200], BF16, "mCbA")
            t_["CbB"] = sb([64, 200], BF16, "mCbB")
            t_["nrA"] = sb([128, 128], BF16, "mnrA")
            t_["nrB"] = sb([64, 128], BF16, "mnrB")
            t_["eibrep"] = [sb([128, 128], BF16, "meibrep%d" % i) for i in range(2)]
            t_["hbA"] = sb([128, 512], F32, "mhbA")
            t_["hbB"] = sb([64, 512], F32, "mhbB")
            t_["so"] = [sb([128, 512], BF16, "mso%d" % i) for i in range(4)]
            mlstm_group._t = t_
        t_ = mlstm_group._t
        xmbB = t_["xmbB"]
        vtok, ktok, gates, lf, eib = t_["vtok"], t_["ktok"], t_["gates"], t_["lf"], t_["eib"]
        ts(mgsA[:, l * 4:(l + 1) * 4], mvA[:, l * 12 + 8:l * 12 + 12], float(math.sqrt(192.0)), ALU.mult)
        ts(mgsB[:, l * 4:(l + 1) * 4], mvB[:, l * 12 + 8:l * 12 + 12], float(math.sqrt(192.0)), ALU.mult)
        QSCALE = float(192.0 ** -0.5)

        def compute_xm_xc(j):
            for (xm_, xmb_, xc_, kk, coff, cw_, mv_) in ((xmA, xmbA, xcA, 128, 0, cwA, mvA), (xmB, xmbB, xcB, 64, 128, cwB, mvB)):
                wx = load_win(l, OFF["xm"] + j * 192 + coff, kk)
                memset(xm_[0:kk, 0:2], 0.0, eng="dve")
                memset(xm_[0:kk, S + 2:S + 4], 0.0, eng="dve")
                for g in range(NG):
                    ps = pbank()
                    proj_fm(ps, wx, 0, kk, g)
                    cp(xm_[0:kk, 2 + g * 512:2 + (g + 1) * 512], ps[0:kk, :], eng="act")
                    cp(xmb_[0:kk, g * 512:(g + 1) * 512], ps[0:kk, :], eng="act")
                cb = l * 20 + j * 5
                ts(cacc[0:kk, 0:S], xm_[0:kk, 0:S], cw_[0:kk, cb:cb + 1], ALU.mult, eng="pool")
                for k in range(1, 5):
                    stt(cacc[0:kk, 0:S], xm_[0:kk, k:k + S], cw_[0:kk, cb + k:cb + k + 1], cacc[0:kk, 0:S],
                        ALU.mult, ALU.add)
                act(xc_[0:kk, 0:S], cacc[0:kk, 0:S], AF.Silu, bias=mv_[0:kk, l * 12 + j:l * 12 + j + 1])

        dma_in(t_["wg"], d_wgA[l])
        dma_in(t_["wgB"], d_wgB[l])
        psg = banks[0]
        psg3 = T(psg.ap[:, 0:NT * 16].rearrange("p (t c) -> p t c", c=16), psg.buf)
        for j in range(4):
            dma_in(t_["bd"], d_bdA[l, j])
            dma_in(t_["bdB"], d_bdB[l, j])
            for (bd_, wg_, G_, kk) in ((t_["bd"], t_["wg"], t_["G"], 128), (t_["bdB"], t_["wgB"], t_["GB"], 64)):
                pq = pquart()
                mm(pq[0:kk, 0:16], bd_[0:kk, 3 * kk:4 * kk], wg_[0:kk, (j * 3 + 0) * 16:(j * 3 + 1) * 16], start=True, stop=False)
                mm(pq[0:kk, 0:16], bd_[0:kk, 4 * kk:5 * kk], wg_[0:kk, (j * 3 + 1) * 16:(j * 3 + 2) * 16], start=False, stop=True)
                mm(pq[0:kk, 16:32], bd_[0:kk, 5 * kk:6 * kk], wg_[0:kk, (j * 3 + 2) * 16:(j * 3 + 3) * 16], start=True, stop=True)
                cp(G_[0:kk, j * 32:(j + 1) * 32], pq[0:kk, 0:32])
            compute_xm_xc(j)
            for t in range(NT):
                tsl = slice(t * 128, (t + 1) * 128)
                mm(psg3[:, t, :], xcA[:, tsl], t_["G"][:, j * 32:j * 32 + 16], start=True, stop=False)
                mm(psg3[:, t, :], xmbA[:, tsl], t_["G"][:, j * 32 + 16:j * 32 + 32], start=False, stop=False)
                mm(psg3[:, t, :], xcB[0:64, tsl], t_["GB"][0:64, j * 32:j * 32 + 16], start=False, stop=False)
                mm(psg3[:, t, :], xmbB[0:64, tsl], t_["GB"][0:64, j * 32 + 16:j * 32 + 32], start=False, stop=True)
            if j == 0:
                for t in range(NT):
                    tt(gates[:, t, :], psg3[:, t, :], bgs[:, l * 16:(l + 1) * 16], ALU.add)
            else:
                tt(gates, gates, psg3, ALU.add)
        etmp = sb([128, NT, 8], F32, "metmp") if "etmp" not in t_ else t_["etmp"]
        t_["etmp"] = etmp
        act(etmp[:, :, 0:4], gates[:, :, 4:8], AF.Exp, scale=-1.0)
        act(etmp[:, :, 4:8], gates[:, :, 12:16], AF.Exp, scale=-1.0)
        act(lf, etmp, AF.Ln, bias=one_c)
        ts(lf, lf, -1.0, ALU.mult)
        bcs = sb([128, NT, 8], F32, "mbcs") if "bcs" not in t_ else t_["bcs"]
        t_["bcs"] = bcs
        psb = banks[1]
        psb3 = T(psb.ap[:, 0:NT * 8].rearrange("p (t c) -> p t c", c=8), psb.buf)
        for t in range(NT):
            mm(psb3[:, t, 0:4], U_f, lf[:, t, 0:4])
            mm(psb3[:, t, 4:8], L_f, lf[:, t, 4:8])
        cp(bcs, psb3)
        tt(etmp[:, :, 0:4], gates[:, :, 0:4], bcs[:, :, 0:4], ALU.subtract)
        tt(etmp[:, :, 4:8], gates[:, :, 8:12], bcs[:, :, 4:8], ALU.subtract)
        act(eib, etmp, AF.Exp)
        dbg_out("gates", T(gates.ap.rearrange("p t c -> p (t c)"), gates.buf), [128, NT * 16], F32)

        cntu = {"u": 0}
        for j in range(4):
            dma_in(t_["bd"], d_bdA[l, j])
            dma_in(t_["bdB"], d_bdB[l, j])
            cp(t_["bdb"], t_["bd"][:, 0:384], eng="pool")
            cp(t_["bdbB"], t_["bdB"][:, 0:192], eng="pool")
            bdb, bdbB = t_["bdb"], t_["bdbB"]
            compute_xm_xc(j)
            for g in range(NG):
                gs = slice(g * 512, (g + 1) * 512)
                for (dst, src, w_, kk, sc) in ((mqA, xcA, bdb[:, 0:128], 128, QSCALE), (mqB, xcB, bdbB[:, 0:64], 64, QSCALE),
                                               (mkA, xcA, bdb[:, 128:256], 128, 1.0), (mkB, xcB, bdbB[:, 64:128], 64, 1.0)):
                    ps = pbank()
                    mm(ps[0:kk, :], w_[0:kk, :], src[0:kk, gs])
                    act(dst[0:kk, gs], ps[0:kk, :], AF.Copy, scale=sc)
            for t in range(NT):
                tsl = slice(t * 128, (t + 1) * 128)
                ps = pbank()
                mm(ps[:, 0:128], xmbA[:, tsl], bdb[:, 256:384])
                mm(ps[:, 128:192], xmbB[0:64, tsl], bdbB[0:64, 128:192])
                mm(ps[:, 192:320], xcA[:, tsl], bdb[:, 128:256])
                mm(ps[:, 320:384], xcB[0:64, tsl], bdbB[0:64, 64:128])
                cp(vtok[:, t, 0:192], ps[:, 0:192], eng="act")
                cp(ktok[:, t, :], ps[:, 192:384], eng="act")
            memset(vtok[:, :, 192:193], 1.0, eng="dve")
            NSX = 4
            if "vp" not in t_:
                t_["vp"] = [sb([128, 200], BF16, "mvp%d" % i) for i in range(NSX)]
                for nm_, shp_, dt_ in (("Eb", [128, 128], F32), ("wT", [128, 128], BF16), ("qsA", [128, 128], BF16),
                                       ("qsB", [64, 128], BF16), ("eibrep", [128, 128], BF16)):
                    t_[nm_] = t_[nm_] + [sb(shp_, dt_, "mx%s%d" % (nm_, i)) for i in range(2, NSX)]
            for dr in range(2):
                order = list(range(NT)) if dr == 0 else list(range(NT - 1, -1, -1))
                gcol = dr * 4 + j
                tri = U_f if dr == 0 else L_f
                mneg = mnegF if dr == 0 else mnegB
                units = []
                for i_, c in enumerate(order):
                    units.append(dict(c=c, first=(i_ == 0), u=cntu["u"]))
                    cntu["u"] += 1

                def st0(un, dr=dr, gcol=gcol, tri=tri, mneg=mneg):
                    c, u, first = un["c"], un["u"], un["first"]
                    csl = slice(c * 128, (c + 1) * 128)
                    sx = u % NSX
                    Eb, wT, qsA, qsB = t_["Eb"][sx], t_["wT"][sx], t_["qsA"][sx], t_["qsB"][sx]
                    vpc, eibrep = t_["vp"][sx], t_["eibrep"][sx]
                    EbM, lfrep = t_["EbM"][u % 2], t_["lfrep"][u % 2]
                    ts(vpc[:, 0:193], vtok[:, c, 0:193], eib[:, c, gcol:gcol + 1], ALU.mult, eng="pool")
                    ts(eibrep, ones_f, eib[:, c, gcol:gcol + 1], ALU.mult, eng="pool")
                    bk1 = pbank()
                    pss = bk1[:, 0:128]
                    psl = bk1[:, 128:256]
                    pslm = bk1[:, 256:384]
                    mm(pss, mkA[:, csl], mqA[:, csl], start=True, stop=False)
                    mm(pss, mkB[0:64, csl], mqB[0:64, csl], start=False, stop=True)
                    ts(lfrep, ones_f, lf[:, c, gcol:gcol + 1], ALU.mult, eng="pool")
                    mm(psl, lfrep, tri)
                    mm(pslm, lfrep, tri, start=True, stop=False)
                    mm(pslm, ident_f, mneg, start=False, stop=True)
                    act(Eb, psl, AF.Exp)
                    act(EbM, pslm, AF.Exp)
                    tt(wT, pss, EbM, ALU.mult)
                    if not first:
                        tt(qsA, mqA[:, csl], Eb, ALU.mult)
                        tt(qsB, mqB[0:64, csl], Eb[0:64, :], ALU.mult)
                    bk3 = xbank()
                    pcA = bk3[:, 0:256]
                    pcB = bk3[:, 256:512]
                    mm(pcA[:, 0:193], ktok[:, c, 0:128], vpc[:, 0:193])
                    mm(pcB[0:64, 0:193], ktok[:, c, 128:192], vpc[:, 0:193])
                    un.update(Eb=Eb, wT=wT, qsA=qsA, qsB=qsB, vpc=vpc, eibrep=eibrep, pcA=pcA, pcB=pcB)

                def st1(un, dr=dr):
                    c, u, first = un["c"], un["u"], un["first"]
                    csl = slice(c * 128, (c + 1) * 128)
                    Eb, wT, qsA, qsB = un["Eb"], un["wT"], un["qsA"], un["qsB"]
                    vpc, eibrep, pcA, pcB = un["vpc"], un["eibrep"], un["pcA"], un["pcB"]
                    dm, rd = t_["dm"][u % 2], t_["rd"][u % 2]
                    bk2 = pbank()
                    pnA = bk2[:, 0:128]
                    pnB = bk2[:, 128:256]
                    pdn = bk2[:, 256:384]
                    mm(pnA, vpc[:, 0:128], wT, start=True, stop=first)
                    if not first:
                        mm(pnA, t_["CbA"][:, 0:128], qsA, start=False, stop=False)
                        mm(pnA, t_["CbB"][:, 0:128], qsB, start=False, stop=True)
                    mm(pnB[0:64, :], vpc[:, 128:192], wT, start=True, stop=first)
                    if not first:
                        mm(pnB[0:64, :], t_["CbA"][:, 128:192], qsA, start=False, stop=False)
                        mm(pnB[0:64, :], t_["CbB"][:, 128:192], qsB, start=False, stop=True)
                    mm(pdn, eibrep, wT, start=True, stop=first)
                    if not first:
                        mm(pdn, t_["nrA"], qsA, start=False, stop=False)
                        mm(pdn, t_["nrB"], qsB, start=False, stop=True)
                    ebl = Eb[:, 127:128] if dr == 0 else Eb[:, 0:1]
                    if first:
                        act(t_["CA"][:, 0:193], pcA[:, 0:193], AF.Copy, scale=ebl)
                        act(t_["CB"][:, 0:193], pcB[0:64, 0:193], AF.Copy, scale=ebl[0:64, :])
                        act(t_["CbA"][:, 0:193], pcA[:, 0:193], AF.Copy, scale=ebl)
                        act(t_["CbB"][:, 0:193], pcB[0:64, 0:193], AF.Copy, scale=ebl[0:64, :])
                    else:
                        tt(t_["CtA"][:, 0:193], pcA[:, 0:193], t_["CA"][:, 0:193], ALU.add)
                        tt(t_["CtB"][:, 0:193], pcB[0:64, 0:193], t_["CB"][:, 0:193], ALU.add)
                        act(t_["CA"][:, 0:193], t_["CtA"][:, 0:193], AF.Copy, scale=ebl)
                        act(t_["CB"][:, 0:193], t_["CtB"][:, 0:193], AF.Copy, scale=ebl[0:64, :])
                        act(t_["CbA"][:, 0:193], t_["CtA"][:, 0:193], AF.Copy, scale=ebl)
                        act(t_["CbB"][:, 0:193], t_["CtB"][:, 0:193], AF.Copy, scale=ebl[0:64, :])
                    ts(t_["nrA"], ones_f, t_["CA"][:, 192:193], ALU.mult, eng="pool")
                    ts(t_["nrB"], ones_f[0:64, :], t_["CB"][:, 192:193], ALU.mult, eng="pool")
                    ts(rd, pdn, -1.0, ALU.mult, 1.0, ALU.max)
                    stt(dm, pdn, 1.0, rd, ALU.max, ALU.max)
                    recip(rd, dm)
                    if dr == 0:
                        tt(hfA[:, csl], pnA, rd, ALU.mult)
                        tt(hfB[0:64, csl], pnB[0:64, :], rd[0:64, :], ALU.mult)
                    else:
                        o4 = (c % 4) * 128
                        tt(t_["hbA"][:, o4:o4 + 128], pnA, rd, ALU.mult)
                        tt(t_["hbB"][0:64, o4:o4 + 128], pnB[0:64, :], rd[0:64, :], ALU.mult)
                    if dr == 1 and c % 4 == 0:
                        g = c // 4
                        gs = slice(g * 512, (g + 1) * 512)
                        hbA, hbB = t_["hbA"], t_["hbB"]
                        tt(hbA, hbA, hfA[:, gs], ALU.add)
                        tt(hbB, hbB, hfB[0:64, gs], ALU.add)
                        so = t_["so"]
                        for (idx, nm, coff, kk, fn) in ((0, "om", 0, 128, AF.Sigmoid), (1, "om", 128, 64, AF.Sigmoid),
                                                        (2, "zm", 0, 128, AF.Silu), (3, "zm", 128, 64, AF.Silu)):
                            wz = load_win(l, OFF[nm] + j * 192 + coff, kk)
                            ps = pbank()
                            proj_fm(ps, wz, 0, kk, g)
                            act(so[idx][0:kk, :], ps[0:kk, :], fn)
                        tt(hbA, hbA, so[0], ALU.mult)
                        tt(hbB, hbB, so[1][0:64, :], ALU.mult)
                        rstd = tf()
                        sumsq_rstd(rstd, [(hbA, 128), (hbB, 64)], 192, 512)
                        tt(hbA, hbA, rstd, ALU.mult)
                        tt(hbB, hbB, rstd[0:64, :], ALU.mult)
                        skA = tf()
                        skB = tf()
                        ts(skA, xcA[:, gs], mvA[:, l * 12 + 4 + j:l * 12 + 5 + j], ALU.mult, eng="pool")
                        ts(skB[0:64, :], xcB[0:64, gs], mvB[:, l * 12 + 4 + j:l * 12 + 5 + j], ALU.mult, eng="pool")
                        stt(hbA, hbA, mgsA[:, l * 4 + j:l * 4 + j + 1], skA, ALU.mult, ALU.add)
                        stt(hbB, hbB, mgsB[:, l * 4 + j:l * 4 + j + 1], skB[0:64, :], ALU.mult, ALU.add)
                        tt(yA[:, gs], hbA, so[2], ALU.mult)
                        tt(yB[0:64, gs], hbB, so[3][0:64, :], ALU.mult)

                run_pipeline(units, [st0, st1], SKEW)
            if j == 0:
                dbg_out("ym", yA, [128, S])
            out_proj(l, [((lambda g: yA[:, g * 512:(g + 1) * 512]), 128, Y_OFF["m"] + j * 192),
                         ((lambda g: yB[0:64, g * 512:(g + 1) * 512]), 64, Y_OFF["m"] + j * 192 + 128)])

    cur = {"b": 0}
    for l in range(NL):
        lb_for_layer(l, l)

    for b in range(NSEQ):
        cur["b"] = b
        xsrc[b] = [d_x[b, t * 128:(t + 1) * 128, :] for t in range(NT)]
        for l in range(NL):
            for t in range(NT):
                g = t // 4
                o = (t % 4) * 128
                rrx["i"] = (rrx["i"] + 1) % 2
                xt = xt_tiles[rrx["i"]]
                P.op("sp", (lambda dst, s_: (lambda e: e.dma_start(out=dst.ap, in_=s_)))(xt, xsrc[b][t]),
                     reads=[xdr[b][t]], writes=[xt], is_dma=True, dkey=xt.buf.id)
                xi = xin[t % 2]
                P.op("act", (lambda o_, i_, a_: (lambda e: e.activation(out=o_.ap, in_=i_.ap, func=AF.Square, accum_out=a_.ap)))(xi, xt, ssq[:, 0:1]),
                     reads=[xt], writes=[xi, ssq])
                rstd_from(ssq[:, 1:2], ssq[:, 0:1], 1024)
                ts(xi, xt, ssq[:, 1:2], ALU.mult)
                for kq in range(2):
                    ps = pbank()
                    for k4 in range(4):
                        k = kq * 4 + k4
                        transp(ps[:, k4 * 128:(k4 + 1) * 128], xi[:, k * 128:(k + 1) * 128], ident_f)
                    for k4 in range(4):
                        k = kq * 4 + k4
                        ts(hTg[k][g][:, o:o + 128], ps[:, k4 * 128:(k4 + 1) * 128], ng32[:, l * 8 + k:l * 8 + k + 1], ALU.mult)
            if "a" in groups:
                ctab = W_f32[0]
                stab = W_f32[1]
                angt = W_f32[2]
                posi = T(angt.ap[:, 0:S].bitcast(I32), angt.buf)
                dma_in(posi, d_pos[b:b + 1, :].to_broadcast([128, S]))
                cp(angt[:, 0:S], posi)
                ts(angt[:, 0:S], angt[:, 0:S], invf, ALU.mult)
                ts(ctab[:, 0:S], angt[:, 0:S], float(1.0 / (2 * math.pi)), ALU.mult)
                ki = T(stab.ap[:, 0:S].bitcast(I32), stab.buf)
                cp(ki, ctab[:, 0:S])
                cp(ctab[:, 0:S], ki)
                stt(angt[:, 0:S], ctab[:, 0:S], float(-2 * math.pi), angt[:, 0:S], ALU.mult, ALU.add)
                act(stab[:, 0:S], angt[:, 0:S], AF.Sin, scale=0.25)
                tt(stab[:, 0:S], stab[:, 0:S], stab[:, 0:S], ALU.mult)
                ts(stab[:, 0:S], stab[:, 0:S], -2.0, ALU.mult, 1.0, ALU.add)
                act(angt[:, 0:S], angt[:, 0:S], AF.Sin, scale=0.5)
                stt(stab[:, 0:S], angt[:, 0:S], 2.0, stab[:, 0:S], ALU.mult, ALU.mult)
                ts(stab[:, 0:S], stab[:, 0:S], sgn, ALU.mult)
                tt(ctab[:, 0:S], angt[:, 0:S], angt[:, 0:S], ALU.mult)
                ts(ctab[:, 0:S], ctab[:, 0:S], -2.0, ALU.mult, 1.0, ALU.add)
                attention_group(l, ctab, stab)
            excl["on"] = True
            if "h" in groups:
                hgrn_group(l)
            if "m" in groups:
                mlstm_group(l)
            excl["on"] = False
        if final_norm:
            dma_in(fgb, d_fgrow[0:1, :].to_broadcast([128, 1024]))
            ts(fgb, fgb, 32.0, ALU.mult)
        for t in range(NT):
            rrx["i"] = (rrx["i"] + 1) % 2
            xt = xt_tiles[rrx["i"]]
            P.op("sp", (lambda dst, s_: (lambda e: e.dma_start(out=dst.ap, in_=s_)))(xt, xsrc[b][t]),
                 reads=[xdr[b][t]], writes=[xt], is_dma=True, dkey=xt.buf.id)
            if final_norm:
                xi = xin[t % 2]
                P.op("act", (lambda o_, i_, a_: (lambda e: e.activation(out=o_.ap, in_=i_.ap, func=AF.Square, accum_out=a_.ap)))(xi, xt, ssq[:, 2:3]),
                     reads=[xt], writes=[xi, ssq])
                rstd_from(ssq[:, 3:4], ssq[:, 2:3], 1024)
                stt(xt, xt, ssq[:, 3:4], fgb, ALU.mult, ALU.mult)
            tok = T(None)
            P.op("sp", (lambda src_, d_: (lambda e: e.dma_start(out=d_, in_=src_.ap)))(xt, d_out[b, t * 128:(t + 1) * 128, :]),
                 reads=[xt], writes=[xdr[b][t], tok], is_dma=True, dkey="o%d" % xt.buf.id)
            fin.append(tok)
    P.op("sp", None, reads=fin)
    P.emit(nc, ctx)
    P.sbuf_left = nc.sbuf_bytes_remaining
    ctx.close()
    return nc, P, dbg_outs


def prep_params(inp, layers, depth_all):
    NL = len(layers)
    f = np.float32
    out = {}
    out["w_in"] = np.ascontiguousarray(inp["w_in"][layers], dtype=f)
    out["w_out"] = np.ascontiguousarray(inp["w_out"][layers], dtype=f)
    out["consts"] = make_consts()
    ng = inp["norm_g"][layers].reshape(NL, 8, 128)
    out["ng"] = np.ascontiguousarray(ng.transpose(2, 0, 1).reshape(128, NL * 8), dtype=f)
    out["fgrow"] = np.ascontiguousarray(inp["final_g"].reshape(1, 1024), dtype=f)
    out["alam"] = np.ascontiguousarray(inp["a_lambda"][layers].reshape(1, NL * 256), dtype=f)
    out["ang"] = np.ascontiguousarray(inp["a_norm_g"][layers].reshape(NL, 4, 128).transpose(2, 0, 1).reshape(128, NL * 4), dtype=f)
    lb = inp["h_lb_logits"].reshape(depth_all, 2, 6, 128)
    out["lb"] = np.ascontiguousarray(lb.transpose(3, 1, 2, 0).reshape(128, 12 * depth_all), dtype=f)
    out["hng"] = np.ascontiguousarray(inp["h_norm_g"][layers].reshape(NL, 6, 128).transpose(2, 0, 1).reshape(128, NL * 6), dtype=f)
    cw = inp["m_conv_w"][layers].reshape(NL, 5, 4, 192)
    out["cwA"] = np.ascontiguousarray(cw[:, :, :, 0:128].transpose(3, 0, 2, 1).reshape(128, NL * 20), dtype=f)
    out["cwB"] = np.ascontiguousarray(cw[:, :, :, 128:192].transpose(3, 0, 2, 1).reshape(64, NL * 20), dtype=f)
    mv = np.stack([inp["m_conv_b"][layers], inp["m_skip"][layers], inp["m_norm_g"][layers]], axis=1)
    mv = mv.reshape(NL, 3, 4, 192)
    out["mvA"] = np.ascontiguousarray(mv[:, :, :, 0:128].transpose(3, 0, 1, 2).reshape(128, NL * 12), dtype=f)
    out["mvB"] = np.ascontiguousarray(mv[:, :, :, 128:192].transpose(3, 0, 1, 2).reshape(64, NL * 12), dtype=f)
    bdA = np.zeros((NL, 4, 128, 6, 128), f)
    bdB = np.zeros((NL, 4, 64, 6, 64), f)
    for wi, nm in enumerate(("m_wq", "m_wk", "m_wv")):
        w = inp[nm][layers].reshape(NL, 4, 48, 4, 4)
        for g_ in range(32):
            bdA[:, :, 4 * g_:4 * g_ + 4, wi, 4 * g_:4 * g_ + 4] = w[:, :, g_]
            bdA[:, :, 4 * g_:4 * g_ + 4, 3 + wi, 4 * g_:4 * g_ + 4] = w[:, :, g_].transpose(0, 1, 3, 2)
        for g_ in range(16):
            bdB[:, :, 4 * g_:4 * g_ + 4, wi, 4 * g_:4 * g_ + 4] = w[:, :, 32 + g_]
            bdB[:, :, 4 * g_:4 * g_ + 4, 3 + wi, 4 * g_:4 * g_ + 4] = w[:, :, 32 + g_].transpose(0, 1, 3, 2)
    out["bdA"] = bdA.reshape(NL, 4, 128, 6 * 128)
    out["bdB"] = bdB.reshape(NL, 4, 64, 6 * 64)
    wg = inp["m_w_gates"][layers].reshape(NL, 3, 4, 192, 16)
    out["wgA"] = np.ascontiguousarray(wg[:, :, :, 0:128].transpose(0, 3, 2, 1, 4).reshape(NL, 128, 4 * 3 * 16), dtype=f)
    out["wgB"] = np.ascontiguousarray(wg[:, :, :, 128:192].transpose(0, 3, 2, 1, 4).reshape(NL, 64, 4 * 3 * 16), dtype=f)
    out["bg"] = np.ascontiguousarray(inp["m_b_gates"][layers].reshape(1, NL * 16), dtype=f)
    return out


_CACHE = {}


def _get_prog(S, NSEQ, NL, depth_all, lam_inits, final_norm):
    key = (S, NSEQ, NL, depth_all, tuple(lam_inits), final_norm)
    if key not in _CACHE:
        _CACHE[key] = build(S, NSEQ, NL, depth_all, lam_inits, final_norm=final_norm)[0]
    return _CACHE[key]


def kernel(**inputs):
    inp = {k: np.asarray(v) for k, v in inputs.items()}
    x = np.ascontiguousarray(inp["x"], dtype=np.float32)
    pos = np.ascontiguousarray(inp["positions"], dtype=np.int32)
    B, S, D = x.shape
    DEPTH = inp["w_in"].shape[0]
    NCORE = 8
    per = B // NCORE
    lam_all = [0.8 - 0.6 * math.exp(-0.3 * l) for l in range(DEPTH)]
    params = prep_params(inp, list(range(DEPTH)), DEPTH)
    nc = _get_prog(S, per, DEPTH, DEPTH, lam_all, True)
    in_maps = []
    for c in range(NCORE):
        m = dict(params)
        m["x"] = x[c * per:(c + 1) * per]
        m["pos"] = pos[c * per:(c + 1) * per]
        in_maps.append(m)
    res = run_bass_kernel_spmd(nc, in_maps, core_ids=list(range(NCORE)))
    out = np.concatenate([np.asarray(r["out"]) for r in res.results], axis=0)
    return out.astype(np.float32)
```

```python
import math
from contextlib import ExitStack
import numpy as np
import concourse.bass as bass
import concourse.mybir as mybir
from concourse.bass_utils import run_bass_kernel_spmd

F32 = mybir.dt.float32
BF16 = mybir.dt.bfloat16
I32 = mybir.dt.int32
ALU = mybir.AluOpType
AF = mybir.ActivationFunctionType
AX = mybir.AxisListType

ENGS = ("pe", "act", "dve", "pool", "sp")
SEM_WRAP = 30000
EPS = 1e-6


class Buf:
    __slots__ = ("last_w", "readers", "id")
    _n = 0

    def __init__(self):
        self.last_w = None
        self.readers = []
        Buf._n += 1
        self.id = Buf._n


class T:
    __slots__ = ("ap", "buf")

    def __init__(self, ap, buf=None):
        self.ap = ap
        self.buf = buf if buf is not None else Buf()

    def __getitem__(self, idx):
        return T(self.ap[idx], self.buf)

    def sub(self, idx):
        return T(self.ap[idx], Buf())


class Op:
    __slots__ = ("eng", "fn", "deps", "idx", "is_dma", "sig", "dkey")


class Prog:
    def __init__(self):
        self.ops = []

    def op(self, eng, fn, reads=(), writes=(), is_dma=False, dkey=None):
        o = Op()
        o.eng = eng
        o.fn = fn
        o.is_dma = is_dma
        o.dkey = dkey
        o.sig = None
        o.idx = len(self.ops)
        deps = set()
        for t in reads:
            b = t.buf
            if b.last_w is not None:
                deps.add(b.last_w)
        for t in writes:
            b = t.buf
            if b.last_w is not None:
                deps.add(b.last_w)
            deps.update(b.readers)
        for t in reads:
            t.buf.readers.append(o.idx)
        for t in writes:
            t.buf.last_w = o.idx
            t.buf.readers = []
        deps.discard(o.idx)
        o.deps = deps
        self.ops.append(o)
        return o

    def emit(self, nc, ctx):
        ops = self.ops
        needed = set()
        for o in ops:
            best = {}
            for d in o.deps:
                p = ops[d]
                if p.eng == "pe" and o.eng == "pe" and not p.is_dma and not o.is_dma:
                    continue
                key = ("dma", p.dkey) if p.is_dma else ("eng", p.eng)
                if key not in best or best[key] < d:
                    best[key] = d
            o.deps = sorted(best.values())
            needed.update(o.deps)
        counters = {}
        sems = {}

        def getsem(name):
            if name not in sems:
                sems[name] = ctx.enter_context(nc.semaphore(name))
            return sems[name]

        for o in ops:
            if o.idx not in needed and not (o.is_dma and o.fn is not None):
                continue
            if o.is_dma:
                base = "d%s" % (o.dkey,)
                inc = 16
            else:
                base = "e" + o.eng
                inc = 1
            cnt, epoch = counters.get(base, (0, 0))
            if cnt + inc > SEM_WRAP:
                epoch += 1
                cnt = 0
            cnt += inc
            counters[base] = (cnt, epoch)
            o.sig = ("%s_%d" % (base, epoch), cnt, inc)
        for o in ops:
            if o.sig:
                getsem(o.sig[0])
        self.n_sems = len(sems)
        per_eng = {e: [] for e in ENGS}
        for o in ops:
            per_eng[o.eng].append(o)
        block = ctx.enter_context(nc.Block())

        def run(eng_obj, lst):
            last_wait = {}
            for o in lst:
                for d in o.deps:
                    sname, val, _ = ops[d].sig
                    if last_wait.get(sname, 0) >= val:
                        continue
                    last_wait[sname] = val
                    eng_obj.wait_ge(sems[sname], val)
                if o.fn is None:
                    continue
                ins = o.fn(eng_obj)
                if o.sig is not None:
                    ins.then_inc(sems[o.sig[0]], o.sig[2])

        @block.tensor
        def _(e):
            run(e, per_eng["pe"])

        @block.scalar
        def _(e):
            run(e, per_eng["act"])

        @block.vector
        def _(e):
            run(e, per_eng["dve"])

        @block.gpsimd
        def _(e):
            run(e, per_eng["pool"])

        @block.sync
        def _(e):
            run(e, per_eng["sp"])


D_MODEL = 1024
D_MIX = 2048
M_W = 768
H_W = 768
A_W = 512
IN_COLS = 8192
OFF = dict(xm=0, om=768, zm=1536, hq=2304, hff=3072, hfb=3840, hi=4608, hz=5376,
           aq=6144, ak=6656, av=7168, az=7680)
Y_OFF = dict(m=0, h=768, a=1536)
ROPE_THETA = 500000.0
NEG = -30000.0

C_IDENT = 0
C_U = 128
C_L = 256
C_PSW = 384
C_ONES = 512
C_VEC = 640
NCONST = 648


def make_consts():
    c = np.zeros((128, NCONST), np.float32)
    r = np.arange(128)
    c[:, C_IDENT:C_IDENT + 128] = np.eye(128, dtype=np.float32)
    c[:, C_U:C_U + 128] = (r[:, None] <= r[None, :]).astype(np.float32)
    c[:, C_L:C_L + 128] = (r[:, None] >= r[None, :]).astype(np.float32)
    psw = np.zeros((128, 128), np.float32)
    for m in range(128):
        d = m % 64
        if d < 8:
            psw[m + 8, m] = 1.0
        elif d < 16:
            psw[m - 8, m] = 1.0
    c[:, C_PSW:C_PSW + 128] = psw
    c[:, C_ONES:C_ONES + 128] = 1.0
    half = 8
    inv = ROPE_THETA ** (-np.arange(half, dtype=np.float32) / half)
    for p in range(128):
        d = p % 64
        if d < 16:
            c[p, C_VEC + 0] = inv[d % 8]
            c[p, C_VEC + 1] = -1.0 if d < 8 else 1.0
        c[p, C_VEC + 2] = 1.0 if p < 64 else 0.0
        c[p, C_VEC + 3] = 0.0 if p < 64 else 1.0
    c[:, C_VEC + 4] = 1024 * EPS
    c[:, C_VEC + 5] = 128 * EPS
    c[:, C_VEC + 6] = 192 * EPS
    c[:, C_VEC + 7] = 1.0
    return c


def build(S, NSEQ, NL, DEPTH_ALL, lam_inits, final_norm=True, groups=("a", "h", "m"), dbg=()):
    NT = S // 128
    NG = S // 512
    nc = bass.Bass("TRN2", target_bir_lowering=False)
    P = Prog()
    ctx = ExitStack()

    def dram(name, shape, dt=F32, kind="ExternalInput"):
        return nc.dram_tensor(name, shape, dt, kind=kind).ap()

    d_x = dram("x", [NSEQ, S, D_MODEL])
    d_pos = dram("pos", [NSEQ, S], I32)
    d_win = dram("w_in", [NL, D_MODEL, IN_COLS])
    d_wout = dram("w_out", [NL, D_MIX, D_MODEL])
    d_consts = dram("consts", [128, NCONST])
    d_ng = dram("ng", [128, NL * 8])
    d_fgrow = dram("fgrow", [1, 1024])
    d_alam = dram("alam", [1, NL * 256])
    d_ang = dram("ang", [128, NL * 4])
    d_lb = dram("lb", [128, 2 * 6 * DEPTH_ALL])
    d_hng = dram("hng", [128, NL * 6])
    d_cwA = dram("cwA", [128, NL * 4 * 5])
    d_cwB = dram("cwB", [64, NL * 4 * 5])
    d_mvA = dram("mvA", [128, NL * 4 * 3])
    d_mvB = dram("mvB", [64, NL * 4 * 3])
    d_bdA = dram("bdA", [NL, 4, 128, 6 * 128])
    d_bdB = dram("bdB", [NL, 4, 64, 6 * 64])
    d_wgA = dram("wgA", [NL, 128, 4 * 3 * 16])
    d_wgB = dram("wgB", [NL, 64, 4 * 3 * 16])
    d_bg = dram("bg", [1, NL * 16])
    d_out = dram("out", [NSEQ, S, D_MODEL], kind="ExternalOutput")
    dbg_outs = {}

    def sb(shape, dt=F32, name=None):
        return T(ctx.enter_context(nc.sbuf_tensor("s_" + name, shape, dt))[:])

    def dma_in(dst, src_ap, eng="sp"):
        P.op(eng, lambda e: e.dma_start(out=dst.ap, in_=src_ap), writes=[dst], is_dma=True, dkey=dst.buf.id)

    def dma_out(dst_ap, src, eng="sp"):
        tok = T(None)
        P.op(eng, lambda e: e.dma_start(out=dst_ap, in_=src.ap), reads=[src], writes=[tok], is_dma=True,
             dkey="o%d" % src.buf.id)
        return tok

    def mm(out, lhsT, rhs, start=True, stop=True, extra_reads=()):
        P.op("pe", lambda e: e.matmul(out.ap, lhsT=lhsT.ap, rhs=rhs.ap, start=start, stop=stop),
             reads=[lhsT, rhs] + list(extra_reads), writes=[out])

    def transp(out, in_, ident):
        P.op("pe", lambda e: e.transpose(out=out.ap, in_=in_.ap, identity=ident.ap), reads=[in_, ident], writes=[out])

    def act(out, in_, func, bias=None, scale=None, eng="act", extra_reads=()):
        kw = {}
        rd = [in_] + list(extra_reads)
        if bias is not None:
            if isinstance(bias, T):
                kw["bias"] = bias.ap
                rd.append(bias)
            else:
                kw["bias"] = bias
        if scale is not None:
            if isinstance(scale, T):
                kw["scale"] = scale.ap
                rd.append(scale)
            else:
                kw["scale"] = scale
        P.op(eng, lambda e: e.activation(out=out.ap, in_=in_.ap, func=func, **kw), reads=rd, writes=[out])

    def tt(out, a, b, op, eng="dve"):
        P.op(eng, lambda e: e.tensor_tensor(out=out.ap, in0=a.ap, in1=b.ap, op=op), reads=[a, b], writes=[out])

    def ts(out, a, s1, op0, s2=None, op1=None, eng="dve"):
        rd = [a]
        v1 = s1
        v2 = s2
        if isinstance(s1, T):
            rd.append(s1)
            v1 = s1.ap
        if isinstance(s2, T):
            rd.append(s2)
            v2 = s2.ap
        if op1 is None:
            P.op(eng, lambda e: e.tensor_scalar(out=out.ap, in0=a.ap, scalar1=v1, scalar2=None, op0=op0),
                 reads=rd, writes=[out])
        else:
            P.op(eng, lambda e: e.tensor_scalar(out=out.ap, in0=a.ap, scalar1=v1, scalar2=v2, op0=op0, op1=op1),
                 reads=rd, writes=[out])

    def stt(out, a, s, b, op0, op1, eng="dve"):
        rd = [a, b]
        v = s
        if isinstance(s, T):
            rd.append(s)
            v = s.ap
        P.op(eng, lambda e: e.scalar_tensor_tensor(out=out.ap, in0=a.ap, scalar=v, in1=b.ap, op0=op0, op1=op1),
             reads=rd, writes=[out])

    def cp(out, in_, eng="dve"):
        if eng == "act":
            P.op("act", lambda e: e.copy(out=out.ap, in_=in_.ap), reads=[in_], writes=[out])
        else:
            P.op(eng, lambda e: e.tensor_copy(out=out.ap, in_=in_.ap), reads=[in_], writes=[out])

    def memset(out, val, eng="pool"):
        P.op(eng, lambda e: e.memset(out.ap, val), writes=[out])

    def recip(out, in_):
        P.op("dve", lambda e: e.reciprocal(out=out.ap, in_=in_.ap), reads=[in_], writes=[out])

    def scan_cumsum(out, ones, in_):
        P.op("dve", lambda e: e.tensor_tensor_scan(out=out.ap, data0=ones.ap, data1=in_.ap, initial=0.0,
                                                   op0=ALU.mult, op1=ALU.add), reads=[ones, in_], writes=[out])

    def dbg_out(name, src, shape, dt=BF16):
        if name not in dbg:
            return
        d = dram("dbg_" + name, shape, dt, kind="ExternalOutput")
        dbg_outs[name] = d
        fin.append(dma_out(d, src))

    fin = []

    banks = [T(ctx.enter_context(nc.psum_tensor("pb%d" % i, [128, 512], F32))[:]) for i in range(8)]
    rr = {"b": 0, "x": 0}
    XB = (5, 6, 7)
    excl = {"on": False}
    SKEW = 2

    def pbank():
        while True:
            rr["b"] = (rr["b"] + 1) % 8
            if excl["on"] and rr["b"] in XB:
                continue
            return banks[rr["b"]]

    def xbank():
        rr["x"] = (rr["x"] + 1) % len(XB)
        return banks[XB[rr["x"]]]

    def pquart():
        return pbank()[:, 0:128]

    def run_pipeline(units, stages, skew):
        n = len(units)
        K = len(stages)
        for step in range(n + (K - 1) * skew):
            for s in reversed(range(K)):
                ui = step - s * skew
                if 0 <= ui < n:
                    stages[s](units[ui])

    consts_f = sb([128, NCONST], F32, "consts_f")
    consts_b = sb([128, 640], BF16, "consts_b")
    dma_in(consts_f, d_consts)
    cp(consts_b, consts_f[:, 0:640])
    ident_f = consts_f[:, C_IDENT:C_IDENT + 128]
    U_f = consts_f[:, C_U:C_U + 128]
    L_f = consts_f[:, C_L:C_L + 128]
    psw_b = consts_b[:, C_PSW:C_PSW + 128]
    ones_b = consts_b[:, C_ONES:C_ONES + 128]
    ones_f = consts_f[:, C_ONES:C_ONES + 128]
    U_b = consts_b[:, C_U:C_U + 128]
    L_b = consts_b[:, C_L:C_L + 128]
    invf = consts_f[:, C_VEC + 0:C_VEC + 1]
    sgn = consts_f[:, C_VEC + 1:C_VEC + 2]
    m1 = consts_f[:, C_VEC + 2:C_VEC + 3]
    m2 = consts_f[:, C_VEC + 3:C_VEC + 4]
    epsc = {1024: consts_f[:, C_VEC + 4:C_VEC + 5], 128: consts_f[:, C_VEC + 5:C_VEC + 6], 192: consts_f[:, C_VEC + 6:C_VEC + 7]}
    one_c = consts_f[:, C_VEC + 7:C_VEC + 8]

    def rstd_from(out, v, n):
        act(out, v, AF.Ln, bias=epsc[n][0:out.ap.shape[0], :])
        act(out, out, AF.Exp, scale=-0.5)
    mnegF = sb([128, 128], F32, "mnegF")
    mnegB = sb([128, 128], F32, "mnegB")
    ts(mnegF, U_f, 1.0, ALU.subtract, -NEG, ALU.mult)
    ts(mnegB, L_f, 1.0, ALU.subtract, -NEG, ALU.mult)

    ng = sb([128, NL * 8], F32, "ng")
    dma_in(ng, d_ng)
    ng32 = sb([128, NL * 8], F32, "ng32")
    ts(ng32, ng, 32.0, ALU.mult)
    ang = sb([128, NL * 4], F32, "angs")
    dma_in(ang, d_ang)
    lbl = sb([128, 2 * 6 * DEPTH_ALL], F32, "lbl")
    dma_in(lbl, d_lb)
    hng = sb([128, NL * 6], F32, "hngs")
    dma_in(hng, d_hng)
    cwA = sb([128, NL * 20], F32, "cwA")
    dma_in(cwA, d_cwA)
    cwB = sb([64, NL * 20], F32, "cwB")
    dma_in(cwB, d_cwB)
    mvA = sb([128, NL * 12], F32, "mvA")
    dma_in(mvA, d_mvA)
    mvB = sb([64, NL * 12], F32, "mvB")
    dma_in(mvB, d_mvB)
    bgs = sb([128, NL * 16], F32, "bgs")
    dma_in(bgs, d_bg[0:1, :].to_broadcast([128, NL * 16]))

    neglam = sb([128, NL], F32, "neglam")
    gsa = sb([128, NL * 4], F32, "gsa")
    lamtmp = sb([128, 64], F32, "lamtmp")
    lam2 = sb([128, 4], F32, "lam2")
    alam_t = sb([128, 256], F32, "alam")
    for l in range(NL):
        dma_in(alam_t, d_alam[0:1, l * 256:(l + 1) * 256].to_broadcast([128, 256]))
        for j in range(2):
            tt(lamtmp, alam_t[:, j * 128:j * 128 + 64],
               alam_t[:, j * 128 + 64:j * 128 + 128], ALU.mult)
            P.op("dve", (lambda o, i: (lambda e: e.reduce_sum(out=o.ap, in_=i.ap, axis=AX.X)))(lam2[:, j:j + 1], lamtmp),
                 reads=[lamtmp], writes=[lam2])
        act(lam2[:, 2:4], lam2[:, 0:2], AF.Exp)
        tt(lam2[:, 0:1], lam2[:, 3:4], lam2[:, 2:3], ALU.subtract)
        ts(neglam[:, l:l + 1], lam2[:, 0:1], -float(lam_inits[l]), ALU.add)
        ts(gsa[:, l * 4:(l + 1) * 4], ang[:, l * 4:(l + 1) * 4], float((1.0 - lam_inits[l]) * math.sqrt(128.0)), ALU.mult)
    NLB = 12 * DEPTH_ALL
    lbe = sb([128, NLB], F32, "lbe")
    act(lbe, lbl, AF.Exp)
    lbs = sb([128, 12], F32, "lbs")
    P.op("dve", lambda e: e.reduce_sum(out=lbs.ap, in_=lbe.ap.rearrange("p (a l) -> p a l", l=DEPTH_ALL), axis=AX.X),
         reads=[lbe], writes=[lbs])
    lbr = sb([128, 12], F32, "lbr")
    recip(lbr, lbs)
    lbv = sb([128, NL * 12], F32, "lbv")
    omlb = sb([128, NL * 12], F32, "omlb")
    lbt = sb([128, 12], F32, "lbt")

    def lb_for_layer(l, lglob):
        dst = lbv[:, l * 12:(l + 1) * 12]
        if lglob == 0:
            memset(dst, 0.0, eng="dve")
        else:
            P.op("dve", lambda e: e.reduce_sum(
                out=lbt.ap, in_=lbe.ap.rearrange("p (a l) -> p a l", l=DEPTH_ALL)[:, :, 1:lglob + 1], axis=AX.X),
                reads=[lbe], writes=[lbt])
            tt(dst, lbt, lbr, ALU.mult)
        ts(omlb[:, l * 12:(l + 1) * 12], dst, -1.0, ALU.mult, 1.0, ALU.add)

    hgs = sb([128, NL * 6], F32, "hgs")
    ts(hgs, hng, float(math.sqrt(128.0)), ALU.mult)
    mgsA = sb([128, NL * 4], F32, "mgsA")
    mgsB = sb([64, NL * 4], F32, "mgsB")

    xdr = [[T(None) for t in range(NT)] for b in range(NSEQ)]
    xsrc = {}
    hT = [sb([128, S], BF16, "hT%d" % k) for k in range(8)]
    hTg = [[hT[k].sub((slice(None), slice(g * 512, (g + 1) * 512))) for g in range(NG)] for k in range(8)]
    W_bf = [sb([128, S], BF16, "wbf%d" % i) for i in range(10)]
    W_f32 = [sb([128, max(S + 4, 1024)], F32, "wf%d" % i) for i in range(3)]
    xin = [W_f32[0][:, 0:1024], W_f32[1][:, 0:1024]]
    fgb = W_f32[2][:, 0:1024]
    xt_tiles = [sb([128, 1024], F32, "xtile%d" % i) for i in range(2)]
    rrx = {"i": 0}
    ssq = sb([128, 4], F32, "ssq")
    tmpf = [sb([128, 512], F32, "tmpf%d" % i) for i in range(4)]
    tmpb = [sb([128, 512], BF16, "tmpb%d" % i) for i in range(5)]
    rrt = {"f": 0, "b": 0}

    def tf():
        rrt["f"] = (rrt["f"] + 1) % len(tmpf)
        return tmpf[rrt["f"]]

    def tb():
        rrt["b"] = (rrt["b"] + 1) % len(tmpb)
        return tmpb[rrt["b"]]

    NWB = 3
    wbf = [sb([128, 8, 128], BF16, "wbf16_%d" % i) for i in range(NWB)]
    rrw = {"s": 0, "b": 0, "os": 0, "ob": 0}
    wo_bf = [sb([128, 1024], BF16, "wobf%d" % i) for i in range(4)]

    def load_win(l, col0, ncols):
        rrw["b"] = (rrw["b"] + 1) % NWB
        wb = wbf[rrw["b"]]
        src_ = d_win[l, :, col0:col0 + ncols].rearrange("(k p) c -> p k c", p=128)
        dma_in(wb[:, :, 0:ncols], src_, eng="pool")
        return wb

    def load_wout(l, row0, nrows):
        rrw["ob"] = (rrw["ob"] + 1) % 4
        wb = wo_bf[rrw["ob"]]
        dma_in(wb[0:nrows, :], d_wout[l, row0:row0 + nrows, :], eng="pool")
        return wb

    def proj_fm(ps, wb, c0, ncols, g):
        for k in range(8):
            mm(ps[0:ncols, :], wb[:, k, c0:c0 + ncols], hTg[k][g], start=(k == 0), stop=(k == 7))

    def proj_tm(ps_view, wb, c0, ncols, tt_):
        g = tt_ // 4
        o = (tt_ % 4) * 128
        for k in range(8):
            mm(ps_view, hTg[k][g][:, o:o + 128], wb[:, k, c0:c0 + ncols], start=(k == 0), stop=(k == 7))

    def sumsq_rstd(rstd_out, srcs, n, g_cols):
        ps = pbank()
        for i, (s, kk) in enumerate(srcs):
            sq = tb()
            act(sq[0:kk, 0:g_cols], s, AF.Square)
            mm(ps[:, 0:g_cols], ones_b[0:kk, :], sq[0:kk, 0:g_cols], start=(i == 0), stop=(i == len(srcs) - 1))
        rstd_from(rstd_out, ps[:, 0:g_cols], n)

    def out_proj(l, ysrcs):
        b = cur["b"]
        wts = [load_wout(l, r0, kk) for (_, kk, r0) in ysrcs]
        for t in range(NT):
            g = t // 4
            o = (t % 4) * 128
            rrx["i"] = (rrx["i"] + 1) % 2
            xt = xt_tiles[rrx["i"]]
            P.op("sp", (lambda dst, s_: (lambda e: e.dma_start(out=dst.ap, in_=s_)))(xt, xsrc[b][t]),
                 reads=[xdr[b][t]], writes=[xt], is_dma=True, dkey=xt.buf.id)
            for hf in range(2):
                ps = pbank()
                for i, (yf, kk, r0) in enumerate(ysrcs):
                    mm(ps, yf(g)[:, o:o + 128], wts[i][0:kk, hf * 512:(hf + 1) * 512], start=(i == 0), stop=(i == len(ysrcs) - 1))
                tt(xt[:, hf * 512:(hf + 1) * 512], xt[:, hf * 512:(hf + 1) * 512], ps, ALU.add)
            P.op("sp", (lambda src_, d_: (lambda e: e.dma_start(out=d_, in_=src_.ap)))(xt, d_out[b, t * 128:(t + 1) * 128, :]),
                 reads=[xt], writes=[xdr[b][t]], is_dma=True, dkey="o%d" % xt.buf.id)
            xsrc[b][t] = d_out[b, t * 128:(t + 1) * 128, :]

    def attention_group(l, ctab, stab):
        qt = W_bf[0]
        k1 = W_bf[1]
        k2 = W_bf[2]
        vtok = W_bf[3]
        sz = W_bf[4]
        ys = [W_bf[5], W_bf[6], W_bf[7], W_bf[8]]
        vt3 = T(vtok.ap.rearrange("p (t c) -> p t c", c=128), vtok.buf)
        for hd in range(4):
            wq = load_win(l, OFF["aq"] + hd * 128, 128)
            for g in range(NG):
                gs = slice(g * 512, (g + 1) * 512)
                ps = pbank()
                proj_fm(ps, wq, 0, 128, g)
                a_bf = tb()
                cp(a_bf, ps, eng="act")
                ps2 = pbank()
                mm(ps2, psw_b, a_bf)
                t1 = tf()
                tt(t1, ps2, stab[:, gs], ALU.mult)
                t2 = tf()
                tt(t2, ps, ctab[:, gs], ALU.mult)
                tt(qt[:, gs], t1, t2, ALU.add)
            wk = load_win(l, OFF["ak"] + hd * 128, 128)
            for g in range(NG):
                gs = slice(g * 512, (g + 1) * 512)
                ps = pbank()
                proj_fm(ps, wk, 0, 128, g)
                a_bf = tb()
                cp(a_bf, ps, eng="act")
                ps2 = pbank()
                mm(ps2, psw_b, a_bf)
                t1 = tf()
                tt(t1, ps2, stab[:, gs], ALU.mult)
                t2 = tf()
                tt(t2, ps, ctab[:, gs], ALU.mult)
                ktmp = tb()
                tt(ktmp, t1, t2, ALU.add)
                ts(k1[:, gs], ktmp, m1, ALU.mult)
                ts(k2[:, gs], ktmp, m2, ALU.mult)
            wv = load_win(l, OFF["av"] + hd * 128, 128)
            for g in range(NG):
                ps = pbank()
                for j in range(4):
                    proj_tm(ps[:, j * 128:(j + 1) * 128], wv, 0, 128, g * 4 + j)
                cp(T(vtok.ap[:, g * 512:(g + 1) * 512], vtok.buf), ps, eng="act")
            wz = load_win(l, OFF["az"] + hd * 128, 128)
            for g in range(NG):
                ps = pbank()
                proj_fm(ps, wz, 0, 128, g)
                act(sz[:, g * 512:(g + 1) * 512], ps, AF.Silu)
            kk = [k1, k2]
            for g in range(NG):
                gs = slice(g * 512, (g + 1) * 512)
                num = [banks[0], banks[1]]
                den = [banks[2], banks[3]]
                sc_banks = [banks[4], banks[5], banks[6]]
                iters = [(kt, c) for kt in range(NT) for c in range(2)]

                def score(i, gs=gs):
                    kt, c = iters[i]
                    mm(sc_banks[i % 3], kk[c][:, kt * 128:(kt + 1) * 128], qt[:, gs])

                score(0)
                for i in range(len(iters)):
                    if i + 1 < len(iters):
                        score(i + 1)
                    kt, c = iters[i]
                    pt = tb()
                    act(pt, sc_banks[i % 3], AF.Exp, scale=0.125)
                    mm(num[c], vt3[:, kt, :], pt, start=(kt == 0), stop=(kt == NT - 1))
                    mm(den[c], ones_b, pt, start=(kt == 0), stop=(kt == NT - 1))
                r1 = tf()
                recip(r1, den[0])
                r2 = tf()
                recip(r2, den[1])
                o1 = tf()
                tt(o1, num[0], r1, ALU.mult)
                o2 = tf()
                tt(o2, num[1], r2, ALU.mult)
                o = tf()
                stt(o, o2, neglam[:, l:l + 1], o1, ALU.mult, ALU.add)
                rstd = tf()
                sumsq_rstd(rstd, [(o, 128)], 128, 512)
                y1 = o1
                tt(y1, o, rstd, ALU.mult)
                stt(ys[hd][:, gs], y1, gsa[:, l * 4 + hd:l * 4 + hd + 1], sz[:, gs], ALU.mult, ALU.mult)
        dbg_out("ya", ys[0], [128, S])
        out_proj(l, [((lambda g, hd=hd: ys[hd][:, g * 512:(g + 1) * 512]), 128, Y_OFF["a"] + hd * 128) for hd in range(4)])

    def hgrn_group(l):
        qT_ = W_bf[0]
        kT_ = W_bf[1]
        vtok = W_bf[2]
        vt3 = T(vtok.ap.rearrange("p (t c) -> p t c", c=128), vtok.buf)
        sz = W_bf[3]
        ys = [W_bf[4], W_bf[5], W_bf[6]]
        a_pad = W_f32[0]
        na_pad = W_f32[1]
        gtmp = W_f32[1]
        oT = W_f32[2]
        NS0 = 2
        NSXH = 4
        if not hasattr(hgrn_group, "_t"):
            tl_ = dict()
            tl_["ek"] = [[sb([128, 128], BF16, "hek%d_%d" % (s_, i)) for i in range(4)] for s_ in range(NS0)]
            for s_ in range(NS0):
                for i in range(4):
                    memset(tl_["ek"][s_][i], 0.0)
            tl_["eq"] = [sb([128, 128], F32, "heq%d" % i) for i in range(NS0)]
            tl_["ekf"] = [sb([128, 448], F32, "hekf%d" % i) for i in range(NS0)]
            tl_["ekr"] = [{32: t_e.sub((slice(None), slice(0, 32))), 64: t_e.sub((slice(None), slice(32, 96))),
                           96: t_e.sub((slice(None), slice(96, 192))), 128: t_e.sub((slice(None), slice(192, 320))),
                           "kend": t_e.sub((slice(None), slice(320, 448)))} for t_e in tl_["ekf"]]
            tl_["eq2"] = [sb([128, 128], F32, "heq2_%d" % i) for i in range(NS0)]
            tl_["qtl"] = [sb([128, 128], BF16, "hqtl%d" % i) for i in range(NS0)]
            tl_["kend"] = [sb([128, 128], F32, "hkend%d" % i) for i in range(NS0)]
            tl_["kendT"] = [sb([128, 128], BF16, "hkendT%d" % i) for i in range(NS0)]
            tl_["qc"] = [sb([128, 128], BF16, "hqc%d" % i) for i in range(NSXH)]
            tl_["wT"] = [sb([128, 128], BF16, "hwT%d" % i) for i in range(NSXH)]
            tl_["dec"] = sb([128, NSXH], F32, "hdec")
            tl_["Sst"] = sb([128, 128], F32, "hSst")
            tl_["Sbf"] = sb([128, 128], BF16, "hSbf")
            hgrn_group._t = tl_
        tl = hgrn_group._t
        cnt = {"u": 0}
        for hd in range(6):
            wq = load_win(l, OFF["hq"] + hd * 128, 128)
            for g in range(NG):
                ps = pbank()
                proj_fm(ps, wq, 0, 128, g)
                cp(qT_[:, g * 512:(g + 1) * 512], ps, eng="act")
            wv = load_win(l, OFF["hi"] + hd * 128, 128)
            for g in range(NG):
                ps = pbank()
                for j in range(4):
                    proj_tm(ps[:, j * 128:(j + 1) * 128], wv, 0, 128, g * 4 + j)
                cp(T(vtok.ap[:, g * 512:(g + 1) * 512], vtok.buf), ps, eng="act")
            wz = load_win(l, OFF["hz"] + hd * 128, 128)
            for g in range(NG):
                ps = pbank()
                proj_fm(ps, wz, 0, 128, g)
                act(sz[:, g * 512:(g + 1) * 512], ps, AF.Silu)
            for dr in range(2):
                wf = load_win(l, OFF["hff" if dr == 0 else "hfb"] + hd * 128, 128)
                lbc = lbv[:, l * 12 + dr * 6 + hd:l * 12 + dr * 6 + hd + 1]
                olbc = omlb[:, l * 12 + dr * 6 + hd:l * 12 + dr * 6 + hd + 1]
                for g in range(NG):
                    gs = slice(g * 512, (g + 1) * 512)
                    ps = pbank()
                    proj_fm(ps, wf, 0, 128, g)
                    sg = tf()
                    act(sg, ps, AF.Sigmoid)
                    ff = tf()
                    ts(ff, sg, olbc, ALU.mult, lbc, ALU.add)
                    act(gtmp[:, gs], ff, AF.Ln)
                    ts(kT_[:, gs], ff, -1.0, ALU.mult, 1.0, ALU.add)
                memset(a_pad[:, 0:1], 0.0, eng="dve")
                scan_cumsum(a_pad[:, 1:S + 1], T(ones_f.ap[:, 0:1].to_broadcast([128, S]), ones_f.buf), gtmp[:, 0:S])
                ts(na_pad[:, 0:S + 1], a_pad[:, 0:S + 1], -1.0, ALU.mult)
                order = list(range(NT)) if dr == 0 else list(range(NT - 1, -1, -1))
                units = []
                for i_, c in enumerate(order):
                    units.append(dict(c=c, first=(i_ == 0), u=cnt["u"]))
                    cnt["u"] += 1

                def st0(un, dr=dr):
                    c, u = un["c"], un["u"]
                    c0 = c * 128
                    s0 = u % NS0
                    sx = u % NSXH
                    eq, ekr, qtl, kend, kendT = tl["eq"][s0], tl["ekr"][s0], tl["qtl"][s0], tl["kend"][s0], tl["kendT"][s0]
                    eq2 = tl["eq2"][s0]
                    ek = tl["ek"][s0]
                    qc, wT = tl["qc"][sx], tl["wT"][sx]
                    dcol = tl["dec"][:, sx:sx + 1]
                    if dr == 0:
                        for I in range(4):
                            act(eq[:, 32 * I:32 * I + 32], a_pad[:, 1 + c0 + 32 * I:1 + c0 + 32 * I + 32], AF.Exp,
                                bias=na_pad[:, c0 + 32 * I:c0 + 32 * I + 1])
                        tt(qtl, qT_[:, c0:c0 + 128], eq, ALU.mult)
                        for I in range(4):
                            w_ = 32 * (I + 1)
                            act(ekr[w_], na_pad[:, 1 + c0:1 + c0 + w_], AF.Exp, bias=a_pad[:, c0 + 32 * I:c0 + 32 * I + 1])
                            tt(ek[I][:, 0:w_], kT_[:, c0:c0 + w_], ekr[w_], ALU.mult)
                        act(eq2, a_pad[:, 1 + c0:1 + c0 + 128], AF.Exp, bias=na_pad[:, c0:c0 + 1])
                        tt(qc, qT_[:, c0:c0 + 128], eq2, ALU.mult)
                        act(ekr["kend"], na_pad[:, 1 + c0:1 + c0 + 128], AF.Exp, bias=a_pad[:, c0 + 128:c0 + 129])
                        tt(kend, kT_[:, c0:c0 + 128], ekr["kend"], ALU.mult)
                        mask = U_f
                    else:
                        for I in range(4):
                            act(eq[:, 32 * I:32 * I + 32], na_pad[:, c0 + 32 * I:c0 + 32 * I + 32], AF.Exp,
                                bias=a_pad[:, c0 + 32 * (I + 1):c0 + 32 * (I + 1) + 1])
                        tt(qtl, qT_[:, c0:c0 + 128], eq, ALU.mult)
                        for I in range(4):
                            lo = 32 * I
                            act(ekr[128 - lo], a_pad[:, c0 + lo:c0 + 128], AF.Exp,
                                bias=na_pad[:, c0 + 32 * (I + 1):c0 + 32 * (I + 1) + 1])
                            tt(ek[I][:, lo:128], kT_[:, c0 + lo:c0 + 128], ekr[128 - lo], ALU.mult)
                        act(eq2, na_pad[:, c0:c0 + 128], AF.Exp, bias=a_pad[:, c0 + 128:c0 + 129])
                        tt(qc, qT_[:, c0:c0 + 128], eq2, ALU.mult)
                        act(ekr["kend"], a_pad[:, c0:c0 + 128], AF.Exp, bias=na_pad[:, c0:c0 + 1])
                        tt(kend, kT_[:, c0:c0 + 128], ekr["kend"], ALU.mult)
                        mask = L_f
                    act(dcol, a_pad[:, c0 + 128:c0 + 129], AF.Exp, bias=na_pad[:, c0:c0 + 1])
                    pb = pbank()
                    pss = pb[:, 0:128]
                    pst = pb[:, 128:256]
                    for I in range(4):
                        mm(pss[:, 32 * I:32 * I + 32], ek[I], qtl[:, 32 * I:32 * I + 32])
                    tt(wT, pss, mask, ALU.mult)
                    transp(pst, kend, ident_f)
                    cp(kendT, pst, eng="act")
                    psd = xbank()[:, 0:128]
                    mm(psd, kendT, vt3[:, c, :])
                    un.update(qc=qc, wT=wT, dcol=dcol, psd=psd)

                def st1(un, dr=dr):
                    c, first = un["c"], un["first"]
                    c0 = c * 128
                    qc, wT, dcol, psd = un["qc"], un["wT"], un["dcol"], un["psd"]
                    pso = pquart()
                    mm(pso, vt3[:, c, :], wT, start=True, stop=first)
                    if not first:
                        mm(pso, tl["Sbf"], qc, start=False, stop=True)
                    if first:
                        cp(tl["Sst"], psd)
                    else:
                        stt(tl["Sst"], tl["Sst"], dcol, psd, ALU.mult, ALU.add)
                    cp(tl["Sbf"], tl["Sst"], eng="act")
                    if dr == 0:
                        cp(oT[:, c0:c0 + 128], pso, eng="act")
                    else:
                        tt(oT[:, c0:c0 + 128], oT[:, c0:c0 + 128], pso, ALU.add)

                run_pipeline(units, [st0, st1], SKEW)
            yh = ys[hd % 3]
            for g in range(NG):
                gs = slice(g * 512, (g + 1) * 512)
                rstd = tf()
                sumsq_rstd(rstd, [(oT[:, gs], 128)], 128, 512)
                y1 = tf()
                tt(y1, oT[:, gs], rstd, ALU.mult)
                stt(yh[:, gs], y1, hgs[:, l * 6 + hd:l * 6 + hd + 1], sz[:, gs], ALU.mult, ALU.mult)
            if hd == 0:
                dbg_out("yh", yh, [128, S])
            if hd % 3 == 2:
                h0 = hd - 2
                out_proj(l, [((lambda g, j=j: ys[j][:, g * 512:(g + 1) * 512]), 128, Y_OFF["h"] + (h0 + j) * 128) for j in range(3)])

    def mlstm_group(l):
        xmA = W_f32[0]
        xmB = W_f32[1]
        cacc = W_f32[2]
        xcA = W_bf[0]
        xcB = W_bf[1]
        mqA, mqB, mkA, mkB = W_bf[2], W_bf[3], W_bf[4], W_bf[5]
        hfA, hfB = W_bf[6], W_bf[7]
        yA, yB = W_bf[8], W_bf[9]
        xmbA = sb([128, S], BF16, "xmbA") if not hasattr(mlstm_group, "_t") else mlstm_group._t["xmbA"]
        if not hasattr(mlstm_group, "_t"):
            t_ = dict(xmbA=xmbA)
            t_["xmbB"] = sb([128, S], BF16, "xmbB")
            t_["vtok"] = sb([128, NT, 200], BF16, "mvtok")
            t_["ktok"] = sb([128, NT, 192], BF16, "mktok")
            t_["gates"] = sb([128, NT, 16], F32, "mgates")
            t_["lf"] = sb([128, NT, 8], F32, "mlf")
            t_["eib"] = sb([128, NT, 8], F32, "meib")
            t_["bd"] = sb([128, 6 * 128], F32, "mbd")
            t_["bdB"] = sb([64, 6 * 64], F32, "mbdB")
            t_["bdb"] = sb([128, 3 * 128], BF16, "mbdb")
            t_["bdbB"] = sb([64, 3 * 64], BF16, "mbdbB")
            t_["wg"] = sb([128, 4 * 3 * 16], F32, "mwg")
            t_["wgB"] = sb([64, 4 * 3 * 16], F32, "mwgB")
            t_["G"] = sb([128, 4 * 2 * 16], BF16, "mG")
            t_["GB"] = sb([64, 4 * 2 * 16], BF16, "mGB")
            t_["lfrep"] = [sb([128, 128], F32, "mlfrep%d" % i) for i in range(2)]
            t_["Eb"] = [sb([128, 128], F32, "mEb%d" % i) for i in range(2)]
            t_["EbM"] = [sb([128, 128], F32, "mEbM%d" % i) for i in range(2)]
            t_["wT"] = [sb([128, 128], BF16, "mwT%d" % i) for i in range(2)]
            t_["qsA"] = [sb([128, 128], BF16, "mqsA%d" % i) for i in range(2)]
            t_["qsB"] = [sb([64, 128], BF16, "mqsB%d" % i) for i in range(2)]
            t_["dm"] = [sb([128, 128], F32, "mdm%d" % i) for i in range(2)]
            t_["rd"] = [sb([128, 128], F32, "mrd%d" % i) for i in range(2)]
            t_["CA"] = sb([128, 200], F32, "mCA")
            t_["CB"] = sb([64, 200], F32, "mCB")
            t_["CtA"] = sb([128, 200], F32, "mCtA")
            t_["CtB"] = sb([64, 200], F32, "mCtB")
            t_["CbA"] = sb([128, 200], BF16, "mCbA")
            t_["CbB"] = sb([64, 200], BF16, "mCbB")
            t_["nrA"] = sb([128, 128], BF16, "mnrA")
            t_["nrB"] = sb([64, 128], BF16, "mnrB")
            t_["eibrep"] = [sb([128, 128], BF16, "meibrep%d" % i) for i in range(2)]
            t_["hbA"] = sb([128, 512], F32, "mhbA")
            t_["hbB"] = sb([64, 512], F32, "mhbB")
            t_["so"] = [sb([128, 512], BF16, "mso%d" % i) for i in range(4)]
            mlstm_group._t = t_
        t_ = mlstm_group._t
        xmbB = t_["xmbB"]
        vtok, ktok, gates, lf, eib = t_["vtok"], t_["ktok"], t_["gates"], t_["lf"], t_["eib"]
        ts(mgsA[:, l * 4:(l + 1) * 4], mvA[:, l * 12 + 8:l * 12 + 12], float(math.sqrt(192.0)), ALU.mult)
        ts(mgsB[:, l * 4:(l + 1) * 4], mvB[:, l * 12 + 8:l * 12 + 12], float(math.sqrt(192.0)), ALU.mult)
        QSCALE = float(192.0 ** -0.5)

        def compute_xm_xc(j):
            for (xm_, xmb_, xc_, kk, coff, cw_, mv_) in ((xmA, xmbA, xcA, 128, 0, cwA, mvA), (xmB, xmbB, xcB, 64, 128, cwB, mvB)):
                wx = load_win(l, OFF["xm"] + j * 192 + coff, kk)
                memset(xm_[0:kk, 0:2], 0.0, eng="dve")
                memset(xm_[0:kk, S + 2:S + 4], 0.0, eng="dve")
                for g in range(NG):
                    ps = pbank()
                    proj_fm(ps, wx, 0, kk, g)
                    cp(xm_[0:kk, 2 + g * 512:2 + (g + 1) * 512], ps[0:kk, :], eng="act")
                    cp(xmb_[0:kk, g * 512:(g + 1) * 512], ps[0:kk, :], eng="act")
                cb = l * 20 + j * 5
                ts(cacc[0:kk, 0:S], xm_[0:kk, 0:S], cw_[0:kk, cb:cb + 1], ALU.mult)
                for k in range(1, 5):
                    stt(cacc[0:kk, 0:S], xm_[0:kk, k:k + S], cw_[0:kk, cb + k:cb + k + 1], cacc[0:kk, 0:S],
                        ALU.mult, ALU.add)
                act(xc_[0:kk, 0:S], cacc[0:kk, 0:S], AF.Silu, bias=mv_[0:kk, l * 12 + j:l * 12 + j + 1])

        dma_in(t_["wg"], d_wgA[l])
        dma_in(t_["wgB"], d_wgB[l])
        psg = banks[0]
        psg3 = T(psg.ap[:, 0:NT * 16].rearrange("p (t c) -> p t c", c=16), psg.buf)
        for j in range(4):
            dma_in(t_["bd"], d_bdA[l, j])
            dma_in(t_["bdB"], d_bdB[l, j])
            for (bd_, wg_, G_, kk) in ((t_["bd"], t_["wg"], t_["G"], 128), (t_["bdB"], t_["wgB"], t_["GB"], 64)):
                pq = pquart()
                mm(pq[0:kk, 0:16], bd_[0:kk, 3 * kk:4 * kk], wg_[0:kk, (j * 3 + 0) * 16:(j * 3 + 1) * 16], start=True, stop=False)
                mm(pq[0:kk, 0:16], bd_[0:kk, 4 * kk:5 * kk], wg_[0:kk, (j * 3 + 1) * 16:(j * 3 + 2) * 16], start=False, stop=True)
                mm(pq[0:kk, 16:32], bd_[0:kk, 5 * kk:6 * kk], wg_[0:kk, (j * 3 + 2) * 16:(j * 3 + 3) * 16], start=True, stop=True)
                cp(G_[0:kk, j * 32:(j + 1) * 32], pq[0:kk, 0:32])
            compute_xm_xc(j)
            for t in range(NT):
                tsl = slice(t * 128, (t + 1) * 128)
                mm(psg3[:, t, :], xcA[:, tsl], t_["G"][:, j * 32:j * 32 + 16], start=True, stop=False)
                mm(psg3[:, t, :], xmbA[:, tsl], t_["G"][:, j * 32 + 16:j * 32 + 32], start=False, stop=False)
                mm(psg3[:, t, :], xcB[0:64, tsl], t_["GB"][0:64, j * 32:j * 32 + 16], start=False, stop=False)
                mm(psg3[:, t, :], xmbB[0:64, tsl], t_["GB"][0:64, j * 32 + 16:j * 32 + 32], start=False, stop=True)
            if j == 0:
                for t in range(NT):
                    tt(gates[:, t, :], psg3[:, t, :], bgs[:, l * 16:(l + 1) * 16], ALU.add)
            else:
                tt(gates, gates, psg3, ALU.add)
        etmp = sb([128, NT, 8], F32, "metmp") if "etmp" not in t_ else t_["etmp"]
        t_["etmp"] = etmp
        act(etmp[:, :, 0:4], gates[:, :, 4:8], AF.Exp, scale=-1.0)
        act(etmp[:, :, 4:8], gates[:, :, 12:16], AF.Exp, scale=-1.0)
        act(lf, etmp, AF.Ln, bias=one_c)
        ts(lf, lf, -1.0, ALU.mult)
        bcs = sb([128, NT, 8], F32, "mbcs") if "bcs" not in t_ else t_["bcs"]
        t_["bcs"] = bcs
        psb = banks[1]
        psb3 = T(psb.ap[:, 0:NT * 8].rearrange("p (t c) -> p t c", c=8), psb.buf)
        for t in range(NT):
            mm(psb3[:, t, 0:4], U_f, lf[:, t, 0:4])
            mm(psb3[:, t, 4:8], L_f, lf[:, t, 4:8])
        cp(bcs, psb3)
        tt(etmp[:, :, 0:4], gates[:, :, 0:4], bcs[:, :, 0:4], ALU.subtract)
        tt(etmp[:, :, 4:8], gates[:, :, 8:12], bcs[:, :, 4:8], ALU.subtract)
        act(eib, etmp, AF.Exp)
        dbg_out("gates", T(gates.ap.rearrange("p t c -> p (t c)"), gates.buf), [128, NT * 16], F32)

        cntu = {"u": 0}
        for j in range(4):
            dma_in(t_["bd"], d_bdA[l, j])
            dma_in(t_["bdB"], d_bdB[l, j])
            cp(t_["bdb"], t_["bd"][:, 0:384], eng="pool")
            cp(t_["bdbB"], t_["bdB"][:, 0:192], eng="pool")
            bdb, bdbB = t_["bdb"], t_["bdbB"]
            compute_xm_xc(j)
            for g in range(NG):
                gs = slice(g * 512, (g + 1) * 512)
                for (dst, src, w_, kk, sc) in ((mqA, xcA, bdb[:, 0:128], 128, QSCALE), (mqB, xcB, bdbB[:, 0:64], 64, QSCALE),
                                               (mkA, xcA, bdb[:, 128:256], 128, 1.0), (mkB, xcB, bdbB[:, 64:128], 64, 1.0)):
                    ps = pbank()
                    mm(ps[0:kk, :], w_[0:kk, :], src[0:kk, gs])
                    act(dst[0:kk, gs], ps[0:kk, :], AF.Copy, scale=sc)
            for t in range(NT):
                tsl = slice(t * 128, (t + 1) * 128)
                ps = pbank()
                mm(ps[:, 0:128], xmbA[:, tsl], bdb[:, 256:384])
                mm(ps[:, 128:192], xmbB[0:64, tsl], bdbB[0:64, 128:192])
                mm(ps[:, 192:320], xcA[:, tsl], bdb[:, 128:256])
                mm(ps[:, 320:384], xcB[0:64, tsl], bdbB[0:64, 64:128])
                cp(vtok[:, t, 0:192], ps[:, 0:192], eng="act")
                cp(ktok[:, t, :], ps[:, 192:384], eng="act")
            memset(vtok[:, :, 192:193], 1.0, eng="dve")
            NSX = 4
            if "vp" not in t_:
                t_["vp"] = [sb([128, 200], BF16, "mvp%d" % i) for i in range(NSX)]
                for nm_, shp_, dt_ in (("Eb", [128, 128], F32), ("wT", [128, 128], BF16), ("qsA", [128, 128], BF16),
                                       ("qsB", [64, 128], BF16), ("eibrep", [128, 128], BF16)):
                    t_[nm_] = t_[nm_] + [sb(shp_, dt_, "mx%s%d" % (nm_, i)) for i in range(2, NSX)]
            for dr in range(2):
                order = list(range(NT)) if dr == 0 else list(range(NT - 1, -1, -1))
                gcol = dr * 4 + j
                tri = U_f if dr == 0 else L_f
                mneg = mnegF if dr == 0 else mnegB
                units = []
                for i_, c in enumerate(order):
                    units.append(dict(c=c, first=(i_ == 0), u=cntu["u"]))
                    cntu["u"] += 1

                def st0(un, dr=dr, gcol=gcol, tri=tri, mneg=mneg):
                    c, u, first = un["c"], un["u"], un["first"]
                    csl = slice(c * 128, (c + 1) * 128)
                    sx = u % NSX
                    Eb, wT, qsA, qsB = t_["Eb"][sx], t_["wT"][sx], t_["qsA"][sx], t_["qsB"][sx]
                    vpc, eibrep = t_["vp"][sx], t_["eibrep"][sx]
                    EbM, lfrep = t_["EbM"][u % 2], t_["lfrep"][u % 2]
                    ts(vpc[:, 0:193], vtok[:, c, 0:193], eib[:, c, gcol:gcol + 1], ALU.mult)
                    act(eibrep, ones_f, AF.Copy, scale=eib[:, c, gcol:gcol + 1])
                    bk1 = pbank()
                    pss = bk1[:, 0:128]
                    psl = bk1[:, 128:256]
                    pslm = bk1[:, 256:384]
                    mm(pss, mkA[:, csl], mqA[:, csl], start=True, stop=False)
                    mm(pss, mkB[0:64, csl], mqB[0:64, csl], start=False, stop=True)
                    act(lfrep, ones_f, AF.Copy, scale=lf[:, c, gcol:gcol + 1])
                    mm(psl, lfrep, tri)
                    mm(pslm, lfrep, tri, start=True, stop=False)
                    mm(pslm, ident_f, mneg, start=False, stop=True)
                    act(Eb, psl, AF.Exp)
                    act(EbM, pslm, AF.Exp)
                    tt(wT, pss, EbM, ALU.mult)
                    if not first:
                        tt(qsA, mqA[:, csl], Eb, ALU.mult)
                        tt(qsB, mqB[0:64, csl], Eb[0:64, :], ALU.mult)
                    bk3 = xbank()
                    pcA = bk3[:, 0:256]
                    pcB = bk3[:, 256:512]
                    mm(pcA[:, 0:193], ktok[:, c, 0:128], vpc[:, 0:193])
                    mm(pcB[0:64, 0:193], ktok[:, c, 128:192], vpc[:, 0:193])
                    un.update(Eb=Eb, wT=wT, qsA=qsA, qsB=qsB, vpc=vpc, eibrep=eibrep, pcA=pcA, pcB=pcB)

                def st1(un, dr=dr):
                    c, u, first = un["c"], un["u"], un["first"]
                    csl = slice(c * 128, (c + 1) * 128)
                    Eb, wT, qsA, qsB = un["Eb"], un["wT"], un["qsA"], un["qsB"]
                    vpc, eibrep, pcA, pcB = un["vpc"], un["eibrep"], un["pcA"], un["pcB"]
                    dm, rd = t_["dm"][u % 2], t_["rd"][u % 2]
                    bk2 = pbank()
                    pnA = bk2[:, 0:128]
                    pnB = bk2[:, 128:256]
                    pdn = bk2[:, 256:384]
                    mm(pnA, vpc[:, 0:128], wT, start=True, stop=first)
                    if not first:
                        mm(pnA, t_["CbA"][:, 0:128], qsA, start=False, stop=False)
                        mm(pnA, t_["CbB"][:, 0:128], qsB, start=False, stop=True)
                    mm(pnB[0:64, :], vpc[:, 128:192], wT, start=True, stop=first)
                    if not first:
                        mm(pnB[0:64, :], t_["CbA"][:, 128:192], qsA, start=False, stop=False)
                        mm(pnB[0:64, :], t_["CbB"][:, 128:192], qsB, start=False, stop=True)
                    mm(pdn, eibrep, wT, start=True, stop=first)
                    if not first:
                        mm(pdn, t_["nrA"], qsA, start=False, stop=False)
                        mm(pdn, t_["nrB"], qsB, start=False, stop=True)
                    ebl = Eb[:, 127:128] if dr == 0 else Eb[:, 0:1]
                    if first:
                        act(t_["CA"][:, 0:193], pcA[:, 0:193], AF.Copy, scale=ebl)
                        act(t_["CB"][:, 0:193], pcB[0:64, 0:193], AF.Copy, scale=ebl[0:64, :])
                        act(t_["CbA"][:, 0:193], pcA[:, 0:193], AF.Copy, scale=ebl)
                        act(t_["CbB"][:, 0:193], pcB[0:64, 0:193], AF.Copy, scale=ebl[0:64, :])
                    else:
                        tt(t_["CtA"][:, 0:193], pcA[:, 0:193], t_["CA"][:, 0:193], ALU.add)
                        tt(t_["CtB"][:, 0:193], pcB[0:64, 0:193], t_["CB"][:, 0:193], ALU.add)
                        act(t_["CA"][:, 0:193], t_["CtA"][:, 0:193], AF.Copy, scale=ebl)
                        act(t_["CB"][:, 0:193], t_["CtB"][:, 0:193], AF.Copy, scale=ebl[0:64, :])
                        act(t_["CbA"][:, 0:193], t_["CtA"][:, 0:193], AF.Copy, scale=ebl)
                        act(t_["CbB"][:, 0:193], t_["CtB"][:, 0:193], AF.Copy, scale=ebl[0:64, :])
                    act(t_["nrA"], ones_f, AF.Copy, scale=t_["CA"][:, 192:193])
                    act(t_["nrB"], ones_f[0:64, :], AF.Copy, scale=t_["CB"][:, 192:193])
                    ts(rd, pdn, -1.0, ALU.mult, 1.0, ALU.max)
                    stt(dm, pdn, 1.0, rd, ALU.max, ALU.max)
                    recip(rd, dm)
                    if dr == 0:
                        tt(hfA[:, csl], pnA, rd, ALU.mult)
                        tt(hfB[0:64, csl], pnB[0:64, :], rd[0:64, :], ALU.mult)
                    else:
                        o4 = (c % 4) * 128
                        tt(t_["hbA"][:, o4:o4 + 128], pnA, rd, ALU.mult)
                        tt(t_["hbB"][0:64, o4:o4 + 128], pnB[0:64, :], rd[0:64, :], ALU.mult)
                    if dr == 1 and c % 4 == 0:
                        g = c // 4
                        gs = slice(g * 512, (g + 1) * 512)
                        hbA, hbB = t_["hbA"], t_["hbB"]
                        tt(hbA, hbA, hfA[:, gs], ALU.add)
                        tt(hbB, hbB, hfB[0:64, gs], ALU.add)
                        so = t_["so"]
                        for (idx, nm, coff, kk, fn) in ((0, "om", 0, 128, AF.Sigmoid), (1, "om", 128, 64, AF.Sigmoid),
                                                        (2, "zm", 0, 128, AF.Silu), (3, "zm", 128, 64, AF.Silu)):
                            wz = load_win(l, OFF[nm] + j * 192 + coff, kk)
                            ps = pbank()
                            proj_fm(ps, wz, 0, kk, g)
                            act(so[idx][0:kk, :], ps[0:kk, :], fn)
                        tt(hbA, hbA, so[0], ALU.mult)
                        tt(hbB, hbB, so[1][0:64, :], ALU.mult)
                        rstd = tf()
                        sumsq_rstd(rstd, [(hbA, 128), (hbB, 64)], 192, 512)
                        tt(hbA, hbA, rstd, ALU.mult)
                        tt(hbB, hbB, rstd[0:64, :], ALU.mult)
                        skA = tf()
                        skB = tf()
                        act(skA, xcA[:, gs], AF.Copy, scale=mvA[:, l * 12 + 4 + j:l * 12 + 5 + j])
                        act(skB[0:64, :], xcB[0:64, gs], AF.Copy, scale=mvB[:, l * 12 + 4 + j:l * 12 + 5 + j])
                        stt(hbA, hbA, mgsA[:, l * 4 + j:l * 4 + j + 1], skA, ALU.mult, ALU.add)
                        stt(hbB, hbB, mgsB[:, l * 4 + j:l * 4 + j + 1], skB[0:64, :], ALU.mult, ALU.add)
                        tt(yA[:, gs], hbA, so[2], ALU.mult)
                        tt(yB[0:64, gs], hbB, so[3][0:64, :], ALU.mult)

                run_pipeline(units, [st0, st1], SKEW)
            if j == 0:
                dbg_out("ym", yA, [128, S])
            out_proj(l, [((lambda g: yA[:, g * 512:(g + 1) * 512]), 128, Y_OFF["m"] + j * 192),
                         ((lambda g: yB[0:64, g * 512:(g + 1) * 512]), 64, Y_OFF["m"] + j * 192 + 128)])

    cur = {"b": 0}
    for l in range(NL):
        lb_for_layer(l, l)

    for b in range(NSEQ):
        cur["b"] = b
        xsrc[b] = [d_x[b, t * 128:(t + 1) * 128, :] for t in range(NT)]
        for l in range(NL):
            for t in range(NT):
                g = t // 4
                o = (t % 4) * 128
                rrx["i"] = (rrx["i"] + 1) % 2
                xt = xt_tiles[rrx["i"]]
                P.op("sp", (lambda dst, s_: (lambda e: e.dma_start(out=dst.ap, in_=s_)))(xt, xsrc[b][t]),
                     reads=[xdr[b][t]], writes=[xt], is_dma=True, dkey=xt.buf.id)
                xi = xin[t % 2]
                P.op("act", (lambda o_, i_, a_: (lambda e: e.activation(out=o_.ap, in_=i_.ap, func=AF.Square, accum_out=a_.ap)))(xi, xt, ssq[:, 0:1]),
                     reads=[xt], writes=[xi, ssq])
                rstd_from(ssq[:, 1:2], ssq[:, 0:1], 1024)
                ts(xi, xt, ssq[:, 1:2], ALU.mult)
                for kq in range(2):
                    ps = pbank()
                    for k4 in range(4):
                        k = kq * 4 + k4
                        transp(ps[:, k4 * 128:(k4 + 1) * 128], xi[:, k * 128:(k + 1) * 128], ident_f)
                    for k4 in range(4):
                        k = kq * 4 + k4
                        ts(hTg[k][g][:, o:o + 128], ps[:, k4 * 128:(k4 + 1) * 128], ng32[:, l * 8 + k:l * 8 + k + 1], ALU.mult)
            if "a" in groups:
                ctab = W_f32[0]
                stab = W_f32[1]
                angt = W_f32[2]
                posi = T(angt.ap[:, 0:S].bitcast(I32), angt.buf)
                dma_in(posi, d_pos[b:b + 1, :].to_broadcast([128, S]))
                cp(angt[:, 0:S], posi)
                ts(angt[:, 0:S], angt[:, 0:S], invf, ALU.mult)
                ts(ctab[:, 0:S], angt[:, 0:S], float(1.0 / (2 * math.pi)), ALU.mult)
                ki = T(stab.ap[:, 0:S].bitcast(I32), stab.buf)
                cp(ki, ctab[:, 0:S])
                cp(ctab[:, 0:S], ki)
                stt(angt[:, 0:S], ctab[:, 0:S], float(-2 * math.pi), angt[:, 0:S], ALU.mult, ALU.add)
                act(stab[:, 0:S], angt[:, 0:S], AF.Sin, scale=0.25)
                tt(stab[:, 0:S], stab[:, 0:S], stab[:, 0:S], ALU.mult)
                ts(stab[:, 0:S], stab[:, 0:S], -2.0, ALU.mult, 1.0, ALU.add)
                act(angt[:, 0:S], angt[:, 0:S], AF.Sin, scale=0.5)
                stt(stab[:, 0:S], angt[:, 0:S], 2.0, stab[:, 0:S], ALU.mult, ALU.mult)
                ts(stab[:, 0:S], stab[:, 0:S], sgn, ALU.mult)
                tt(ctab[:, 0:S], angt[:, 0:S], angt[:, 0:S], ALU.mult)
                ts(ctab[:, 0:S], ctab[:, 0:S], -2.0, ALU.mult, 1.0, ALU.add)
                attention_group(l, ctab, stab)
            excl["on"] = True
            if "h" in groups:
                hgrn_group(l)
            if "m" in groups:
                mlstm_group(l)
            excl["on"] = False
        if final_norm:
            dma_in(fgb, d_fgrow[0:1, :].to_broadcast([128, 1024]))
            ts(fgb, fgb, 32.0, ALU.mult)
        for t in range(NT):
            rrx["i"] = (rrx["i"] + 1) % 2
            xt = xt_tiles[rrx["i"]]
            P.op("sp", (lambda dst, s_: (lambda e: e.dma_start(out=dst.ap, in_=s_)))(xt, xsrc[b][t]),
                 reads=[xdr[b][t]], writes=[xt], is_dma=True, dkey=xt.buf.id)
            if final_norm:
                xi = xin[t % 2]
                P.op("act", (lambda o_, i_, a_: (lambda e: e.activation(out=o_.ap, in_=i_.ap, func=AF.Square, accum_out=a_.ap)))(xi, xt, ssq[:, 2:3]),
                     reads=[xt], writes=[xi, ssq])
                rstd_from(ssq[:, 3:4], ssq[:, 2:3], 1024)
                stt(xt, xt, ssq[:, 3:4], fgb, ALU.mult, ALU.mult)
            tok = T(None)
            P.op("sp", (lambda src_, d_: (lambda e: e.dma_start(out=d_, in_=src_.ap)))(xt, d_out[b, t * 128:(t + 1) * 128, :]),
                 reads=[xt], writes=[xdr[b][t], tok], is_dma=True, dkey="o%d" % xt.buf.id)
            fin.append(tok)
    P.op("sp", None, reads=fin)
    P.emit(nc, ctx)
    P.sbuf_left = nc.sbuf_bytes_remaining
    ctx.close()
    return nc, P, dbg_outs


def prep_params(inp, layers, depth_all):
    NL = len(layers)
    f = np.float32
    out = {}
    out["w_in"] = np.ascontiguousarray(inp["w_in"][layers], dtype=f)
    out["w_out"] = np.ascontiguousarray(inp["w_out"][layers], dtype=f)
    out["consts"] = make_consts()
    ng = inp["norm_g"][layers].reshape(NL, 8, 128)
    out["ng"] = np.ascontiguousarray(ng.transpose(2, 0, 1).reshape(128, NL * 8), dtype=f)
    out["fgrow"] = np.ascontiguousarray(inp["final_g"].reshape(1, 1024), dtype=f)
    out["alam"] = np.ascontiguousarray(inp["a_lambda"][layers].reshape(1, NL * 256), dtype=f)
    out["ang"] = np.ascontiguousarray(inp["a_norm_g"][layers].reshape(NL, 4, 128).transpose(2, 0, 1).reshape(128, NL * 4), dtype=f)
    lb = inp["h_lb_logits"].reshape(depth_all, 2, 6, 128)
    out["lb"] = np.ascontiguousarray(lb.transpose(3, 1, 2, 0).reshape(128, 12 * depth_all), dtype=f)
    out["hng"] = np.ascontiguousarray(inp["h_norm_g"][layers].reshape(NL, 6, 128).transpose(2, 0, 1).reshape(128, NL * 6), dtype=f)
    cw = inp["m_conv_w"][layers].reshape(NL, 5, 4, 192)
    out["cwA"] = np.ascontiguousarray(cw[:, :, :, 0:128].transpose(3, 0, 2, 1).reshape(128, NL * 20), dtype=f)
    out["cwB"] = np.ascontiguousarray(cw[:, :, :, 128:192].transpose(3, 0, 2, 1).reshape(64, NL * 20), dtype=f)
    mv = np.stack([inp["m_conv_b"][layers], inp["m_skip"][layers], inp["m_norm_g"][layers]], axis=1)
    mv = mv.reshape(NL, 3, 4, 192)
    out["mvA"] = np.ascontiguousarray(mv[:, :, :, 0:128].transpose(3, 0, 1, 2).reshape(128, NL * 12), dtype=f)
    out["mvB"] = np.ascontiguousarray(mv[:, :, :, 128:192].transpose(3, 0, 1, 2).reshape(64, NL * 12), dtype=f)
    bdA = np.zeros((NL, 4, 128, 6, 128), f)
    bdB = np.zeros((NL, 4, 64, 6, 64), f)
    for wi, nm in enumerate(("m_wq", "m_wk", "m_wv")):
        w = inp[nm][layers].reshape(NL, 4, 48, 4, 4)
        for g_ in range(32):
            bdA[:, :, 4 * g_:4 * g_ + 4, wi, 4 * g_:4 * g_ + 4] = w[:, :, g_]
            bdA[:, :, 4 * g_:4 * g_ + 4, 3 + wi, 4 * g_:4 * g_ + 4] = w[:, :, g_].transpose(0, 1, 3, 2)
        for g_ in range(16):
            bdB[:, :, 4 * g_:4 * g_ + 4, wi, 4 * g_:4 * g_ + 4] = w[:, :, 32 + g_]
            bdB[:, :, 4 * g_:4 * g_ + 4, 3 + wi, 4 * g_:4 * g_ + 4] = w[:, :, 32 + g_].transpose(0, 1, 3, 2)
    out["bdA"] = bdA.reshape(NL, 4, 128, 6 * 128)
    out["bdB"] = bdB.reshape(NL, 4, 64, 6 * 64)
    wg = inp["m_w_gates"][layers].reshape(NL, 3, 4, 192, 16)
    out["wgA"] = np.ascontiguousarray(wg[:, :, :, 0:128].transpose(0, 3, 2, 1, 4).reshape(NL, 128, 4 * 3 * 16), dtype=f)
    out["wgB"] = np.ascontiguousarray(wg[:, :, :, 128:192].transpose(0, 3, 2, 1, 4).reshape(NL, 64, 4 * 3 * 16), dtype=f)
    out["bg"] = np.ascontiguousarray(inp["m_b_gates"][layers].reshape(1, NL * 16), dtype=f)
    return out


_CACHE = {}


def _get_prog(S, NSEQ, NL, depth_all, lam_inits, final_norm):
    key = (S, NSEQ, NL, depth_all, tuple(lam_inits), final_norm)
    if key not in _CACHE:
        _CACHE[key] = build(S, NSEQ, NL, depth_all, lam_inits, final_norm=final_norm)[0]
    return _CACHE[key]


def kernel(**inputs):
    inp = {k: np.asarray(v) for k, v in inputs.items()}
    x = np.ascontiguousarray(inp["x"], dtype=np.float32)
    pos = np.ascontiguousarray(inp["positions"], dtype=np.int32)
    B, S, D = x.shape
    DEPTH = inp["w_in"].shape[0]
    NCORE = 8
    per = B // NCORE
    lam_all = [0.8 - 0.6 * math.exp(-0.3 * l) for l in range(DEPTH)]
    params = prep_params(inp, list(range(DEPTH)), DEPTH)
    nc = _get_prog(S, per, DEPTH, DEPTH, lam_all, True)
    in_maps = []
    for c in range(NCORE):
        m = dict(params)
        m["x"] = x[c * per:(c + 1) * per]
        m["pos"] = pos[c * per:(c + 1) * per]
        in_maps.append(m)
    res = run_bass_kernel_spmd(nc, in_maps, core_ids=list(range(NCORE)))
    out = np.concatenate([np.asarray(r["out"]) for r in res.results], axis=0)
    return out.astype(np.float32)
```

```python
import math
from contextlib import ExitStack
import numpy as np
import concourse.bass as bass
import concourse.mybir as mybir
from concourse.bass_utils import run_bass_kernel_spmd

F32 = mybir.dt.float32
BF16 = mybir.dt.bfloat16
I32 = mybir.dt.int32
ALU = mybir.AluOpType
AF = mybir.ActivationFunctionType
AX = mybir.AxisListType

ENGS = ("pe", "act", "dve", "pool", "sp")
SEM_WRAP = 30000
EPS = 1e-6


class Buf:
    __slots__ = ("last_w", "readers", "id")
    _n = 0

    def __init__(self):
        self.last_w = None
        self.readers = []
        Buf._n += 1
        self.id = Buf._n


class T:
    __slots__ = ("ap", "buf")

    def __init__(self, ap, buf=None):
        self.ap = ap
        self.buf = buf if buf is not None else Buf()

    def __getitem__(self, idx):
        return T(self.ap[idx], self.buf)

    def sub(self, idx):
        return T(self.ap[idx], Buf())


class Op:
    __slots__ = ("eng", "fn", "deps", "idx", "is_dma", "sig", "dkey")


class Prog:
    def __init__(self):
        self.ops = []

    def op(self, eng, fn, reads=(), writes=(), is_dma=False, dkey=None):
        o = Op()
        o.eng = eng
        o.fn = fn
        o.is_dma = is_dma
        o.dkey = dkey
        o.sig = None
        o.idx = len(self.ops)
        deps = set()
        for t in reads:
            b = t.buf
            if b.last_w is not None:
                deps.add(b.last_w)
        for t in writes:
            b = t.buf
            if b.last_w is not None:
                deps.add(b.last_w)
            deps.update(b.readers)
        for t in reads:
            t.buf.readers.append(o.idx)
        for t in writes:
            t.buf.last_w = o.idx
            t.buf.readers = []
        deps.discard(o.idx)
        o.deps = deps
        self.ops.append(o)
        return o

    def emit(self, nc, ctx):
        ops = self.ops
        needed = set()
        for o in ops:
            best = {}
            for d in o.deps:
                p = ops[d]
                if p.eng == "pe" and o.eng == "pe" and not p.is_dma and not o.is_dma:
                    continue
                key = ("dma", p.dkey) if p.is_dma else ("eng", p.eng)
                if key not in best or best[key] < d:
                    best[key] = d
            o.deps = sorted(best.values())
            needed.update(o.deps)
        counters = {}
        sems = {}

        def getsem(name):
            if name not in sems:
                sems[name] = ctx.enter_context(nc.semaphore(name))
            return sems[name]

        for o in ops:
            if o.idx not in needed and not (o.is_dma and o.fn is not None):
                continue
            if o.is_dma:
                base = "d%s" % (o.dkey,)
                inc = 16
            else:
                base = "e" + o.eng
                inc = 1
            cnt, epoch = counters.get(base, (0, 0))
            if cnt + inc > SEM_WRAP:
                epoch += 1
                cnt = 0
            cnt += inc
            counters[base] = (cnt, epoch)
            o.sig = ("%s_%d" % (base, epoch), cnt, inc)
        for o in ops:
            if o.sig:
                getsem(o.sig[0])
        self.n_sems = len(sems)
        per_eng = {e: [] for e in ENGS}
        for o in ops:
            per_eng[o.eng].append(o)
        block = ctx.enter_context(nc.Block())

        def run(eng_obj, lst):
            last_wait = {}
            for o in lst:
                for d in o.deps:
                    sname, val, _ = ops[d].sig
                    if last_wait.get(sname, 0) >= val:
                        continue
                    last_wait[sname] = val
                    eng_obj.wait_ge(sems[sname], val)
                if o.fn is None:
                    continue
                ins = o.fn(eng_obj)
                if o.sig is not None:
                    ins.then_inc(sems[o.sig[0]], o.sig[2])

        @block.tensor
        def _(e):
            run(e, per_eng["pe"])

        @block.scalar
        def _(e):
            run(e, per_eng["act"])

        @block.vector
        def _(e):
            run(e, per_eng["dve"])

        @block.gpsimd
        def _(e):
            run(e, per_eng["pool"])

        @block.sync
        def _(e):
            run(e, per_eng["sp"])


D_MODEL = 1024
D_MIX = 2048
M_W = 768
H_W = 768
A_W = 512
IN_COLS = 8192
OFF = dict(xm=0, om=768, zm=1536, hq=2304, hff=3072, hfb=3840, hi=4608, hz=5376,
           aq=6144, ak=6656, av=7168, az=7680)
Y_OFF = dict(m=0, h=768, a=1536)
ROPE_THETA = 500000.0
NEG = -30000.0

C_IDENT = 0
C_U = 128
C_L = 256
C_PSW = 384
C_ONES = 512
C_VEC = 640
NCONST = 648


def make_consts():
    c = np.zeros((128, NCONST), np.float32)
    r = np.arange(128)
    c[:, C_IDENT:C_IDENT + 128] = np.eye(128, dtype=np.float32)
    c[:, C_U:C_U + 128] = (r[:, None] <= r[None, :]).astype(np.float32)
    c[:, C_L:C_L + 128] = (r[:, None] >= r[None, :]).astype(np.float32)
    psw = np.zeros((128, 128), np.float32)
    for m in range(128):
        d = m % 64
        if d < 8:
            psw[m + 8, m] = 1.0
        elif d < 16:
            psw[m - 8, m] = 1.0
    c[:, C_PSW:C_PSW + 128] = psw
    c[:, C_ONES:C_ONES + 128] = 1.0
    half = 8
    inv = ROPE_THETA ** (-np.arange(half, dtype=np.float32) / half)
    for p in range(128):
        d = p % 64
        if d < 16:
            c[p, C_VEC + 0] = inv[d % 8]
            c[p, C_VEC + 1] = -1.0 if d < 8 else 1.0
        c[p, C_VEC + 2] = 1.0 if p < 64 else 0.0
        c[p, C_VEC + 3] = 0.0 if p < 64 else 1.0
    c[:, C_VEC + 4] = 1024 * EPS
    c[:, C_VEC + 5] = 128 * EPS
    c[:, C_VEC + 6] = 192 * EPS
    c[:, C_VEC + 7] = 1.0
    return c


def build(S, NSEQ, NL, DEPTH_ALL, lam_inits, final_norm=True, groups=("a", "h", "m"), dbg=()):
    NT = S // 128
    NG = S // 512
    nc = bass.Bass("TRN2", target_bir_lowering=False)
    P = Prog()
    ctx = ExitStack()

    def dram(name, shape, dt=F32, kind="ExternalInput"):
        return nc.dram_tensor(name, shape, dt, kind=kind).ap()

    d_x = dram("x", [NSEQ, S, D_MODEL])
    d_pos = dram("pos", [NSEQ, S], I32)
    d_win = dram("w_in", [NL, D_MODEL, IN_COLS])
    d_wout = dram("w_out", [NL, D_MIX, D_MODEL])
    d_consts = dram("consts", [128, NCONST])
    d_ng = dram("ng", [128, NL * 8])
    d_fgrow = dram("fgrow", [1, 1024])
    d_alam = dram("alam", [1, NL * 256])
    d_ang = dram("ang", [128, NL * 4])
    d_lb = dram("lb", [128, 2 * 6 * DEPTH_ALL])
    d_hng = dram("hng", [128, NL * 6])
    d_cwA = dram("cwA", [128, NL * 4 * 5])
    d_cwB = dram("cwB", [64, NL * 4 * 5])
    d_mvA = dram("mvA", [128, NL * 4 * 3])
    d_mvB = dram("mvB", [64, NL * 4 * 3])
    d_bdA = dram("bdA", [NL, 4, 128, 6 * 128])
    d_bdB = dram("bdB", [NL, 4, 64, 6 * 64])
    d_wgA = dram("wgA", [NL, 128, 4 * 3 * 16])
    d_wgB = dram("wgB", [NL, 64, 4 * 3 * 16])
    d_bg = dram("bg", [1, NL * 16])
    d_out = dram("out", [NSEQ, S, D_MODEL], kind="ExternalOutput")
    dbg_outs = {}

    def sb(shape, dt=F32, name=None):
        return T(ctx.enter_context(nc.sbuf_tensor("s_" + name, shape, dt))[:])

    def dma_in(dst, src_ap, eng="sp"):
        P.op(eng, lambda e: e.dma_start(out=dst.ap, in_=src_ap), writes=[dst], is_dma=True, dkey=dst.buf.id)

    def dma_out(dst_ap, src, eng="sp"):
        tok = T(None)
        P.op(eng, lambda e: e.dma_start(out=dst_ap, in_=src.ap), reads=[src], writes=[tok], is_dma=True,
             dkey="o%d" % src.buf.id)
        return tok

    def mm(out, lhsT, rhs, start=True, stop=True, extra_reads=()):
        P.op("pe", lambda e: e.matmul(out.ap, lhsT=lhsT.ap, rhs=rhs.ap, start=start, stop=stop),
             reads=[lhsT, rhs] + list(extra_reads), writes=[out])

    def transp(out, in_, ident):
        P.op("pe", lambda e: e.transpose(out=out.ap, in_=in_.ap, identity=ident.ap), reads=[in_, ident], writes=[out])

    def act(out, in_, func, bias=None, scale=None, eng="act", extra_reads=()):
        kw = {}
        rd = [in_] + list(extra_reads)
        if bias is not None:
            if isinstance(bias, T):
                kw["bias"] = bias.ap
                rd.append(bias)
            else:
                kw["bias"] = bias
        if scale is not None:
            if isinstance(scale, T):
                kw["scale"] = scale.ap
                rd.append(scale)
            else:
                kw["scale"] = scale
        P.op(eng, lambda e: e.activation(out=out.ap, in_=in_.ap, func=func, **kw), reads=rd, writes=[out])

    def tt(out, a, b, op, eng="dve"):
        P.op(eng, lambda e: e.tensor_tensor(out=out.ap, in0=a.ap, in1=b.ap, op=op), reads=[a, b], writes=[out])

    def ts(out, a, s1, op0, s2=None, op1=None, eng="dve"):
        rd = [a]
        v1 = s1
        v2 = s2
        if isinstance(s1, T):
            rd.append(s1)
            v1 = s1.ap
        if isinstance(s2, T):
            rd.append(s2)
            v2 = s2.ap
        if op1 is None:
            P.op(eng, lambda e: e.tensor_scalar(out=out.ap, in0=a.ap, scalar1=v1, scalar2=None, op0=op0),
                 reads=rd, writes=[out])
        else:
            P.op(eng, lambda e: e.tensor_scalar(out=out.ap, in0=a.ap, scalar1=v1, scalar2=v2, op0=op0, op1=op1),
                 reads=rd, writes=[out])

    def stt(out, a, s, b, op0, op1, eng="dve"):
        rd = [a, b]
        v = s
        if isinstance(s, T):
            rd.append(s)
            v = s.ap
        P.op(eng, lambda e: e.scalar_tensor_tensor(out=out.ap, in0=a.ap, scalar=v, in1=b.ap, op0=op0, op1=op1),
             reads=rd, writes=[out])

    def cp(out, in_, eng="dve"):
        if eng == "act":
            P.op("act", lambda e: e.copy(out=out.ap, in_=in_.ap), reads=[in_], writes=[out])
        else:
            P.op(eng, lambda e: e.tensor_copy(out=out.ap, in_=in_.ap), reads=[in_], writes=[out])

    def memset(out, val, eng="pool"):
        P.op(eng, lambda e: e.memset(out.ap, val), writes=[out])

    def recip(out, in_):
        P.op("dve", lambda e: e.reciprocal(out=out.ap, in_=in_.ap), reads=[in_], writes=[out])

    def scan_cumsum(out, ones, in_):
        P.op("dve", lambda e: e.tensor_tensor_scan(out=out.ap, data0=ones.ap, data1=in_.ap, initial=0.0,
                                                   op0=ALU.mult, op1=ALU.add), reads=[ones, in_], writes=[out])

    def dbg_out(name, src, shape, dt=BF16):
        if name not in dbg:
            return
        d = dram("dbg_" + name, shape, dt, kind="ExternalOutput")
        dbg_outs[name] = d
        fin.append(dma_out(d, src))

    fin = []

    banks = [T(ctx.enter_context(nc.psum_tensor("pb%d" % i, [128, 512], F32))[:]) for i in range(8)]
    rr = {"b": 0, "x": 0}
    XB = (5, 6, 7)
    excl = {"on": False}
    SKEW = 2

    def pbank():
        while True:
            rr["b"] = (rr["b"] + 1) % 8
            if excl["on"] and rr["b"] in XB:
                continue
            return banks[rr["b"]]

    def xbank():
        rr["x"] = (rr["x"] + 1) % len(XB)
        return banks[XB[rr["x"]]]

    def pquart():
        return pbank()[:, 0:128]

    def run_pipeline(units, stages, skew):
        n = len(units)
        K = len(stages)
        for step in range(n + (K - 1) * skew):
            for s in reversed(range(K)):
                ui = step - s * skew
                if 0 <= ui < n:
                    stages[s](units[ui])

    consts_f = sb([128, NCONST], F32, "consts_f")
    consts_b = sb([128, 640], BF16, "consts_b")
    dma_in(consts_f, d_consts)
    cp(consts_b, consts_f[:, 0:640])
    ident_f = consts_f[:, C_IDENT:C_IDENT + 128]
    U_f = consts_f[:, C_U:C_U + 128]
    L_f = consts_f[:, C_L:C_L + 128]
    psw_b = consts_b[:, C_PSW:C_PSW + 128]
    ones_b = consts_b[:, C_ONES:C_ONES + 128]
    ones_f = consts_f[:, C_ONES:C_ONES + 128]
    U_b = consts_b[:, C_U:C_U + 128]
    L_b = consts_b[:, C_L:C_L + 128]
    invf = consts_f[:, C_VEC + 0:C_VEC + 1]
    sgn = consts_f[:, C_VEC + 1:C_VEC + 2]
    m1 = consts_f[:, C_VEC + 2:C_VEC + 3]
    m2 = consts_f[:, C_VEC + 3:C_VEC + 4]
    epsc = {1024: consts_f[:, C_VEC + 4:C_VEC + 5], 128: consts_f[:, C_VEC + 5:C_VEC + 6], 192: consts_f[:, C_VEC + 6:C_VEC + 7]}
    one_c = consts_f[:, C_VEC + 7:C_VEC + 8]

    def rstd_from(out, v, n):
        act(out, v, AF.Ln, bias=epsc[n][0:out.ap.shape[0], :])
        act(out, out, AF.Exp, scale=-0.5)
    mnegF = sb([128, 128], F32, "mnegF")
    mnegB = sb([128, 128], F32, "mnegB")
    ts(mnegF, U_f, 1.0, ALU.subtract, -NEG, ALU.mult)
    ts(mnegB, L_f, 1.0, ALU.subtract, -NEG, ALU.mult)

    ng = sb([128, NL * 8], F32, "ng")
    dma_in(ng, d_ng)
    ng32 = sb([128, NL * 8], F32, "ng32")
    ts(ng32, ng, 32.0, ALU.mult)
    ang = sb([128, NL * 4], F32, "angs")
    dma_in(ang, d_ang)
    lbl = sb([128, 2 * 6 * DEPTH_ALL], F32, "lbl")
    dma_in(lbl, d_lb)
    hng = sb([128, NL * 6], F32, "hngs")
    dma_in(hng, d_hng)
    cwA = sb([128, NL * 20], F32, "cwA")
    dma_in(cwA, d_cwA)
    cwB = sb([64, NL * 20], F32, "cwB")
    dma_in(cwB, d_cwB)
    mvA = sb([128, NL * 12], F32, "mvA")
    dma_in(mvA, d_mvA)
    mvB = sb([64, NL * 12], F32, "mvB")
    dma_in(mvB, d_mvB)
    bgs = sb([128, NL * 16], F32, "bgs")
    dma_in(bgs, d_bg[0:1, :].to_broadcast([128, NL * 16]))

    neglam = sb([128, NL], F32, "neglam")
    gsa = sb([128, NL * 4], F32, "gsa")
    lamtmp = sb([128, 64], F32, "lamtmp")
    lam2 = sb([128, 4], F32, "lam2")
    alam_t = sb([128, 256], F32, "alam")
    for l in range(NL):
        dma_in(alam_t, d_alam[0:1, l * 256:(l + 1) * 256].to_broadcast([128, 256]))
        for j in range(2):
            tt(lamtmp, alam_t[:, j * 128:j * 128 + 64],
               alam_t[:, j * 128 + 64:j * 128 + 128], ALU.mult)
            P.op("dve", (lambda o, i: (lambda e: e.reduce_sum(out=o.ap, in_=i.ap, axis=AX.X)))(lam2[:, j:j + 1], lamtmp),
                 reads=[lamtmp], writes=[lam2])
        act(lam2[:, 2:4], lam2[:, 0:2], AF.Exp)
        tt(lam2[:, 0:1], lam2[:, 3:4], lam2[:, 2:3], ALU.subtract)
        ts(neglam[:, l:l + 1], lam2[:, 0:1], -float(lam_inits[l]), ALU.add)
        ts(gsa[:, l * 4:(l + 1) * 4], ang[:, l * 4:(l + 1) * 4], float((1.0 - lam_inits[l]) * math.sqrt(128.0)), ALU.mult)
    NLB = 12 * DEPTH_ALL
    lbe = sb([128, NLB], F32, "lbe")
    act(lbe, lbl, AF.Exp)
    lbs = sb([128, 12], F32, "lbs")
    P.op("dve", lambda e: e.reduce_sum(out=lbs.ap, in_=lbe.ap.rearrange("p (a l) -> p a l", l=DEPTH_ALL), axis=AX.X),
         reads=[lbe], writes=[lbs])
    lbr = sb([128, 12], F32, "lbr")
    recip(lbr, lbs)
    lbv = sb([128, NL * 12], F32, "lbv")
    omlb = sb([128, NL * 12], F32, "omlb")
    lbt = sb([128, 12], F32, "lbt")

    def lb_for_layer(l, lglob):
        dst = lbv[:, l * 12:(l + 1) * 12]
        if lglob == 0:
            memset(dst, 0.0, eng="dve")
        else:
            P.op("dve", lambda e: e.reduce_sum(
                out=lbt.ap, in_=lbe.ap.rearrange("p (a l) -> p a l", l=DEPTH_ALL)[:, :, 1:lglob + 1], axis=AX.X),
                reads=[lbe], writes=[lbt])
            tt(dst, lbt, lbr, ALU.mult)
        ts(omlb[:, l * 12:(l + 1) * 12], dst, -1.0, ALU.mult, 1.0, ALU.add)

    hgs = sb([128, NL * 6], F32, "hgs")
    ts(hgs, hng, float(math.sqrt(128.0)), ALU.mult)
    mgsA = sb([128, NL * 4], F32, "mgsA")
    mgsB = sb([64, NL * 4], F32, "mgsB")

    xdr = [[T(None) for t in range(NT)] for b in range(NSEQ)]
    xsrc = {}
    hT = [sb([128, S], BF16, "hT%d" % k) for k in range(8)]
    hTg = [[hT[k].sub((slice(None), slice(g * 512, (g + 1) * 512))) for g in range(NG)] for k in range(8)]
    W_bf = [sb([128, S], BF16, "wbf%d" % i) for i in range(10)]
    W_f32 = [sb([128, max(S + 4, 1024)], F32, "wf%d" % i) for i in range(3)]
    xin = [W_f32[0][:, 0:1024], W_f32[1][:, 0:1024]]
    fgb = W_f32[2][:, 0:1024]
    xt_tiles = [sb([128, 1024], F32, "xtile%d" % i) for i in range(2)]
    rrx = {"i": 0}
    ssq = sb([128, 4], F32, "ssq")
    tmpf = [sb([128, 512], F32, "tmpf%d" % i) for i in range(4)]
    tmpb = [sb([128, 512], BF16, "tmpb%d" % i) for i in range(5)]
    rrt = {"f": 0, "b": 0}

    def tf():
        rrt["f"] = (rrt["f"] + 1) % len(tmpf)
        return tmpf[rrt["f"]]

    def tb():
        rrt["b"] = (rrt["b"] + 1) % len(tmpb)
        return tmpb[rrt["b"]]

    NWB = 3
    wbf = [sb([128, 8, 128], BF16, "wbf16_%d" % i) for i in range(NWB)]
    rrw = {"s": 0, "b": 0, "os": 0, "ob": 0}
    wo_bf = [sb([128, 1024], BF16, "wobf%d" % i) for i in range(4)]

    def load_win(l, col0, ncols):
        rrw["b"] = (rrw["b"] + 1) % NWB
        wb = wbf[rrw["b"]]
        src_ = d_win[l, :, col0:col0 + ncols].rearrange("(k p) c -> p k c", p=128)
        dma_in(wb[:, :, 0:ncols], src_, eng="pool")
        return wb

    def load_wout(l, row0, nrows):
        rrw["ob"] = (rrw["ob"] + 1) % 4
        wb = wo_bf[rrw["ob"]]
        dma_in(wb[0:nrows, :], d_wout[l, row0:row0 + nrows, :], eng="pool")
        return wb

    def proj_fm(ps, wb, c0, ncols, g):
        for k in range(8):
            mm(ps[0:ncols, :], wb[:, k, c0:c0 + ncols], hTg[k][g], start=(k == 0), stop=(k == 7))

    def proj_tm(ps_view, wb, c0, ncols, tt_):
        g = tt_ // 4
        o = (tt_ % 4) * 128
        for k in range(8):
            mm(ps_view, hTg[k][g][:, o:o + 128], wb[:, k, c0:c0 + ncols], start=(k == 0), stop=(k == 7))

    def sumsq_rstd(rstd_out, srcs, n, g_cols):
        ps = pbank()
        for i, (s, kk) in enumerate(srcs):
            sq = tb()
            act(sq[0:kk, 0:g_cols], s, AF.Square)
            mm(ps[:, 0:g_cols], ones_b[0:kk, :], sq[0:kk, 0:g_cols], start=(i == 0), stop=(i == len(srcs) - 1))
        rstd_from(rstd_out, ps[:, 0:g_cols], n)

    def out_proj(l, ysrcs):
        b = cur["b"]
        wts = [load_wout(l, r0, kk) for (_, kk, r0) in ysrcs]
        def ld(t):
            xt_ = xt_tiles[t % 2]
            P.op("sp", (lambda dst, s_: (lambda e: e.dma_start(out=dst.ap, in_=s_)))(xt_, xsrc[b][t]),
                 reads=[xdr[b][t]], writes=[xt_], is_dma=True, dkey=xt_.buf.id)

        ld(0)
        for t in range(NT):
            g = t // 4
            o = (t % 4) * 128
            xt = xt_tiles[t % 2]
            if t + 1 < NT:
                ld(t + 1)
            for hf in range(2):
                ps = pbank()
                for i, (yf, kk, r0) in enumerate(ysrcs):
                    mm(ps, yf(g)[:, o:o + 128], wts[i][0:kk, hf * 512:(hf + 1) * 512], start=(i == 0), stop=(i == len(ysrcs) - 1))
                tt(xt[:, hf * 512:(hf + 1) * 512], xt[:, hf * 512:(hf + 1) * 512], ps, ALU.add)
            P.op("sp", (lambda src_, d_: (lambda e: e.dma_start(out=d_, in_=src_.ap)))(xt, d_out[b, t * 128:(t + 1) * 128, :]),
                 reads=[xt], writes=[xdr[b][t]], is_dma=True, dkey="o%d" % xt.buf.id)
            xsrc[b][t] = d_out[b, t * 128:(t + 1) * 128, :]

    def attention_group(l, ctab, stab):
        qt = W_bf[0]
        k1 = W_bf[1]
        k2 = W_bf[2]
        vtok = W_bf[3]
        sz = W_bf[4]
        ys = [W_bf[5], W_bf[6], W_bf[7], W_bf[8]]
        vt3 = T(vtok.ap.rearrange("p (t c) -> p t c", c=128), vtok.buf)
        for hd in range(4):
            wq = load_win(l, OFF["aq"] + hd * 128, 128)
            for g in range(NG):
                gs = slice(g * 512, (g + 1) * 512)
                ps = pbank()
                proj_fm(ps, wq, 0, 128, g)
                a_bf = tb()
                cp(a_bf, ps, eng="act")
                ps2 = pbank()
                mm(ps2, psw_b, a_bf)
                t1 = tf()
                tt(t1, ps2, stab[:, gs], ALU.mult)
                t2 = tf()
                tt(t2, ps, ctab[:, gs], ALU.mult)
                tt(qt[:, gs], t1, t2, ALU.add)
            wk = load_win(l, OFF["ak"] + hd * 128, 128)
            for g in range(NG):
                gs = slice(g * 512, (g + 1) * 512)
                ps = pbank()
                proj_fm(ps, wk, 0, 128, g)
                a_bf = tb()
                cp(a_bf, ps, eng="act")
                ps2 = pbank()
                mm(ps2, psw_b, a_bf)
                t1 = tf()
                tt(t1, ps2, stab[:, gs], ALU.mult)
                t2 = tf()
                tt(t2, ps, ctab[:, gs], ALU.mult)
                ktmp = tb()
                tt(ktmp, t1, t2, ALU.add)
                ts(k1[:, gs], ktmp, m1, ALU.mult)
                ts(k2[:, gs], ktmp, m2, ALU.mult)
            wv = load_win(l, OFF["av"] + hd * 128, 128)
            for g in range(NG):
                ps = pbank()
                for j in range(4):
                    proj_tm(ps[:, j * 128:(j + 1) * 128], wv, 0, 128, g * 4 + j)
                cp(T(vtok.ap[:, g * 512:(g + 1) * 512], vtok.buf), ps, eng="act")
            wz = load_win(l, OFF["az"] + hd * 128, 128)
            for g in range(NG):
                ps = pbank()
                proj_fm(ps, wz, 0, 128, g)
                act(sz[:, g * 512:(g + 1) * 512], ps, AF.Silu)
            kk = [k1, k2]
            for g in range(NG):
                gs = slice(g * 512, (g + 1) * 512)
                num = [banks[0], banks[1]]
                den = [banks[2], banks[3]]
                sc_banks = [banks[4], banks[5], banks[6]]
                iters = [(kt, c) for kt in range(NT) for c in range(2)]

                def score(i, gs=gs):
                    kt, c = iters[i]
                    mm(sc_banks[i % 3], kk[c][:, kt * 128:(kt + 1) * 128], qt[:, gs])

                score(0)
                for i in range(len(iters)):
                    if i + 1 < len(iters):
                        score(i + 1)
                    kt, c = iters[i]
                    pt = tb()
                    act(pt, sc_banks[i % 3], AF.Exp, scale=0.125)
                    mm(num[c], vt3[:, kt, :], pt, start=(kt == 0), stop=(kt == NT - 1))
                    mm(den[c], ones_b, pt, start=(kt == 0), stop=(kt == NT - 1))
                r1 = tf()
                recip(r1, den[0])
                r2 = tf()
                recip(r2, den[1])
                o1 = tf()
                tt(o1, num[0], r1, ALU.mult)
                o2 = tf()
                tt(o2, num[1], r2, ALU.mult)
                o = tf()
                stt(o, o2, neglam[:, l:l + 1], o1, ALU.mult, ALU.add)
                rstd = tf()
                sumsq_rstd(rstd, [(o, 128)], 128, 512)
                y1 = o1
                tt(y1, o, rstd, ALU.mult)
                stt(ys[hd][:, gs], y1, gsa[:, l * 4 + hd:l * 4 + hd + 1], sz[:, gs], ALU.mult, ALU.mult)
        dbg_out("ya", ys[0], [128, S])
        out_proj(l, [((lambda g, hd=hd: ys[hd][:, g * 512:(g + 1) * 512]), 128, Y_OFF["a"] + hd * 128) for hd in range(4)])

    def hgrn_group(l):
        qT_ = W_bf[0]
        kT_ = W_bf[1]
        vtok = W_bf[2]
        vt3 = T(vtok.ap.rearrange("p (t c) -> p t c", c=128), vtok.buf)
        sz = W_bf[3]
        ys = [W_bf[4], W_bf[5], W_bf[6]]
        a_pad = W_f32[0]
        na_pad = W_f32[1]
        gtmp = W_f32[1]
        oT = W_f32[2]
        NS0 = 2
        NSXH = 4
        if not hasattr(hgrn_group, "_t"):
            tl_ = dict()
            tl_["ek"] = [[sb([128, 128], BF16, "hek%d_%d" % (s_, i)) for i in range(4)] for s_ in range(NS0)]
            for s_ in range(NS0):
                for i in range(4):
                    memset(tl_["ek"][s_][i], 0.0)
            tl_["eq"] = [sb([128, 128], F32, "heq%d" % i) for i in range(NS0)]
            tl_["ekf"] = [sb([128, 448], F32, "hekf%d" % i) for i in range(NS0)]
            tl_["ekr"] = [{32: t_e.sub((slice(None), slice(0, 32))), 64: t_e.sub((slice(None), slice(32, 96))),
                           96: t_e.sub((slice(None), slice(96, 192))), 128: t_e.sub((slice(None), slice(192, 320))),
                           "kend": t_e.sub((slice(None), slice(320, 448)))} for t_e in tl_["ekf"]]
            tl_["eq2"] = [sb([128, 128], F32, "heq2_%d" % i) for i in range(NS0)]
            tl_["qtl"] = [sb([128, 128], BF16, "hqtl%d" % i) for i in range(NS0)]
            tl_["kend"] = [sb([128, 128], F32, "hkend%d" % i) for i in range(NS0)]
            tl_["kendT"] = [sb([128, 128], BF16, "hkendT%d" % i) for i in range(NS0)]
            tl_["qc"] = [sb([128, 128], BF16, "hqc%d" % i) for i in range(NSXH)]
            tl_["wT"] = [sb([128, 128], BF16, "hwT%d" % i) for i in range(NSXH)]
            tl_["dec"] = sb([128, NSXH], F32, "hdec")
            tl_["Sst"] = sb([128, 128], F32, "hSst")
            tl_["Sbf"] = sb([128, 128], BF16, "hSbf")
            hgrn_group._t = tl_
        tl = hgrn_group._t
        cnt = {"u": 0}
        for hd in range(6):
            wq = load_win(l, OFF["hq"] + hd * 128, 128)
            for g in range(NG):
                ps = pbank()
                proj_fm(ps, wq, 0, 128, g)
                cp(qT_[:, g * 512:(g + 1) * 512], ps, eng="act")
            wv = load_win(l, OFF["hi"] + hd * 128, 128)
            for g in range(NG):
                ps = pbank()
                for j in range(4):
                    proj_tm(ps[:, j * 128:(j + 1) * 128], wv, 0, 128, g * 4 + j)
                cp(T(vtok.ap[:, g * 512:(g + 1) * 512], vtok.buf), ps, eng="act")
            wz = load_win(l, OFF["hz"] + hd * 128, 128)
            for g in range(NG):
                ps = pbank()
                proj_fm(ps, wz, 0, 128, g)
                act(sz[:, g * 512:(g + 1) * 512], ps, AF.Silu)
            for dr in range(2):
                wf = load_win(l, OFF["hff" if dr == 0 else "hfb"] + hd * 128, 128)
                lbc = lbv[:, l * 12 + dr * 6 + hd:l * 12 + dr * 6 + hd + 1]
                olbc = omlb[:, l * 12 + dr * 6 + hd:l * 12 + dr * 6 + hd + 1]
                for g in range(NG):
                    gs = slice(g * 512, (g + 1) * 512)
                    ps = pbank()
                    proj_fm(ps, wf, 0, 128, g)
                    sg = tf()
                    act(sg, ps, AF.Sigmoid)
                    ff = tf()
                    ts(ff, sg, olbc, ALU.mult, lbc, ALU.add)
                    act(gtmp[:, gs], ff, AF.Ln)
                    ts(kT_[:, gs], ff, -1.0, ALU.mult, 1.0, ALU.add)
                memset(a_pad[:, 0:1], 0.0, eng="dve")
                scan_cumsum(a_pad[:, 1:S + 1], T(ones_f.ap[:, 0:1].to_broadcast([128, S]), ones_f.buf), gtmp[:, 0:S])
                ts(na_pad[:, 0:S + 1], a_pad[:, 0:S + 1], -1.0, ALU.mult)
                order = list(range(NT)) if dr == 0 else list(range(NT - 1, -1, -1))
                units = []
                for i_, c in enumerate(order):
                    units.append(dict(c=c, first=(i_ == 0), u=cnt["u"]))
                    cnt["u"] += 1

                def st0(un, dr=dr):
                    c, u = un["c"], un["u"]
                    c0 = c * 128
                    s0 = u % NS0
                    sx = u % NSXH
                    eq, ekr, qtl, kend, kendT = tl["eq"][s0], tl["ekr"][s0], tl["qtl"][s0], tl["kend"][s0], tl["kendT"][s0]
                    eq2 = tl["eq2"][s0]
                    ek = tl["ek"][s0]
                    qc, wT = tl["qc"][sx], tl["wT"][sx]
                    dcol = tl["dec"][:, sx:sx + 1]
                    if dr == 0:
                        for I in range(4):
                            act(eq[:, 32 * I:32 * I + 32], a_pad[:, 1 + c0 + 32 * I:1 + c0 + 32 * I + 32], AF.Exp,
                                bias=na_pad[:, c0 + 32 * I:c0 + 32 * I + 1])
                        tt(qtl, qT_[:, c0:c0 + 128], eq, ALU.mult)
                        for I in range(4):
                            w_ = 32 * (I + 1)
                            act(ekr[w_], na_pad[:, 1 + c0:1 + c0 + w_], AF.Exp, bias=a_pad[:, c0 + 32 * I:c0 + 32 * I + 1])
                            tt(ek[I][:, 0:w_], kT_[:, c0:c0 + w_], ekr[w_], ALU.mult)
                        act(eq2, a_pad[:, 1 + c0:1 + c0 + 128], AF.Exp, bias=na_pad[:, c0:c0 + 1])
                        tt(qc, qT_[:, c0:c0 + 128], eq2, ALU.mult)
                        act(ekr["kend"], na_pad[:, 1 + c0:1 + c0 + 128], AF.Exp, bias=a_pad[:, c0 + 128:c0 + 129])
                        tt(kend, kT_[:, c0:c0 + 128], ekr["kend"], ALU.mult)
                        mask = U_f
                    else:
                        for I in range(4):
                            act(eq[:, 32 * I:32 * I + 32], na_pad[:, c0 + 32 * I:c0 + 32 * I + 32], AF.Exp,
                                bias=a_pad[:, c0 + 32 * (I + 1):c0 + 32 * (I + 1) + 1])
                        tt(qtl, qT_[:, c0:c0 + 128], eq, ALU.mult)
                        for I in range(4):
                            lo = 32 * I
                            act(ekr[128 - lo], a_pad[:, c0 + lo:c0 + 128], AF.Exp,
                                bias=na_pad[:, c0 + 32 * (I + 1):c0 + 32 * (I + 1) + 1])
                            tt(ek[I][:, lo:128], kT_[:, c0 + lo:c0 + 128], ekr[128 - lo], ALU.mult)
                        act(eq2, na_pad[:, c0:c0 + 128], AF.Exp, bias=a_pad[:, c0 + 128:c0 + 129])
                        tt(qc, qT_[:, c0:c0 + 128], eq2, ALU.mult)
                        act(ekr["kend"], a_pad[:, c0:c0 + 128], AF.Exp, bias=na_pad[:, c0:c0 + 1])
                        tt(kend, kT_[:, c0:c0 + 128], ekr["kend"], ALU.mult)
                        mask = L_f
                    act(dcol, a_pad[:, c0 + 128:c0 + 129], AF.Exp, bias=na_pad[:, c0:c0 + 1])
                    pb = pbank()
                    pss = pb[:, 0:128]
                    pst = pb[:, 128:256]
                    for I in range(4):
                        mm(pss[:, 32 * I:32 * I + 32], ek[I], qtl[:, 32 * I:32 * I + 32])
                    tt(wT, pss, mask, ALU.mult)
                    transp(pst, kend, ident_f)
                    cp(kendT, pst, eng="act")
                    psd = xbank()[:, 0:128]
                    mm(psd, kendT, vt3[:, c, :])
                    un.update(qc=qc, wT=wT, dcol=dcol, psd=psd)

                def st1(un, dr=dr):
                    c, first = un["c"], un["first"]
                    c0 = c * 128
                    qc, wT, dcol, psd = un["qc"], un["wT"], un["dcol"], un["psd"]
                    pso = pquart()
                    mm(pso, vt3[:, c, :], wT, start=True, stop=first)
                    if not first:
                        mm(pso, tl["Sbf"], qc, start=False, stop=True)
                    if first:
                        cp(tl["Sst"], psd)
                    else:
                        stt(tl["Sst"], tl["Sst"], dcol, psd, ALU.mult, ALU.add)
                    cp(tl["Sbf"], tl["Sst"], eng="act")
                    if dr == 0:
                        cp(oT[:, c0:c0 + 128], pso, eng="act")
                    else:
                        tt(oT[:, c0:c0 + 128], oT[:, c0:c0 + 128], pso, ALU.add)

                run_pipeline(units, [st0, st1], SKEW)
            yh = ys[hd % 3]
            for g in range(NG):
                gs = slice(g * 512, (g + 1) * 512)
                rstd = tf()
                sumsq_rstd(rstd, [(oT[:, gs], 128)], 128, 512)
                y1 = tf()
                tt(y1, oT[:, gs], rstd, ALU.mult)
                stt(yh[:, gs], y1, hgs[:, l * 6 + hd:l * 6 + hd + 1], sz[:, gs], ALU.mult, ALU.mult)
            if hd == 0:
                dbg_out("yh", yh, [128, S])
            if hd % 3 == 2:
                h0 = hd - 2
                out_proj(l, [((lambda g, j=j: ys[j][:, g * 512:(g + 1) * 512]), 128, Y_OFF["h"] + (h0 + j) * 128) for j in range(3)])

    def mlstm_group(l):
        xmA = W_f32[0]
        xmB = W_f32[1]
        cacc = W_f32[2]
        xcA = W_bf[0]
        xcB = W_bf[1]
        mqA, mqB, mkA, mkB = W_bf[2], W_bf[3], W_bf[4], W_bf[5]
        hfA, hfB = W_bf[6], W_bf[7]
        yA, yB = W_bf[8], W_bf[9]
        xmbA = sb([128, S], BF16, "xmbA") if not hasattr(mlstm_group, "_t") else mlstm_group._t["xmbA"]
        if not hasattr(mlstm_group, "_t"):
            t_ = dict(xmbA=xmbA)
            t_["xmbB"] = sb([128, S], BF16, "xmbB")
            t_["vtok"] = sb([128, NT, 200], BF16, "mvtok")
            t_["ktok"] = sb([128, NT, 192], BF16, "mktok")
            t_["gates"] = sb([128, NT, 16], F32, "mgates")
            t_["lf"] = sb([128, NT, 8], F32, "mlf")
            t_["eib"] = sb([128, NT, 8], F32, "meib")
            t_["bd"] = sb([128, 6 * 128], F32, "mbd")
            t_["bdB"] = sb([64, 6 * 64], F32, "mbdB")
            t_["bdb"] = sb([128, 3 * 128], BF16, "mbdb")
            t_["bdbB"] = sb([64, 3 * 64], BF16, "mbdbB")
            t_["wg"] = sb([128, 4 * 3 * 16], F32, "mwg")
            t_["wgB"] = sb([64, 4 * 3 * 16], F32, "mwgB")
            t_["G"] = sb([128, 4 * 2 * 16], BF16, "mG")
            t_["GB"] = sb([64, 4 * 2 * 16], BF16, "mGB")
            t_["lfrep"] = [sb([128, 128], F32, "mlfrep%d" % i) for i in range(2)]
            t_["Eb"] = [sb([128, 128], F32, "mEb%d" % i) for i in range(2)]
            t_["EbM"] = [sb([128, 128], F32, "mEbM%d" % i) for i in range(2)]
            t_["wT"] = [sb([128, 128], BF16, "mwT%d" % i) for i in range(2)]
            t_["qsA"] = [sb([128, 128], BF16, "mqsA%d" % i) for i in range(2)]
            t_["qsB"] = [sb([64, 128], BF16, "mqsB%d" % i) for i in range(2)]
            t_["dm"] = [sb([128, 128], F32, "mdm%d" % i) for i in range(2)]
            t_["rd"] = [sb([128, 128], F32, "mrd%d" % i) for i in range(2)]
            t_["CA"] = sb([128, 200], F32, "mCA")
            t_["CB"] = sb([64, 200], F32, "mCB")
            t_["CtA"] = sb([128, 200], F32, "mCtA")
            t_["CtB"] = sb([64, 200], F32, "mCtB")
            t_["CbA"] = sb([128, 200], BF16, "mCbA")
            t_["CbB"] = sb([64, 200], BF16, "mCbB")
            t_["nrA"] = sb([128, 128], BF16, "mnrA")
            t_["nrB"] = sb([64, 128], BF16, "mnrB")
            t_["eibrep"] = [sb([128, 128], BF16, "meibrep%d" % i) for i in range(2)]
            t_["hbA"] = sb([128, 512], F32, "mhbA")
            t_["hbB"] = sb([64, 512], F32, "mhbB")
            t_["so"] = [sb([128, 512], BF16, "mso%d" % i) for i in range(4)]
            mlstm_group._t = t_
        t_ = mlstm_group._t
        xmbB = t_["xmbB"]
        vtok, ktok, gates, lf, eib = t_["vtok"], t_["ktok"], t_["gates"], t_["lf"], t_["eib"]
        ts(mgsA[:, l * 4:(l + 1) * 4], mvA[:, l * 12 + 8:l * 12 + 12], float(math.sqrt(192.0)), ALU.mult)
        ts(mgsB[:, l * 4:(l + 1) * 4], mvB[:, l * 12 + 8:l * 12 + 12], float(math.sqrt(192.0)), ALU.mult)
        QSCALE = float(192.0 ** -0.5)

        def compute_xm_xc(j):
            for (xm_, xmb_, xc_, kk, coff, cw_, mv_) in ((xmA, xmbA, xcA, 128, 0, cwA, mvA), (xmB, xmbB, xcB, 64, 128, cwB, mvB)):
                wx = load_win(l, OFF["xm"] + j * 192 + coff, kk)
                memset(xm_[0:kk, 0:2], 0.0, eng="dve")
                memset(xm_[0:kk, S + 2:S + 4], 0.0, eng="dve")
                for g in range(NG):
                    ps = pbank()
                    proj_fm(ps, wx, 0, kk, g)
                    cp(xm_[0:kk, 2 + g * 512:2 + (g + 1) * 512], ps[0:kk, :], eng="act")
                    cp(xmb_[0:kk, g * 512:(g + 1) * 512], ps[0:kk, :], eng="act")
                cb = l * 20 + j * 5
                ts(cacc[0:kk, 0:S], xm_[0:kk, 0:S], cw_[0:kk, cb:cb + 1], ALU.mult)
                for k in range(1, 5):
                    stt(cacc[0:kk, 0:S], xm_[0:kk, k:k + S], cw_[0:kk, cb + k:cb + k + 1], cacc[0:kk, 0:S],
                        ALU.mult, ALU.add)
                act(xc_[0:kk, 0:S], cacc[0:kk, 0:S], AF.Silu, bias=mv_[0:kk, l * 12 + j:l * 12 + j + 1])

        dma_in(t_["wg"], d_wgA[l])
        dma_in(t_["wgB"], d_wgB[l])
        psg = banks[0]
        psg3 = T(psg.ap[:, 0:NT * 16].rearrange("p (t c) -> p t c", c=16), psg.buf)
        for j in range(4):
            dma_in(t_["bd"], d_bdA[l, j])
            dma_in(t_["bdB"], d_bdB[l, j])
            for (bd_, wg_, G_, kk) in ((t_["bd"], t_["wg"], t_["G"], 128), (t_["bdB"], t_["wgB"], t_["GB"], 64)):
                pq = pquart()
                mm(pq[0:kk, 0:16], bd_[0:kk, 3 * kk:4 * kk], wg_[0:kk, (j * 3 + 0) * 16:(j * 3 + 1) * 16], start=True, stop=False)
                mm(pq[0:kk, 0:16], bd_[0:kk, 4 * kk:5 * kk], wg_[0:kk, (j * 3 + 1) * 16:(j * 3 + 2) * 16], start=False, stop=True)
                mm(pq[0:kk, 16:32], bd_[0:kk, 5 * kk:6 * kk], wg_[0:kk, (j * 3 + 2) * 16:(j * 3 + 3) * 16], start=True, stop=True)
                cp(G_[0:kk, j * 32:(j + 1) * 32], pq[0:kk, 0:32])
            compute_xm_xc(j)
            for t in range(NT):
                tsl = slice(t * 128, (t + 1) * 128)
                mm(psg3[:, t, :], xcA[:, tsl], t_["G"][:, j * 32:j * 32 + 16], start=True, stop=False)
                mm(psg3[:, t, :], xmbA[:, tsl], t_["G"][:, j * 32 + 16:j * 32 + 32], start=False, stop=False)
                mm(psg3[:, t, :], xcB[0:64, tsl], t_["GB"][0:64, j * 32:j * 32 + 16], start=False, stop=False)
                mm(psg3[:, t, :], xmbB[0:64, tsl], t_["GB"][0:64, j * 32 + 16:j * 32 + 32], start=False, stop=True)
            if j == 0:
                for t in range(NT):
                    tt(gates[:, t, :], psg3[:, t, :], bgs[:, l * 16:(l + 1) * 16], ALU.add)
            else:
                tt(gates, gates, psg3, ALU.add)
        etmp = sb([128, NT, 8], F32, "metmp") if "etmp" not in t_ else t_["etmp"]
        t_["etmp"] = etmp
        act(etmp[:, :, 0:4], gates[:, :, 4:8], AF.Exp, scale=-1.0)
        act(etmp[:, :, 4:8], gates[:, :, 12:16], AF.Exp, scale=-1.0)
        act(lf, etmp, AF.Ln, bias=one_c)
        ts(lf, lf, -1.0, ALU.mult)
        bcs = sb([128, NT, 8], F32, "mbcs") if "bcs" not in t_ else t_["bcs"]
        t_["bcs"] = bcs
        psb = banks[1]
        psb3 = T(psb.ap[:, 0:NT * 8].rearrange("p (t c) -> p t c", c=8), psb.buf)
        for t in range(NT):
            mm(psb3[:, t, 0:4], U_f, lf[:, t, 0:4])
            mm(psb3[:, t, 4:8], L_f, lf[:, t, 4:8])
        cp(bcs, psb3)
        tt(etmp[:, :, 0:4], gates[:, :, 0:4], bcs[:, :, 0:4], ALU.subtract)
        tt(etmp[:, :, 4:8], gates[:, :, 8:12], bcs[:, :, 4:8], ALU.subtract)
        act(eib, etmp, AF.Exp)
        dbg_out("gates", T(gates.ap.rearrange("p t c -> p (t c)"), gates.buf), [128, NT * 16], F32)

        cntu = {"u": 0}
        for j in range(4):
            dma_in(t_["bd"], d_bdA[l, j])
            dma_in(t_["bdB"], d_bdB[l, j])
            cp(t_["bdb"], t_["bd"][:, 0:384], eng="pool")
            cp(t_["bdbB"], t_["bdB"][:, 0:192], eng="pool")
            bdb, bdbB = t_["bdb"], t_["bdbB"]
            compute_xm_xc(j)
            for g in range(NG):
                gs = slice(g * 512, (g + 1) * 512)
                for (dst, src, w_, kk, sc) in ((mqA, xcA, bdb[:, 0:128], 128, QSCALE), (mqB, xcB, bdbB[:, 0:64], 64, QSCALE),
                                               (mkA, xcA, bdb[:, 128:256], 128, 1.0), (mkB, xcB, bdbB[:, 64:128], 64, 1.0)):
                    ps = pbank()
                    mm(ps[0:kk, :], w_[0:kk, :], src[0:kk, gs])
                    act(dst[0:kk, gs], ps[0:kk, :], AF.Copy, scale=sc)
            for t in range(NT):
                tsl = slice(t * 128, (t + 1) * 128)
                ps = pbank()
                mm(ps[:, 0:128], xmbA[:, tsl], bdb[:, 256:384])
                mm(ps[:, 128:192], xmbB[0:64, tsl], bdbB[0:64, 128:192])
                mm(ps[:, 192:320], xcA[:, tsl], bdb[:, 128:256])
                mm(ps[:, 320:384], xcB[0:64, tsl], bdbB[0:64, 64:128])
                cp(vtok[:, t, 0:192], ps[:, 0:192], eng="act")
                cp(ktok[:, t, :], ps[:, 192:384], eng="act")
            memset(vtok[:, :, 192:193], 1.0, eng="dve")
            NSX = 4
            if "vp" not in t_:
                t_["vp"] = [sb([128, 200], BF16, "mvp%d" % i) for i in range(NSX)]
                for nm_, shp_, dt_ in (("Eb", [128, 128], F32), ("wT", [128, 128], BF16), ("qsA", [128, 128], BF16),
                                       ("qsB", [64, 128], BF16), ("eibrep", [128, 128], BF16)):
                    t_[nm_] = t_[nm_] + [sb(shp_, dt_, "mx%s%d" % (nm_, i)) for i in range(2, NSX)]
            for dr in range(2):
                order = list(range(NT)) if dr == 0 else list(range(NT - 1, -1, -1))
                gcol = dr * 4 + j
                tri = U_f if dr == 0 else L_f
                mneg = mnegF if dr == 0 else mnegB
                units = []
                for i_, c in enumerate(order):
                    units.append(dict(c=c, first=(i_ == 0), u=cntu["u"]))
                    cntu["u"] += 1

                def st0(un, dr=dr, gcol=gcol, tri=tri, mneg=mneg):
                    c, u, first = un["c"], un["u"], un["first"]
                    csl = slice(c * 128, (c + 1) * 128)
                    sx = u % NSX
                    Eb, wT, qsA, qsB = t_["Eb"][sx], t_["wT"][sx], t_["qsA"][sx], t_["qsB"][sx]
                    vpc, eibrep = t_["vp"][sx], t_["eibrep"][sx]
                    EbM, lfrep = t_["EbM"][u % 2], t_["lfrep"][u % 2]
                    ts(vpc[:, 0:193], vtok[:, c, 0:193], eib[:, c, gcol:gcol + 1], ALU.mult)
                    act(eibrep, ones_f, AF.Copy, scale=eib[:, c, gcol:gcol + 1])
                    bk1 = pbank()
                    pss = bk1[:, 0:128]
                    psl = bk1[:, 128:256]
                    pslm = bk1[:, 256:384]
                    mm(pss, mkA[:, csl], mqA[:, csl], start=True, stop=False)
                    mm(pss, mkB[0:64, csl], mqB[0:64, csl], start=False, stop=True)
                    act(lfrep, ones_f, AF.Copy, scale=lf[:, c, gcol:gcol + 1])
                    mm(psl, lfrep, tri)
                    mm(pslm, lfrep, tri, start=True, stop=False)
                    mm(pslm, ident_f, mneg, start=False, stop=True)
                    act(Eb, psl, AF.Exp)
                    act(EbM, pslm, AF.Exp)
                    tt(wT, pss, EbM, ALU.mult)
                    if not first:
                        tt(qsA, mqA[:, csl], Eb, ALU.mult)
                        tt(qsB, mqB[0:64, csl], Eb[0:64, :], ALU.mult)
                    bk3 = xbank()
                    pcA = bk3[:, 0:256]
                    pcB = bk3[:, 256:512]
                    mm(pcA[:, 0:193], ktok[:, c, 0:128], vpc[:, 0:193])
                    mm(pcB[0:64, 0:193], ktok[:, c, 128:192], vpc[:, 0:193])
                    un.update(Eb=Eb, wT=wT, qsA=qsA, qsB=qsB, vpc=vpc, eibrep=eibrep, pcA=pcA, pcB=pcB)

                def st1(un, dr=dr):
                    c, u, first = un["c"], un["u"], un["first"]
                    csl = slice(c * 128, (c + 1) * 128)
                    Eb, wT, qsA, qsB = un["Eb"], un["wT"], un["qsA"], un["qsB"]
                    vpc, eibrep, pcA, pcB = un["vpc"], un["eibrep"], un["pcA"], un["pcB"]
                    dm, rd = t_["dm"][u % 2], t_["rd"][u % 2]
                    bk2 = pbank()
                    pnA = bk2[:, 0:128]
                    pnB = bk2[:, 128:256]
                    pdn = bk2[:, 256:384]
                    mm(pnA, vpc[:, 0:128], wT, start=True, stop=first)
                    if not first:
                        mm(pnA, t_["CbA"][:, 0:128], qsA, start=False, stop=False)
                        mm(pnA, t_["CbB"][:, 0:128], qsB, start=False, stop=True)
                    mm(pnB[0:64, :], vpc[:, 128:192], wT, start=True, stop=first)
                    if not first:
                        mm(pnB[0:64, :], t_["CbA"][:, 128:192], qsA, start=False, stop=False)
                        mm(pnB[0:64, :], t_["CbB"][:, 128:192], qsB, start=False, stop=True)
                    mm(pdn, eibrep, wT, start=True, stop=first)
                    if not first:
                        mm(pdn, t_["nrA"], qsA, start=False, stop=False)
                        mm(pdn, t_["nrB"], qsB, start=False, stop=True)
                    ebl = Eb[:, 127:128] if dr == 0 else Eb[:, 0:1]
                    if first:
                        act(t_["CA"][:, 0:193], pcA[:, 0:193], AF.Copy, scale=ebl)
                        act(t_["CB"][:, 0:193], pcB[0:64, 0:193], AF.Copy, scale=ebl[0:64, :])
                        act(t_["CbA"][:, 0:193], pcA[:, 0:193], AF.Copy, scale=ebl)
                        act(t_["CbB"][:, 0:193], pcB[0:64, 0:193], AF.Copy, scale=ebl[0:64, :])
                    else:
                        tt(t_["CtA"][:, 0:193], pcA[:, 0:193], t_["CA"][:, 0:193], ALU.add)
                        tt(t_["CtB"][:, 0:193], pcB[0:64, 0:193], t_["CB"][:, 0:193], ALU.add)
                        act(t_["CA"][:, 0:193], t_["CtA"][:, 0:193], AF.Copy, scale=ebl)
                        act(t_["CB"][:, 0:193], t_["CtB"][:, 0:193], AF.Copy, scale=ebl[0:64, :])
                        act(t_["CbA"][:, 0:193], t_["CtA"][:, 0:193], AF.Copy, scale=ebl)
                        act(t_["CbB"][:, 0:193], t_["CtB"][:, 0:193], AF.Copy, scale=ebl[0:64, :])
                    act(t_["nrA"], ones_f, AF.Copy, scale=t_["CA"][:, 192:193])
                    act(t_["nrB"], ones_f[0:64, :], AF.Copy, scale=t_["CB"][:, 192:193])
                    ts(rd, pdn, -1.0, ALU.mult, 1.0, ALU.max)
                    stt(dm, pdn, 1.0, rd, ALU.max, ALU.max)
                    recip(rd, dm)
                    if dr == 0:
                        tt(hfA[:, csl], pnA, rd, ALU.mult)
                        tt(hfB[0:64, csl], pnB[0:64, :], rd[0:64, :], ALU.mult)
                    else:
                        o4 = (c % 4) * 128
                        tt(t_["hbA"][:, o4:o4 + 128], pnA, rd, ALU.mult)
                        tt(t_["hbB"][0:64, o4:o4 + 128], pnB[0:64, :], rd[0:64, :], ALU.mult)
                    if dr == 1 and c % 4 == 0:
                        g = c // 4
                        gs = slice(g * 512, (g + 1) * 512)
                        hbA, hbB = t_["hbA"], t_["hbB"]
                        tt(hbA, hbA, hfA[:, gs], ALU.add)
                        tt(hbB, hbB, hfB[0:64, gs], ALU.add)
                        so = t_["so"]
                        for (idx, nm, coff, kk, fn) in ((0, "om", 0, 128, AF.Sigmoid), (1, "om", 128, 64, AF.Sigmoid),
                                                        (2, "zm", 0, 128, AF.Silu), (3, "zm", 128, 64, AF.Silu)):
                            wz = load_win(l, OFF[nm] + j * 192 + coff, kk)
                            ps = pbank()
                            proj_fm(ps, wz, 0, kk, g)
                            act(so[idx][0:kk, :], ps[0:kk, :], fn)
                        tt(hbA, hbA, so[0], ALU.mult)
                        tt(hbB, hbB, so[1][0:64, :], ALU.mult)
                        rstd = tf()
                        sumsq_rstd(rstd, [(hbA, 128), (hbB, 64)], 192, 512)
                        tt(hbA, hbA, rstd, ALU.mult)
                        tt(hbB, hbB, rstd[0:64, :], ALU.mult)
                        skA = tf()
                        skB = tf()
                        act(skA, xcA[:, gs], AF.Copy, scale=mvA[:, l * 12 + 4 + j:l * 12 + 5 + j])
                        act(skB[0:64, :], xcB[0:64, gs], AF.Copy, scale=mvB[:, l * 12 + 4 + j:l * 12 + 5 + j])
                        stt(hbA, hbA, mgsA[:, l * 4 + j:l * 4 + j + 1], skA, ALU.mult, ALU.add)
                        stt(hbB, hbB, mgsB[:, l * 4 + j:l * 4 + j + 1], skB[0:64, :], ALU.mult, ALU.add)
                        tt(yA[:, gs], hbA, so[2], ALU.mult)
                        tt(yB[0:64, gs], hbB, so[3][0:64, :], ALU.mult)

                run_pipeline(units, [st0, st1], SKEW)
            if j == 0:
                dbg_out("ym", yA, [128, S])
            out_proj(l, [((lambda g: yA[:, g * 512:(g + 1) * 512]), 128, Y_OFF["m"] + j * 192),
                         ((lambda g: yB[0:64, g * 512:(g + 1) * 512]), 64, Y_OFF["m"] + j * 192 + 128)])

    cur = {"b": 0}
    for l in range(NL):
        lb_for_layer(l, l)

    for b in range(NSEQ):
        cur["b"] = b
        xsrc[b] = [d_x[b, t * 128:(t + 1) * 128, :] for t in range(NT)]
        for l in range(NL):
            for t in range(NT):
                g = t // 4
                o = (t % 4) * 128
                rrx["i"] = (rrx["i"] + 1) % 2
                xt = xt_tiles[rrx["i"]]
                P.op("sp", (lambda dst, s_: (lambda e: e.dma_start(out=dst.ap, in_=s_)))(xt, xsrc[b][t]),
                     reads=[xdr[b][t]], writes=[xt], is_dma=True, dkey=xt.buf.id)
                xi = xin[t % 2]
                P.op("act", (lambda o_, i_, a_: (lambda e: e.activation(out=o_.ap, in_=i_.ap, func=AF.Square, accum_out=a_.ap)))(xi, xt, ssq[:, 0:1]),
                     reads=[xt], writes=[xi, ssq])
                rstd_from(ssq[:, 1:2], ssq[:, 0:1], 1024)
                ts(xi, xt, ssq[:, 1:2], ALU.mult)
                for kq in range(2):
                    ps = pbank()
                    for k4 in range(4):
                        k = kq * 4 + k4
                        transp(ps[:, k4 * 128:(k4 + 1) * 128], xi[:, k * 128:(k + 1) * 128], ident_f)
                    for k4 in range(4):
                        k = kq * 4 + k4
                        ts(hTg[k][g][:, o:o + 128], ps[:, k4 * 128:(k4 + 1) * 128], ng32[:, l * 8 + k:l * 8 + k + 1], ALU.mult)
            if "a" in groups:
                ctab = W_f32[0]
                stab = W_f32[1]
                angt = W_f32[2]
                posi = T(angt.ap[:, 0:S].bitcast(I32), angt.buf)
                dma_in(posi, d_pos[b:b + 1, :].to_broadcast([128, S]))
                cp(angt[:, 0:S], posi)
                ts(angt[:, 0:S], angt[:, 0:S], invf, ALU.mult)
                ts(ctab[:, 0:S], angt[:, 0:S], float(1.0 / (2 * math.pi)), ALU.mult)
                ki = T(stab.ap[:, 0:S].bitcast(I32), stab.buf)
                cp(ki, ctab[:, 0:S])
                cp(ctab[:, 0:S], ki)
                stt(angt[:, 0:S], ctab[:, 0:S], float(-2 * math.pi), angt[:, 0:S], ALU.mult, ALU.add)
                act(stab[:, 0:S], angt[:, 0:S], AF.Sin, scale=0.25)
                tt(stab[:, 0:S], stab[:, 0:S], stab[:, 0:S], ALU.mult)
                ts(stab[:, 0:S], stab[:, 0:S], -2.0, ALU.mult, 1.0, ALU.add)
                act(angt[:, 0:S], angt[:, 0:S], AF.Sin, scale=0.5)
                stt(stab[:, 0:S], angt[:, 0:S], 2.0, stab[:, 0:S], ALU.mult, ALU.mult)
                ts(stab[:, 0:S], stab[:, 0:S], sgn, ALU.mult)
                tt(ctab[:, 0:S], angt[:, 0:S], angt[:, 0:S], ALU.mult)
                ts(ctab[:, 0:S], ctab[:, 0:S], -2.0, ALU.mult, 1.0, ALU.add)
                attention_group(l, ctab, stab)
            excl["on"] = True
            if "h" in groups:
                hgrn_group(l)
            if "m" in groups:
                mlstm_group(l)
            excl["on"] = False
        if final_norm:
            dma_in(fgb, d_fgrow[0:1, :].to_broadcast([128, 1024]))
            ts(fgb, fgb, 32.0, ALU.mult)
        for t in range(NT):
            rrx["i"] = (rrx["i"] + 1) % 2
            xt = xt_tiles[rrx["i"]]
            P.op("sp", (lambda dst, s_: (lambda e: e.dma_start(out=dst.ap, in_=s_)))(xt, xsrc[b][t]),
                 reads=[xdr[b][t]], writes=[xt], is_dma=True, dkey=xt.buf.id)
            if final_norm:
                xi = xin[t % 2]
                P.op("act", (lambda o_, i_, a_: (lambda e: e.activation(out=o_.ap, in_=i_.ap, func=AF.Square, accum_out=a_.ap)))(xi, xt, ssq[:, 2:3]),
                     reads=[xt], writes=[xi, ssq])
                rstd_from(ssq[:, 3:4], ssq[:, 2:3], 1024)
                stt(xt, xt, ssq[:, 3:4], fgb, ALU.mult, ALU.mult)
            tok = T(None)
            P.op("sp", (lambda src_, d_: (lambda e: e.dma_start(out=d_, in_=src_.ap)))(xt, d_out[b, t * 128:(t + 1) * 128, :]),
                 reads=[xt], writes=[xdr[b][t], tok], is_dma=True, dkey="o%d" % xt.buf.id)
            fin.append(tok)
    P.op("sp", None, reads=fin)
    P.emit(nc, ctx)
    P.sbuf_left = nc.sbuf_bytes_remaining
    ctx.close()
    return nc, P, dbg_outs


def prep_params(inp, layers, depth_all):
    NL = len(layers)
    f = np.float32
    out = {}
    out["w_in"] = np.ascontiguousarray(inp["w_in"][layers], dtype=f)
    out["w_out"] = np.ascontiguousarray(inp["w_out"][layers], dtype=f)
    out["consts"] = make_consts()
    ng = inp["norm_g"][layers].reshape(NL, 8, 128)
    out["ng"] = np.ascontiguousarray(ng.transpose(2, 0, 1).reshape(128, NL * 8), dtype=f)
    out["fgrow"] = np.ascontiguousarray(inp["final_g"].reshape(1, 1024), dtype=f)
    out["alam"] = np.ascontiguousarray(inp["a_lambda"][layers].reshape(1, NL * 256), dtype=f)
    out["ang"] = np.ascontiguousarray(inp["a_norm_g"][layers].reshape(NL, 4, 128).transpose(2, 0, 1).reshape(128, NL * 4), dtype=f)
    lb = inp["h_lb_logits"].reshape(depth_all, 2, 6, 128)
    out["lb"] = np.ascontiguousarray(lb.transpose(3, 1, 2, 0).reshape(128, 12 * depth_all), dtype=f)
    out["hng"] = np.ascontiguousarray(inp["h_norm_g"][layers].reshape(NL, 6, 128).transpose(2, 0, 1).reshape(128, NL * 6), dtype=f)
    cw = inp["m_conv_w"][layers].reshape(NL, 5, 4, 192)
    out["cwA"] = np.ascontiguousarray(cw[:, :, :, 0:128].transpose(3, 0, 2, 1).reshape(128, NL * 20), dtype=f)
    out["cwB"] = np.ascontiguousarray(cw[:, :, :, 128:192].transpose(3, 0, 2, 1).reshape(64, NL * 20), dtype=f)
    mv = np.stack([inp["m_conv_b"][layers], inp["m_skip"][layers], inp["m_norm_g"][layers]], axis=1)
    mv = mv.reshape(NL, 3, 4, 192)
    out["mvA"] = np.ascontiguousarray(mv[:, :, :, 0:128].transpose(3, 0, 1, 2).reshape(128, NL * 12), dtype=f)
    out["mvB"] = np.ascontiguousarray(mv[:, :, :, 128:192].transpose(3, 0, 1, 2).reshape(64, NL * 12), dtype=f)
    bdA = np.zeros((NL, 4, 128, 6, 128), f)
    bdB = np.zeros((NL, 4, 64, 6, 64), f)
    for wi, nm in enumerate(("m_wq", "m_wk", "m_wv")):
        w = inp[nm][layers].reshape(NL, 4, 48, 4, 4)
        for g_ in range(32):
            bdA[:, :, 4 * g_:4 * g_ + 4, wi, 4 * g_:4 * g_ + 4] = w[:, :, g_]
            bdA[:, :, 4 * g_:4 * g_ + 4, 3 + wi, 4 * g_:4 * g_ + 4] = w[:, :, g_].transpose(0, 1, 3, 2)
        for g_ in range(16):
            bdB[:, :, 4 * g_:4 * g_ + 4, wi, 4 * g_:4 * g_ + 4] = w[:, :, 32 + g_]
            bdB[:, :, 4 * g_:4 * g_ + 4, 3 + wi, 4 * g_:4 * g_ + 4] = w[:, :, 32 + g_].transpose(0, 1, 3, 2)
    out["bdA"] = bdA.reshape(NL, 4, 128, 6 * 128)
    out["bdB"] = bdB.reshape(NL, 4, 64, 6 * 64)
    wg = inp["m_w_gates"][layers].reshape(NL, 3, 4, 192, 16)
    out["wgA"] = np.ascontiguousarray(wg[:, :, :, 0:128].transpose(0, 3, 2, 1, 4).reshape(NL, 128, 4 * 3 * 16), dtype=f)
    out["wgB"] = np.ascontiguousarray(wg[:, :, :, 128:192].transpose(0, 3, 2, 1, 4).reshape(NL, 64, 4 * 3 * 16), dtype=f)
    out["bg"] = np.ascontiguousarray(inp["m_b_gates"][layers].reshape(1, NL * 16), dtype=f)
    return out


_CACHE = {}


def _get_prog(S, NSEQ, NL, depth_all, lam_inits, final_norm):
    key = (S, NSEQ, NL, depth_all, tuple(lam_inits), final_norm)
    if key not in _CACHE:
        _CACHE[key] = build(S, NSEQ, NL, depth_all, lam_inits, final_norm=final_norm)[0]
    return _CACHE[key]


def kernel(**inputs):
    inp = {k: np.asarray(v) for k, v in inputs.items()}
    x = np.ascontiguousarray(inp["x"], dtype=np.float32)
    pos = np.ascontiguousarray(inp["positions"], dtype=np.int32)
    B, S, D = x.shape
    DEPTH = inp["w_in"].shape[0]
    NCORE = 8
    per = B // NCORE
    lam_all = [0.8 - 0.6 * math.exp(-0.3 * l) for l in range(DEPTH)]
    params = prep_params(inp, list(range(DEPTH)), DEPTH)
    nc = _get_prog(S, per, DEPTH, DEPTH, lam_all, True)
    in_maps = []
    for c in range(NCORE):
        m = dict(params)
        m["x"] = x[c * per:(c + 1) * per]
        m["pos"] = pos[c * per:(c + 1) * per]
        in_maps.append(m)
    res = run_bass_kernel_spmd(nc, in_maps, core_ids=list(range(NCORE)))
    out = np.concatenate([np.asarray(r["out"]) for r in res.results], axis=0)
    return out.astype(np.float32)
```

```python
import math
from contextlib import ExitStack
import numpy as np
import concourse.bass as bass
import concourse.mybir as mybir
from concourse.bass_utils import run_bass_kernel_spmd

F32 = mybir.dt.float32
BF16 = mybir.dt.bfloat16
I32 = mybir.dt.int32
ALU = mybir.AluOpType
AF = mybir.ActivationFunctionType
AX = mybir.AxisListType

ENGS = ("pe", "act", "dve", "pool", "sp")
SEM_WRAP = 30000
EPS = 1e-6


class Buf:
    __slots__ = ("last_w", "readers", "id")
    _n = 0

    def __init__(self):
        self.last_w = None
        self.readers = []
        Buf._n += 1
        self.id = Buf._n


class T:
    __slots__ = ("ap", "buf", "extra")

    def __init__(self, ap, buf=None, extra=()):
        self.ap = ap
        self.buf = buf if buf is not None else Buf()
        self.extra = tuple(extra)

    def __getitem__(self, idx):
        return T(self.ap[idx], self.buf, self.extra)

    def bufs(self):
        return (self.buf,) + self.extra

    def sub(self, idx):
        return T(self.ap[idx], Buf())


class Op:
    __slots__ = ("eng", "fn", "deps", "idx", "is_dma", "sig", "dkey")


class Prog:
    def __init__(self):
        self.ops = []

    def op(self, eng, fn, reads=(), writes=(), is_dma=False, dkey=None):
        o = Op()
        o.eng = eng
        o.fn = fn
        o.is_dma = is_dma
        o.dkey = dkey
        o.sig = None
        o.idx = len(self.ops)
        deps = set()
        rb = [b for t in reads for b in t.bufs()]
        wb = [b for t in writes for b in t.bufs()]
        for b in rb:
            if b.last_w is not None:
                deps.add(b.last_w)
        for b in wb:
            if b.last_w is not None:
                deps.add(b.last_w)
            deps.update(b.readers)
        for b in rb:
            b.readers.append(o.idx)
        for b in wb:
            b.last_w = o.idx
            b.readers = []
        deps.discard(o.idx)
        o.deps = deps
        self.ops.append(o)
        return o

    def emit(self, nc, ctx):
        ops = self.ops
        needed = set()
        for o in ops:
            best = {}
            for d in o.deps:
                p = ops[d]
                if p.eng == "pe" and o.eng == "pe" and not p.is_dma and not o.is_dma:
                    continue
                key = ("dma", p.dkey) if p.is_dma else ("eng", p.eng)
                if key not in best or best[key] < d:
                    best[key] = d
            o.deps = sorted(best.values())
            needed.update(o.deps)
        counters = {}
        sems = {}

        def getsem(name):
            if name not in sems:
                sems[name] = ctx.enter_context(nc.semaphore(name))
            return sems[name]

        for o in ops:
            if o.idx not in needed and not (o.is_dma and o.fn is not None):
                continue
            if o.is_dma:
                base = "d%s" % (o.dkey,)
                inc = 16
            else:
                base = "e" + o.eng
                inc = 1
            cnt, epoch = counters.get(base, (0, 0))
            if cnt + inc > SEM_WRAP:
                epoch += 1
                cnt = 0
            cnt += inc
            counters[base] = (cnt, epoch)
            o.sig = ("%s_%d" % (base, epoch), cnt, inc)
        for o in ops:
            if o.sig:
                getsem(o.sig[0])
        self.n_sems = len(sems)
        per_eng = {e: [] for e in ENGS}
        for o in ops:
            per_eng[o.eng].append(o)
        block = ctx.enter_context(nc.Block())

        def run(eng_obj, lst):
            last_wait = {}
            for o in lst:
                for d in o.deps:
                    sname, val, _ = ops[d].sig
                    if last_wait.get(sname, 0) >= val:
                        continue
                    last_wait[sname] = val
                    eng_obj.wait_ge(sems[sname], val)
                if o.fn is None:
                    continue
                ins = o.fn(eng_obj)
                if o.sig is not None:
                    ins.then_inc(sems[o.sig[0]], o.sig[2])

        @block.tensor
        def _(e):
            run(e, per_eng["pe"])

        @block.scalar
        def _(e):
            run(e, per_eng["act"])

        @block.vector
        def _(e):
            run(e, per_eng["dve"])

        @block.gpsimd
        def _(e):
            run(e, per_eng["pool"])

        @block.sync
        def _(e):
            run(e, per_eng["sp"])


D_MODEL = 1024
D_MIX = 2048
M_W = 768
H_W = 768
A_W = 512
IN_COLS = 8192
OFF = dict(xm=0, om=768, zm=1536, hq=2304, hff=3072, hfb=3840, hi=4608, hz=5376,
           aq=6144, ak=6656, av=7168, az=7680)
Y_OFF = dict(m=0, h=768, a=1536)
ROPE_THETA = 500000.0
NEG = -30000.0

C_IDENT = 0
C_U = 128
C_L = 256
C_PSW = 384
C_ONES = 512
C_VEC = 640
NCONST = 648


def make_consts():
    c = np.zeros((128, NCONST), np.float32)
    r = np.arange(128)
    c[:, C_IDENT:C_IDENT + 128] = np.eye(128, dtype=np.float32)
    c[:, C_U:C_U + 128] = (r[:, None] <= r[None, :]).astype(np.float32)
    c[:, C_L:C_L + 128] = (r[:, None] >= r[None, :]).astype(np.float32)
    psw = np.zeros((128, 128), np.float32)
    for m in range(128):
        d = m % 64
        if d < 8:
            psw[m + 8, m] = 1.0
        elif d < 16:
            psw[m - 8, m] = 1.0
    c[:, C_PSW:C_PSW + 128] = psw
    c[:, C_ONES:C_ONES + 128] = 1.0
    half = 8
    inv = ROPE_THETA ** (-np.arange(half, dtype=np.float32) / half)
    for p in range(128):
        d = p % 64
        if d < 16:
            c[p, C_VEC + 0] = inv[d % 8]
            c[p, C_VEC + 1] = -1.0 if d < 8 else 1.0
        c[p, C_VEC + 2] = 1.0 if p < 64 else 0.0
        c[p, C_VEC + 3] = 0.0 if p < 64 else 1.0
    c[:, C_VEC + 4] = 1024 * EPS
    c[:, C_VEC + 5] = 128 * EPS
    c[:, C_VEC + 6] = 192 * EPS
    c[:, C_VEC + 7] = 1.0
    return c


def build(S, NSEQ, NL, DEPTH_ALL, lam_inits, final_norm=True, groups=("a", "h", "m"), dbg=()):
    NT = S // 128
    NG = S // 512
    nc = bass.Bass("TRN2", target_bir_lowering=False)
    P = Prog()
    ctx = ExitStack()

    def dram(name, shape, dt=F32, kind="ExternalInput"):
        return nc.dram_tensor(name, shape, dt, kind=kind).ap()

    d_x = dram("x", [NSEQ, S, D_MODEL])
    d_pos = dram("pos", [NSEQ, S], I32)
    d_win = dram("w_in", [NL, D_MODEL, IN_COLS])
    d_wout = dram("w_out", [NL, D_MIX, D_MODEL])
    d_consts = dram("consts", [128, NCONST])
    d_ng = dram("ng", [128, NL * 8])
    d_fgrow = dram("fgrow", [1, 1024])
    d_alam = dram("alam", [1, NL * 256])
    d_ang = dram("ang", [128, NL * 4])
    d_lb = dram("lb", [128, 2 * 6 * DEPTH_ALL])
    d_hng = dram("hng", [128, NL * 6])
    d_cwA = dram("cwA", [128, NL * 4 * 5])
    d_cwB = dram("cwB", [64, NL * 4 * 5])
    d_mvA = dram("mvA", [128, NL * 4 * 3])
    d_mvB = dram("mvB", [64, NL * 4 * 3])
    d_bdA = dram("bdA", [NL, 4, 128, 6 * 128])
    d_bdB = dram("bdB", [NL, 4, 64, 6 * 64])
    d_wgA = dram("wgA", [NL, 128, 4 * 3 * 16])
    d_wgB = dram("wgB", [NL, 64, 4 * 3 * 16])
    d_bg = dram("bg", [1, NL * 16])
    d_out = dram("out", [NSEQ, S, D_MODEL], kind="ExternalOutput")
    dbg_outs = {}

    def sb(shape, dt=F32, name=None):
        return T(ctx.enter_context(nc.sbuf_tensor("s_" + name, shape, dt))[:])

    def dma_in(dst, src_ap, eng="sp"):
        P.op(eng, lambda e: e.dma_start(out=dst.ap, in_=src_ap), writes=[dst], is_dma=True, dkey=dst.buf.id)

    def dma_out(dst_ap, src, eng="sp"):
        tok = T(None)
        P.op(eng, lambda e: e.dma_start(out=dst_ap, in_=src.ap), reads=[src], writes=[tok], is_dma=True,
             dkey="o%d" % src.buf.id)
        return tok

    def mm(out, lhsT, rhs, start=True, stop=True, extra_reads=()):
        P.op("pe", lambda e: e.matmul(out.ap, lhsT=lhsT.ap, rhs=rhs.ap, start=start, stop=stop),
             reads=[lhsT, rhs] + list(extra_reads), writes=[out])

    def transp(out, in_, ident):
        P.op("pe", lambda e: e.transpose(out=out.ap, in_=in_.ap, identity=ident.ap), reads=[in_, ident], writes=[out])

    def act(out, in_, func, bias=None, scale=None, eng="act", extra_reads=()):
        kw = {}
        rd = [in_] + list(extra_reads)
        if bias is not None:
            if isinstance(bias, T):
                kw["bias"] = bias.ap
                rd.append(bias)
            else:
                kw["bias"] = bias
        if scale is not None:
            if isinstance(scale, T):
                kw["scale"] = scale.ap
                rd.append(scale)
            else:
                kw["scale"] = scale
        P.op(eng, lambda e: e.activation(out=out.ap, in_=in_.ap, func=func, **kw), reads=rd, writes=[out])

    def tt(out, a, b, op, eng="dve"):
        P.op(eng, lambda e: e.tensor_tensor(out=out.ap, in0=a.ap, in1=b.ap, op=op), reads=[a, b], writes=[out])

    def ts(out, a, s1, op0, s2=None, op1=None, eng="dve"):
        rd = [a]
        v1 = s1
        v2 = s2
        if isinstance(s1, T):
            rd.append(s1)
            v1 = s1.ap
        if isinstance(s2, T):
            rd.append(s2)
            v2 = s2.ap
        if op1 is None:
            P.op(eng, lambda e: e.tensor_scalar(out=out.ap, in0=a.ap, scalar1=v1, scalar2=None, op0=op0),
                 reads=rd, writes=[out])
        else:
            P.op(eng, lambda e: e.tensor_scalar(out=out.ap, in0=a.ap, scalar1=v1, scalar2=v2, op0=op0, op1=op1),
                 reads=rd, writes=[out])

    def stt(out, a, s, b, op0, op1, eng="dve"):
        rd = [a, b]
        v = s
        if isinstance(s, T):
            rd.append(s)
            v = s.ap
        P.op(eng, lambda e: e.scalar_tensor_tensor(out=out.ap, in0=a.ap, scalar=v, in1=b.ap, op0=op0, op1=op1),
             reads=rd, writes=[out])

    def cp(out, in_, eng="dve"):
        if eng == "act":
            P.op("act", lambda e: e.copy(out=out.ap, in_=in_.ap), reads=[in_], writes=[out])
        else:
            P.op(eng, lambda e: e.tensor_copy(out=out.ap, in_=in_.ap), reads=[in_], writes=[out])

    def memset(out, val, eng="pool"):
        P.op(eng, lambda e: e.memset(out.ap, val), writes=[out])

    def recip(out, in_):
        P.op("dve", lambda e: e.reciprocal(out=out.ap, in_=in_.ap), reads=[in_], writes=[out])

    def scan_cumsum(out, ones, in_):
        P.op("dve", lambda e: e.tensor_tensor_scan(out=out.ap, data0=ones.ap, data1=in_.ap, initial=0.0,
                                                   op0=ALU.mult, op1=ALU.add), reads=[ones, in_], writes=[out])

    def dbg_out(name, src, shape, dt=BF16):
        if name not in dbg:
            return
        d = dram("dbg_" + name, shape, dt, kind="ExternalOutput")
        dbg_outs[name] = d
        fin.append(dma_out(d, src))

    fin = []

    banks = [T(ctx.enter_context(nc.psum_tensor("pb%d" % i, [128, 512], F32))[:]) for i in range(8)]
    rr = {"b": 0, "x": 0}
    XB = (5, 6, 7)
    excl = {"on": False}
    SKEW = 2

    def pbank():
        while True:
            rr["b"] = (rr["b"] + 1) % 8
            if excl["on"] and rr["b"] in XB:
                continue
            return banks[rr["b"]]

    def xbank():
        rr["x"] = (rr["x"] + 1) % len(XB)
        return banks[XB[rr["x"]]]

    def pquart():
        return pbank()[:, 0:128]

    def run_pipeline(units, stages, skew):
        n = len(units)
        K = len(stages)
        for step in range(n + (K - 1) * skew):
            for s in reversed(range(K)):
                ui = step - s * skew
                if 0 <= ui < n:
                    stages[s](units[ui])

    consts_f = sb([128, NCONST], F32, "consts_f")
    consts_b = sb([128, 640], BF16, "consts_b")
    dma_in(consts_f, d_consts)
    cp(consts_b, consts_f[:, 0:640])
    ident_f = consts_f[:, C_IDENT:C_IDENT + 128]
    U_f = consts_f[:, C_U:C_U + 128]
    L_f = consts_f[:, C_L:C_L + 128]
    psw_b = consts_b[:, C_PSW:C_PSW + 128]
    ones_b = consts_b[:, C_ONES:C_ONES + 128]
    ones_f = consts_f[:, C_ONES:C_ONES + 128]
    U_b = consts_b[:, C_U:C_U + 128]
    L_b = consts_b[:, C_L:C_L + 128]
    invf = consts_f[:, C_VEC + 0:C_VEC + 1]
    sgn = consts_f[:, C_VEC + 1:C_VEC + 2]
    m1 = consts_f[:, C_VEC + 2:C_VEC + 3]
    m2 = consts_f[:, C_VEC + 3:C_VEC + 4]
    epsc = {1024: consts_f[:, C_VEC + 4:C_VEC + 5], 128: consts_f[:, C_VEC + 5:C_VEC + 6], 192: consts_f[:, C_VEC + 6:C_VEC + 7]}
    one_c = consts_f[:, C_VEC + 7:C_VEC + 8]

    def rstd_from(out, v, n):
        act(out, v, AF.Ln, bias=epsc[n][0:out.ap.shape[0], :])
        act(out, out, AF.Exp, scale=-0.5)
    mnegF = sb([128, 128], F32, "mnegF")
    mnegB = sb([128, 128], F32, "mnegB")
    ts(mnegF, U_f, 1.0, ALU.subtract, -NEG, ALU.mult)
    ts(mnegB, L_f, 1.0, ALU.subtract, -NEG, ALU.mult)

    ng = sb([128, NL * 8], F32, "ng")
    dma_in(ng, d_ng)
    ng32 = sb([128, NL * 8], F32, "ng32")
    ts(ng32, ng, 32.0, ALU.mult)
    ang = sb([128, NL * 4], F32, "angs")
    dma_in(ang, d_ang)
    lbl = sb([128, 2 * 6 * DEPTH_ALL], F32, "lbl")
    dma_in(lbl, d_lb)
    hng = sb([128, NL * 6], F32, "hngs")
    dma_in(hng, d_hng)
    cwA = sb([128, NL * 20], F32, "cwA")
    dma_in(cwA, d_cwA)
    cwB = sb([64, NL * 20], F32, "cwB")
    dma_in(cwB, d_cwB)
    mvA = sb([128, NL * 12], F32, "mvA")
    dma_in(mvA, d_mvA)
    mvB = sb([64, NL * 12], F32, "mvB")
    dma_in(mvB, d_mvB)
    bgs = sb([128, NL * 16], F32, "bgs")
    dma_in(bgs, d_bg[0:1, :].to_broadcast([128, NL * 16]))

    neglam = sb([128, NL], F32, "neglam")
    gsa = sb([128, NL * 4], F32, "gsa")
    lamtmp = sb([128, 64], F32, "lamtmp")
    lam2 = sb([128, 4], F32, "lam2")
    alam_t = sb([128, 256], F32, "alam")
    for l in range(NL):
        dma_in(alam_t, d_alam[0:1, l * 256:(l + 1) * 256].to_broadcast([128, 256]))
        for j in range(2):
            tt(lamtmp, alam_t[:, j * 128:j * 128 + 64],
               alam_t[:, j * 128 + 64:j * 128 + 128], ALU.mult)
            P.op("dve", (lambda o, i: (lambda e: e.reduce_sum(out=o.ap, in_=i.ap, axis=AX.X)))(lam2[:, j:j + 1], lamtmp),
                 reads=[lamtmp], writes=[lam2])
        act(lam2[:, 2:4], lam2[:, 0:2], AF.Exp)
        tt(lam2[:, 0:1], lam2[:, 3:4], lam2[:, 2:3], ALU.subtract)
        ts(neglam[:, l:l + 1], lam2[:, 0:1], -float(lam_inits[l]), ALU.add)
        ts(gsa[:, l * 4:(l + 1) * 4], ang[:, l * 4:(l + 1) * 4], float((1.0 - lam_inits[l]) * math.sqrt(128.0)), ALU.mult)
    NLB = 12 * DEPTH_ALL
    lbe = sb([128, NLB], F32, "lbe")
    act(lbe, lbl, AF.Exp)
    lbs = sb([128, 12], F32, "lbs")
    P.op("dve", lambda e: e.reduce_sum(out=lbs.ap, in_=lbe.ap.rearrange("p (a l) -> p a l", l=DEPTH_ALL), axis=AX.X),
         reads=[lbe], writes=[lbs])
    lbr = sb([128, 12], F32, "lbr")
    recip(lbr, lbs)
    lbv = sb([128, NL * 12], F32, "lbv")
    omlb = sb([128, NL * 12], F32, "omlb")
    lbt = sb([128, 12], F32, "lbt")

    def lb_for_layer(l, lglob):
        dst = lbv[:, l * 12:(l + 1) * 12]
        if lglob == 0:
            memset(dst, 0.0, eng="dve")
        else:
            P.op("dve", lambda e: e.reduce_sum(
                out=lbt.ap, in_=lbe.ap.rearrange("p (a l) -> p a l", l=DEPTH_ALL)[:, :, 1:lglob + 1], axis=AX.X),
                reads=[lbe], writes=[lbt])
            tt(dst, lbt, lbr, ALU.mult)
        ts(omlb[:, l * 12:(l + 1) * 12], dst, -1.0, ALU.mult, 1.0, ALU.add)

    hgs = sb([128, NL * 6], F32, "hgs")
    ts(hgs, hng, float(math.sqrt(128.0)), ALU.mult)
    mgsA = sb([128, NL * 4], F32, "mgsA")
    mgsB = sb([64, NL * 4], F32, "mgsB")

    xdr = [[[T(None), T(None)] for t in range(NT)] for b in range(NSEQ)]
    xsrc = {}
    hT = [sb([128, S], BF16, "hT%d" % k) for k in range(8)]
    hTg = [[hT[k].sub((slice(None), slice(g * 512, (g + 1) * 512))) for g in range(NG)] for k in range(8)]
    W_bf = [sb([128, S], BF16, "wbf%d" % i) for i in range(10)]
    W_f32 = [sb([128, max(S + 4, 1024)], F32, "wf%d" % i) for i in range(3)]
    xin = [W_f32[0][:, 0:1024], W_f32[1][:, 0:1024]]
    fgb = W_f32[2][:, 0:1024]
    _xt_raw = [sb([128, 1024], F32, "xtile%d" % i) for i in range(2)]
    xt_half = []
    xt_tiles = []
    for r_ in _xt_raw:
        h0_ = r_.sub((slice(None), slice(0, 512)))
        h1_ = r_.sub((slice(None), slice(512, 1024)))
        xt_half += [h0_, h1_]
        xt_tiles.append(T(r_.ap, h0_.buf, (h1_.buf,)))
    rrx = {"i": 0}
    ssq = sb([128, 4], F32, "ssq")
    tmpf = [sb([128, 512], F32, "tmpf%d" % i) for i in range(4)]
    tmpb = [sb([128, 512], BF16, "tmpb%d" % i) for i in range(5)]
    rrt = {"f": 0, "b": 0}

    def tf():
        rrt["f"] = (rrt["f"] + 1) % len(tmpf)
        return tmpf[rrt["f"]]

    def tb():
        rrt["b"] = (rrt["b"] + 1) % len(tmpb)
        return tmpb[rrt["b"]]

    NWB = 3
    wbf = [sb([128, 8, 128], BF16, "wbf16_%d" % i) for i in range(NWB)]
    rrw = {"s": 0, "b": 0, "os": 0, "ob": 0}
    wo_bf = [sb([128, 1024], BF16, "wobf%d" % i) for i in range(4)]

    def load_win(l, col0, ncols):
        rrw["b"] = (rrw["b"] + 1) % NWB
        wb = wbf[rrw["b"]]
        src_ = d_win[l, :, col0:col0 + ncols].rearrange("(k p) c -> p k c", p=128)
        dma_in(wb[:, :, 0:ncols], src_, eng="pool")
        return wb

    def load_wout(l, row0, nrows):
        rrw["ob"] = (rrw["ob"] + 1) % 4
        wb = wo_bf[rrw["ob"]]
        dma_in(wb[0:nrows, :], d_wout[l, row0:row0 + nrows, :], eng="pool")
        return wb

    def proj_fm(ps, wb, c0, ncols, g):
        for k in range(8):
            mm(ps[0:ncols, :], wb[:, k, c0:c0 + ncols], hTg[k][g], start=(k == 0), stop=(k == 7))

    def proj_tm(ps_view, wb, c0, ncols, tt_):
        g = tt_ // 4
        o = (tt_ % 4) * 128
        for k in range(8):
            mm(ps_view, hTg[k][g][:, o:o + 128], wb[:, k, c0:c0 + ncols], start=(k == 0), stop=(k == 7))

    def sumsq_rstd(rstd_out, srcs, n, g_cols):
        ps = pbank()
        for i, (s, kk) in enumerate(srcs):
            sq = tb()
            act(sq[0:kk, 0:g_cols], s, AF.Square)
            mm(ps[:, 0:g_cols], ones_b[0:kk, :], sq[0:kk, 0:g_cols], start=(i == 0), stop=(i == len(srcs) - 1))
        rstd_from(rstd_out, ps[:, 0:g_cols], n)

    def out_proj(l, ysrcs):
        b = cur["b"]
        wts = [load_wout(l, r0, kk) for (_, kk, r0) in ysrcs]
        NQ = 2 * NT

        def ld(q):
            t, hf = q // 2, q % 2
            hb = xt_half[q % 4]
            P.op("sp", (lambda dst, s_: (lambda e: e.dma_start(out=dst.ap, in_=s_)))(hb, xsrc[b][t][:, hf * 512:(hf + 1) * 512]),
                 reads=[xdr[b][t][hf]], writes=[hb], is_dma=True, dkey=hb.buf.id)

        ld(0)
        ld(1)
        for q in range(NQ):
            t, hf = q // 2, q % 2
            g = t // 4
            o = (t % 4) * 128
            hb = xt_half[q % 4]
            if q + 2 < NQ:
                ld(q + 2)
            ps = pbank()
            for i, (yf, kk, r0) in enumerate(ysrcs):
                mm(ps, yf(g)[:, o:o + 128], wts[i][0:kk, hf * 512:(hf + 1) * 512], start=(i == 0), stop=(i == len(ysrcs) - 1))
            tt(hb, hb, ps, ALU.add)
            P.op("sp", (lambda src_, d_: (lambda e: e.dma_start(out=d_, in_=src_.ap)))(hb, d_out[b, t * 128:(t + 1) * 128, hf * 512:(hf + 1) * 512]),
                 reads=[hb], writes=[xdr[b][t][hf]], is_dma=True, dkey="o%d" % hb.buf.id)
            if hf == 1:
                xsrc[b][t] = d_out[b, t * 128:(t + 1) * 128, :]

    def attention_group(l, ctab, stab):
        qt = W_bf[0]
        k1 = W_bf[1]
        k2 = W_bf[2]
        vtok = W_bf[3]
        sz = W_bf[4]
        ys = [W_bf[5], W_bf[6], W_bf[7], W_bf[8]]
        vt3 = T(vtok.ap.rearrange("p (t c) -> p t c", c=128), vtok.buf)
        for hd in range(4):
            wq = load_win(l, OFF["aq"] + hd * 128, 128)
            for g in range(NG):
                gs = slice(g * 512, (g + 1) * 512)
                ps = pbank()
                proj_fm(ps, wq, 0, 128, g)
                a_bf = tb()
                cp(a_bf, ps, eng="act")
                ps2 = pbank()
                mm(ps2, psw_b, a_bf)
                t1 = tf()
                tt(t1, ps2, stab[:, gs], ALU.mult)
                t2 = tf()
                tt(t2, ps, ctab[:, gs], ALU.mult)
                tt(qt[:, gs], t1, t2, ALU.add)
            wk = load_win(l, OFF["ak"] + hd * 128, 128)
            for g in range(NG):
                gs = slice(g * 512, (g + 1) * 512)
                ps = pbank()
                proj_fm(ps, wk, 0, 128, g)
                a_bf = tb()
                cp(a_bf, ps, eng="act")
                ps2 = pbank()
                mm(ps2, psw_b, a_bf)
                t1 = tf()
                tt(t1, ps2, stab[:, gs], ALU.mult)
                t2 = tf()
                tt(t2, ps, ctab[:, gs], ALU.mult)
                ktmp = tb()
                tt(ktmp, t1, t2, ALU.add)
                ts(k1[:, gs], ktmp, m1, ALU.mult)
                ts(k2[:, gs], ktmp, m2, ALU.mult)
            wv = load_win(l, OFF["av"] + hd * 128, 128)
            for g in range(NG):
                ps = pbank()
                for j in range(4):
                    proj_tm(ps[:, j * 128:(j + 1) * 128], wv, 0, 128, g * 4 + j)
                cp(T(vtok.ap[:, g * 512:(g + 1) * 512], vtok.buf), ps, eng="act")
            wz = load_win(l, OFF["az"] + hd * 128, 128)
            for g in range(NG):
                ps = pbank()
                proj_fm(ps, wz, 0, 128, g)
                act(sz[:, g * 512:(g + 1) * 512], ps, AF.Silu)
            kk = [k1, k2]
            for g in range(NG):
                gs = slice(g * 512, (g + 1) * 512)
                num = [banks[0], banks[1]]
                den = [banks[2], banks[3]]
                sc_banks = [banks[4], banks[5], banks[6]]
                iters = [(kt, c) for kt in range(NT) for c in range(2)]

                def score(i, gs=gs):
                    kt, c = iters[i]
                    mm(sc_banks[i % 3], kk[c][:, kt * 128:(kt + 1) * 128], qt[:, gs])

                score(0)
                for i in range(len(iters)):
                    if i + 1 < len(iters):
                        score(i + 1)
                    kt, c = iters[i]
                    pt = tb()
                    act(pt, sc_banks[i % 3], AF.Exp, scale=0.125)
                    mm(num[c], vt3[:, kt, :], pt, start=(kt == 0), stop=(kt == NT - 1))
                    mm(den[c], ones_b, pt, start=(kt == 0), stop=(kt == NT - 1))
                r1 = tf()
                recip(r1, den[0])
                r2 = tf()
                recip(r2, den[1])
                o1 = tf()
                tt(o1, num[0], r1, ALU.mult)
                o2 = tf()
                tt(o2, num[1], r2, ALU.mult)
                o = tf()
                stt(o, o2, neglam[:, l:l + 1], o1, ALU.mult, ALU.add)
                rstd = tf()
                sumsq_rstd(rstd, [(o, 128)], 128, 512)
                y1 = o1
                tt(y1, o, rstd, ALU.mult)
                stt(ys[hd][:, gs], y1, gsa[:, l * 4 + hd:l * 4 + hd + 1], sz[:, gs], ALU.mult, ALU.mult)
        dbg_out("ya", ys[0], [128, S])
        out_proj(l, [((lambda g, hd=hd: ys[hd][:, g * 512:(g + 1) * 512]), 128, Y_OFF["a"] + hd * 128) for hd in range(4)])

    def hgrn_group(l):
        qT_ = W_bf[0]
        kT_ = W_bf[1]
        vtok = W_bf[2]
        vt3 = T(vtok.ap.rearrange("p (t c) -> p t c", c=128), vtok.buf)
        sz = W_bf[3]
        ys = [W_bf[4], W_bf[5], W_bf[6]]
        a_pad = W_f32[0]
        na_pad = W_f32[1]
        gtmp = W_f32[1]
        oT = W_f32[2]
        NS0 = 2
        NSXH = 4
        if not hasattr(hgrn_group, "_t"):
            tl_ = dict()
            tl_["ek"] = [[sb([128, 128], BF16, "hek%d_%d" % (s_, i)) for i in range(4)] for s_ in range(NS0)]
            for s_ in range(NS0):
                for i in range(4):
                    memset(tl_["ek"][s_][i], 0.0)
            tl_["eq"] = [sb([128, 128], F32, "heq%d" % i) for i in range(NS0)]
            tl_["ekf"] = [sb([128, 448], F32, "hekf%d" % i) for i in range(NS0)]
            tl_["ekr"] = [{32: t_e.sub((slice(None), slice(0, 32))), 64: t_e.sub((slice(None), slice(32, 96))),
                           96: t_e.sub((slice(None), slice(96, 192))), 128: t_e.sub((slice(None), slice(192, 320))),
                           "kend": t_e.sub((slice(None), slice(320, 448)))} for t_e in tl_["ekf"]]
            tl_["eq2"] = [sb([128, 128], F32, "heq2_%d" % i) for i in range(NS0)]
            tl_["qtl"] = [sb([128, 128], BF16, "hqtl%d" % i) for i in range(NS0)]
            tl_["kend"] = [sb([128, 128], F32, "hkend%d" % i) for i in range(NS0)]
            tl_["kendT"] = [sb([128, 128], BF16, "hkendT%d" % i) for i in range(NS0)]
            tl_["qc"] = [sb([128, 128], BF16, "hqc%d" % i) for i in range(NSXH)]
            tl_["wT"] = [sb([128, 128], BF16, "hwT%d" % i) for i in range(NSXH)]
            tl_["dec"] = sb([128, NSXH], F32, "hdec")
            tl_["Sst"] = sb([128, 128], F32, "hSst")
            tl_["Sbf"] = sb([128, 128], BF16, "hSbf")
            hgrn_group._t = tl_
        tl = hgrn_group._t
        cnt = {"u": 0}
        for hd in range(6):
            wq = load_win(l, OFF["hq"] + hd * 128, 128)
            for g in range(NG):
                ps = pbank()
                proj_fm(ps, wq, 0, 128, g)
                cp(qT_[:, g * 512:(g + 1) * 512], ps, eng="act")
            wv = load_win(l, OFF["hi"] + hd * 128, 128)
            for g in range(NG):
                ps = pbank()
                for j in range(4):
                    proj_tm(ps[:, j * 128:(j + 1) * 128], wv, 0, 128, g * 4 + j)
                cp(T(vtok.ap[:, g * 512:(g + 1) * 512], vtok.buf), ps, eng="act")
            wz = load_win(l, OFF["hz"] + hd * 128, 128)
            for g in range(NG):
                ps = pbank()
                proj_fm(ps, wz, 0, 128, g)
                act(sz[:, g * 512:(g + 1) * 512], ps, AF.Silu)
            for dr in range(2):
                wf = load_win(l, OFF["hff" if dr == 0 else "hfb"] + hd * 128, 128)
                lbc = lbv[:, l * 12 + dr * 6 + hd:l * 12 + dr * 6 + hd + 1]
                olbc = omlb[:, l * 12 + dr * 6 + hd:l * 12 + dr * 6 + hd + 1]
                for g in range(NG):
                    gs = slice(g * 512, (g + 1) * 512)
                    ps = pbank()
                    proj_fm(ps, wf, 0, 128, g)
                    sg = tf()
                    act(sg, ps, AF.Sigmoid)
                    ff = tf()
                    ts(ff, sg, olbc, ALU.mult, lbc, ALU.add)
                    act(gtmp[:, gs], ff, AF.Ln)
                    ts(kT_[:, gs], ff, -1.0, ALU.mult, 1.0, ALU.add)
                memset(a_pad[:, 0:1], 0.0, eng="dve")
                scan_cumsum(a_pad[:, 1:S + 1], T(ones_f.ap[:, 0:1].to_broadcast([128, S]), ones_f.buf), gtmp[:, 0:S])
                ts(na_pad[:, 0:S + 1], a_pad[:, 0:S + 1], -1.0, ALU.mult)
                order = list(range(NT)) if dr == 0 else list(range(NT - 1, -1, -1))
                units = []
                for i_, c in enumerate(order):
                    units.append(dict(c=c, first=(i_ == 0), u=cnt["u"]))
                    cnt["u"] += 1

                def st0(un, dr=dr):
                    c, u = un["c"], un["u"]
                    c0 = c * 128
                    s0 = u % NS0
                    sx = u % NSXH
                    eq, ekr, qtl, kend, kendT = tl["eq"][s0], tl["ekr"][s0], tl["qtl"][s0], tl["kend"][s0], tl["kendT"][s0]
                    eq2 = tl["eq2"][s0]
                    ek = tl["ek"][s0]
                    qc, wT = tl["qc"][sx], tl["wT"][sx]
                    dcol = tl["dec"][:, sx:sx + 1]
                    if dr == 0:
                        for I in range(4):
                            act(eq[:, 32 * I:32 * I + 32], a_pad[:, 1 + c0 + 32 * I:1 + c0 + 32 * I + 32], AF.Exp,
                                bias=na_pad[:, c0 + 32 * I:c0 + 32 * I + 1])
                        tt(qtl, qT_[:, c0:c0 + 128], eq, ALU.mult)
                        for I in range(4):
                            w_ = 32 * (I + 1)
                            act(ekr[w_], na_pad[:, 1 + c0:1 + c0 + w_], AF.Exp, bias=a_pad[:, c0 + 32 * I:c0 + 32 * I + 1])
                            tt(ek[I][:, 0:w_], kT_[:, c0:c0 + w_], ekr[w_], ALU.mult)
                        act(eq2, a_pad[:, 1 + c0:1 + c0 + 128], AF.Exp, bias=na_pad[:, c0:c0 + 1])
                        tt(qc, qT_[:, c0:c0 + 128], eq2, ALU.mult)
                        act(ekr["kend"], na_pad[:, 1 + c0:1 + c0 + 128], AF.Exp, bias=a_pad[:, c0 + 128:c0 + 129])
                        tt(kend, kT_[:, c0:c0 + 128], ekr["kend"], ALU.mult)
                        mask = U_f
                    else:
                        for I in range(4):
                            act(eq[:, 32 * I:32 * I + 32], na_pad[:, c0 + 32 * I:c0 + 32 * I + 32], AF.Exp,
                                bias=a_pad[:, c0 + 32 * (I + 1):c0 + 32 * (I + 1) + 1])
                        tt(qtl, qT_[:, c0:c0 + 128], eq, ALU.mult)
                        for I in range(4):
                            lo = 32 * I
                            act(ekr[128 - lo], a_pad[:, c0 + lo:c0 + 128], AF.Exp,
                                bias=na_pad[:, c0 + 32 * (I + 1):c0 + 32 * (I + 1) + 1])
                            tt(ek[I][:, lo:128], kT_[:, c0 + lo:c0 + 128], ekr[128 - lo], ALU.mult)
                        act(eq2, na_pad[:, c0:c0 + 128], AF.Exp, bias=a_pad[:, c0 + 128:c0 + 129])
                        tt(qc, qT_[:, c0:c0 + 128], eq2, ALU.mult)
                        act(ekr["kend"], a_pad[:, c0:c0 + 128], AF.Exp, bias=na_pad[:, c0:c0 + 1])
                        tt(kend, kT_[:, c0:c0 + 128], ekr["kend"], ALU.mult)
                        mask = L_f
                    act(dcol, a_pad[:, c0 + 128:c0 + 129], AF.Exp, bias=na_pad[:, c0:c0 + 1])
                    pb = pbank()
                    pss = pb[:, 0:128]
                    pst = pb[:, 128:256]
                    for I in range(4):
                        mm(pss[:, 32 * I:32 * I + 32], ek[I], qtl[:, 32 * I:32 * I + 32])
                    tt(wT, pss, mask, ALU.mult)
                    transp(pst, kend, ident_f)
                    cp(kendT, pst, eng="act")
                    psd = xbank()[:, 0:128]
                    mm(psd, kendT, vt3[:, c, :])
                    un.update(qc=qc, wT=wT, dcol=dcol, psd=psd)

                def st1(un, dr=dr):
                    c, first = un["c"], un["first"]
                    c0 = c * 128
                    qc, wT, dcol, psd = un["qc"], un["wT"], un["dcol"], un["psd"]
                    pso = pquart()
                    mm(pso, vt3[:, c, :], wT, start=True, stop=first)
                    if not first:
                        mm(pso, tl["Sbf"], qc, start=False, stop=True)
                    if first:
                        cp(tl["Sst"], psd)
                    else:
                        stt(tl["Sst"], tl["Sst"], dcol, psd, ALU.mult, ALU.add)
                    cp(tl["Sbf"], tl["Sst"])
                    if dr == 0:
                        cp(oT[:, c0:c0 + 128], pso)
                    else:
                        tt(oT[:, c0:c0 + 128], oT[:, c0:c0 + 128], pso, ALU.add)

                run_pipeline(units, [st0, st1], SKEW)
            yh = ys[hd % 3]
            for g in range(NG):
                gs = slice(g * 512, (g + 1) * 512)
                rstd = tf()
                sumsq_rstd(rstd, [(oT[:, gs], 128)], 128, 512)
                y1 = tf()
                tt(y1, oT[:, gs], rstd, ALU.mult)
                stt(yh[:, gs], y1, hgs[:, l * 6 + hd:l * 6 + hd + 1], sz[:, gs], ALU.mult, ALU.mult)
            if hd == 0:
                dbg_out("yh", yh, [128, S])
            if hd % 3 == 2:
                h0 = hd - 2
                out_proj(l, [((lambda g, j=j: ys[j][:, g * 512:(g + 1) * 512]), 128, Y_OFF["h"] + (h0 + j) * 128) for j in range(3)])

    def mlstm_group(l):
        xmA = W_f32[0]
        xmB = W_f32[1]
        cacc = W_f32[2]
        xcA = W_bf[0]
        xcB = W_bf[1]
        mqA, mqB, mkA, mkB = W_bf[2], W_bf[3], W_bf[4], W_bf[5]
        hfA, hfB = W_bf[6], W_bf[7]
        yA, yB = W_bf[8], W_bf[9]
        xmbA = sb([128, S], BF16, "xmbA") if not hasattr(mlstm_group, "_t") else mlstm_group._t["xmbA"]
        if not hasattr(mlstm_group, "_t"):
            t_ = dict(xmbA=xmbA)
            t_["xmbB"] = sb([128, S], BF16, "xmbB")
            t_["vtok"] = sb([128, NT, 200], BF16, "mvtok")
            t_["ktok"] = sb([128, NT, 192], BF16, "mktok")
            t_["gates"] = sb([128, NT, 16], F32, "mgates")
            t_["lf"] = sb([128, NT, 8], F32, "mlf")
            t_["eib"] = sb([128, NT, 8], F32, "meib")
            t_["bd"] = sb([128, 6 * 128], F32, "mbd")
            t_["bdB"] = sb([64, 6 * 64], F32, "mbdB")
            t_["bdb"] = sb([128, 3 * 128], BF16, "mbdb")
            t_["bdbB"] = sb([64, 3 * 64], BF16, "mbdbB")
            t_["wg"] = sb([128, 4 * 3 * 16], F32, "mwg")
            t_["wgB"] = sb([64, 4 * 3 * 16], F32, "mwgB")
            t_["G"] = sb([128, 4 * 2 * 16], BF16, "mG")
            t_["GB"] = sb([64, 4 * 2 * 16], BF16, "mGB")
            t_["lfrep"] = [sb([128, 128], F32, "mlfrep%d" % i) for i in range(2)]
            t_["Eb"] = [sb([128, 128], F32, "mEb%d" % i) for i in range(2)]
            t_["EbM"] = [sb([128, 128], F32, "mEbM%d" % i) for i in range(2)]
            t_["wT"] = [sb([128, 128], BF16, "mwT%d" % i) for i in range(2)]
            t_["qsA"] = [sb([128, 128], BF16, "mqsA%d" % i) for i in range(2)]
            t_["qsB"] = [sb([64, 128], BF16, "mqsB%d" % i) for i in range(2)]
            t_["dm"] = [sb([128, 128], F32, "mdm%d" % i) for i in range(2)]
            t_["rd"] = [sb([128, 128], F32, "mrd%d" % i) for i in range(2)]
            t_["CA"] = sb([128, 200], F32, "mCA")
            t_["CB"] = sb([64, 200], F32, "mCB")
            t_["CtA"] = sb([128, 200], F32, "mCtA")
            t_["CtB"] = sb([64, 200], F32, "mCtB")
            t_["CbA"] = sb([128, 200], BF16, "mCbA")
            t_["CbB"] = sb([64, 200], BF16, "mCbB")
            t_["nrA"] = sb([128, 128], BF16, "mnrA")
            t_["nrB"] = sb([64, 128], BF16, "mnrB")
            t_["eibrep"] = [sb([128, 128], BF16, "meibrep%d" % i) for i in range(2)]
            t_["hbA"] = sb([128, 512], F32, "mhbA")
            t_["hbB"] = sb([64, 512], F32, "mhbB")
            t_["so"] = [sb([128, 512], BF16, "mso%d" % i) for i in range(4)]
            mlstm_group._t = t_
        t_ = mlstm_group._t
        xmbB = t_["xmbB"]
        vtok, ktok, gates, lf, eib = t_["vtok"], t_["ktok"], t_["gates"], t_["lf"], t_["eib"]
        ts(mgsA[:, l * 4:(l + 1) * 4], mvA[:, l * 12 + 8:l * 12 + 12], float(math.sqrt(192.0)), ALU.mult)
        ts(mgsB[:, l * 4:(l + 1) * 4], mvB[:, l * 12 + 8:l * 12 + 12], float(math.sqrt(192.0)), ALU.mult)
        QSCALE = float(192.0 ** -0.5)

        def compute_xm_xc(j):
            for (xm_, xmb_, xc_, kk, coff, cw_, mv_) in ((xmA, xmbA, xcA, 128, 0, cwA, mvA), (xmB, xmbB, xcB, 64, 128, cwB, mvB)):
                wx = load_win(l, OFF["xm"] + j * 192 + coff, kk)
                memset(xm_[0:kk, 0:2], 0.0, eng="dve")
                memset(xm_[0:kk, S + 2:S + 4], 0.0, eng="dve")
                for g in range(NG):
                    ps = pbank()
                    proj_fm(ps, wx, 0, kk, g)
                    cp(xm_[0:kk, 2 + g * 512:2 + (g + 1) * 512], ps[0:kk, :], eng="act")
                    cp(xmb_[0:kk, g * 512:(g + 1) * 512], ps[0:kk, :], eng="act")
                cb = l * 20 + j * 5
                ts(cacc[0:kk, 0:S], xm_[0:kk, 0:S], cw_[0:kk, cb:cb + 1], ALU.mult)
                for k in range(1, 5):
                    stt(cacc[0:kk, 0:S], xm_[0:kk, k:k + S], cw_[0:kk, cb + k:cb + k + 1], cacc[0:kk, 0:S],
                        ALU.mult, ALU.add)
                act(xc_[0:kk, 0:S], cacc[0:kk, 0:S], AF.Silu, bias=mv_[0:kk, l * 12 + j:l * 12 + j + 1])

        dma_in(t_["wg"], d_wgA[l])
        dma_in(t_["wgB"], d_wgB[l])
        psg = banks[0]
        psg3 = T(psg.ap[:, 0:NT * 16].rearrange("p (t c) -> p t c", c=16), psg.buf)
        for j in range(4):
            dma_in(t_["bd"], d_bdA[l, j])
            dma_in(t_["bdB"], d_bdB[l, j])
            for (bd_, wg_, G_, kk) in ((t_["bd"], t_["wg"], t_["G"], 128), (t_["bdB"], t_["wgB"], t_["GB"], 64)):
                pq = pquart()
                mm(pq[0:kk, 0:16], bd_[0:kk, 3 * kk:4 * kk], wg_[0:kk, (j * 3 + 0) * 16:(j * 3 + 1) * 16], start=True, stop=False)
                mm(pq[0:kk, 0:16], bd_[0:kk, 4 * kk:5 * kk], wg_[0:kk, (j * 3 + 1) * 16:(j * 3 + 2) * 16], start=False, stop=True)
                mm(pq[0:kk, 16:32], bd_[0:kk, 5 * kk:6 * kk], wg_[0:kk, (j * 3 + 2) * 16:(j * 3 + 3) * 16], start=True, stop=True)
                cp(G_[0:kk, j * 32:(j + 1) * 32], pq[0:kk, 0:32])
            compute_xm_xc(j)
            for t in range(NT):
                tsl = slice(t * 128, (t + 1) * 128)
                mm(psg3[:, t, :], xcA[:, tsl], t_["G"][:, j * 32:j * 32 + 16], start=True, stop=False)
                mm(psg3[:, t, :], xmbA[:, tsl], t_["G"][:, j * 32 + 16:j * 32 + 32], start=False, stop=False)
                mm(psg3[:, t, :], xcB[0:64, tsl], t_["GB"][0:64, j * 32:j * 32 + 16], start=False, stop=False)
                mm(psg3[:, t, :], xmbB[0:64, tsl], t_["GB"][0:64, j * 32 + 16:j * 32 + 32], start=False, stop=True)
            if j == 0:
                for t in range(NT):
                    tt(gates[:, t, :], psg3[:, t, :], bgs[:, l * 16:(l + 1) * 16], ALU.add)
            else:
                tt(gates, gates, psg3, ALU.add)
        etmp = sb([128, NT, 8], F32, "metmp") if "etmp" not in t_ else t_["etmp"]
        t_["etmp"] = etmp
        act(etmp[:, :, 0:4], gates[:, :, 4:8], AF.Exp, scale=-1.0)
        act(etmp[:, :, 4:8], gates[:, :, 12:16], AF.Exp, scale=-1.0)
        act(lf, etmp, AF.Ln, bias=one_c)
        ts(lf, lf, -1.0, ALU.mult)
        bcs = sb([128, NT, 8], F32, "mbcs") if "bcs" not in t_ else t_["bcs"]
        t_["bcs"] = bcs
        psb = banks[1]
        psb3 = T(psb.ap[:, 0:NT * 8].rearrange("p (t c) -> p t c", c=8), psb.buf)
        for t in range(NT):
            mm(psb3[:, t, 0:4], U_f, lf[:, t, 0:4])
            mm(psb3[:, t, 4:8], L_f, lf[:, t, 4:8])
        cp(bcs, psb3)
        tt(etmp[:, :, 0:4], gates[:, :, 0:4], bcs[:, :, 0:4], ALU.subtract)
        tt(etmp[:, :, 4:8], gates[:, :, 8:12], bcs[:, :, 4:8], ALU.subtract)
        act(eib, etmp, AF.Exp)
        dbg_out("gates", T(gates.ap.rearrange("p t c -> p (t c)"), gates.buf), [128, NT * 16], F32)

        cntu = {"u": 0}
        for j in range(4):
            dma_in(t_["bd"], d_bdA[l, j])
            dma_in(t_["bdB"], d_bdB[l, j])
            cp(t_["bdb"], t_["bd"][:, 0:384], eng="pool")
            cp(t_["bdbB"], t_["bdB"][:, 0:192], eng="pool")
            bdb, bdbB = t_["bdb"], t_["bdbB"]
            compute_xm_xc(j)
            for g in range(NG):
                gs = slice(g * 512, (g + 1) * 512)
                for (dst, src, w_, kk, sc) in ((mqA, xcA, bdb[:, 0:128], 128, QSCALE), (mqB, xcB, bdbB[:, 0:64], 64, QSCALE),
                                               (mkA, xcA, bdb[:, 128:256], 128, 1.0), (mkB, xcB, bdbB[:, 64:128], 64, 1.0)):
                    ps = pbank()
                    mm(ps[0:kk, :], w_[0:kk, :], src[0:kk, gs])
                    act(dst[0:kk, gs], ps[0:kk, :], AF.Copy, scale=sc)
            for t in range(NT):
                tsl = slice(t * 128, (t + 1) * 128)
                ps = pbank()
                mm(ps[:, 0:128], xmbA[:, tsl], bdb[:, 256:384])
                mm(ps[:, 128:192], xmbB[0:64, tsl], bdbB[0:64, 128:192])
                mm(ps[:, 192:320], xcA[:, tsl], bdb[:, 128:256])
                mm(ps[:, 320:384], xcB[0:64, tsl], bdbB[0:64, 64:128])
                cp(vtok[:, t, 0:192], ps[:, 0:192], eng="act")
                cp(ktok[:, t, :], ps[:, 192:384], eng="act")
            memset(vtok[:, :, 192:193], 1.0, eng="dve")
            NSX = 4
            if "vp" not in t_:
                t_["vp"] = [sb([128, 200], BF16, "mvp%d" % i) for i in range(NSX)]
                for nm_, shp_, dt_ in (("Eb", [128, 128], F32), ("wT", [128, 128], BF16), ("qsA", [128, 128], BF16),
                                       ("qsB", [64, 128], BF16), ("eibrep", [128, 128], BF16)):
                    t_[nm_] = t_[nm_] + [sb(shp_, dt_, "mx%s%d" % (nm_, i)) for i in range(2, NSX)]
            for dr in range(2):
                order = list(range(NT)) if dr == 0 else list(range(NT - 1, -1, -1))
                gcol = dr * 4 + j
                tri = U_f if dr == 0 else L_f
                mneg = mnegF if dr == 0 else mnegB
                units = []
                for i_, c in enumerate(order):
                    units.append(dict(c=c, first=(i_ == 0), u=cntu["u"]))
                    cntu["u"] += 1

                def st0(un, dr=dr, gcol=gcol, tri=tri, mneg=mneg):
                    c, u, first = un["c"], un["u"], un["first"]
                    csl = slice(c * 128, (c + 1) * 128)
                    sx = u % NSX
                    Eb, wT, qsA, qsB = t_["Eb"][sx], t_["wT"][sx], t_["qsA"][sx], t_["qsB"][sx]
                    vpc, eibrep = t_["vp"][sx], t_["eibrep"][sx]
                    EbM, lfrep = t_["EbM"][u % 2], t_["lfrep"][u % 2]
                    ts(vpc[:, 0:193], vtok[:, c, 0:193], eib[:, c, gcol:gcol + 1], ALU.mult)
                    act(eibrep, ones_f, AF.Copy, scale=eib[:, c, gcol:gcol + 1])
                    bk1 = pbank()
                    pss = bk1[:, 0:128]
                    psl = bk1[:, 128:256]
                    pslm = bk1[:, 256:384]
                    mm(pss, mkA[:, csl], mqA[:, csl], start=True, stop=False)
                    mm(pss, mkB[0:64, csl], mqB[0:64, csl], start=False, stop=True)
                    act(lfrep, ones_f, AF.Copy, scale=lf[:, c, gcol:gcol + 1])
                    mm(psl, lfrep, tri)
                    act(Eb, psl, AF.Exp)
                    tt(EbM, Eb, tri, ALU.mult)
                    tt(wT, pss, EbM, ALU.mult)
                    if not first:
                        tt(qsA, mqA[:, csl], Eb, ALU.mult)
                        tt(qsB, mqB[0:64, csl], Eb[0:64, :], ALU.mult)
                    bk3 = xbank()
                    pcA = bk3[:, 0:256]
                    pcB = bk3[:, 256:512]
                    mm(pcA[:, 0:193], ktok[:, c, 0:128], vpc[:, 0:193])
                    mm(pcB[0:64, 0:193], ktok[:, c, 128:192], vpc[:, 0:193])
                    un.update(Eb=Eb, wT=wT, qsA=qsA, qsB=qsB, vpc=vpc, eibrep=eibrep, pcA=pcA, pcB=pcB)

                def st1(un, dr=dr):
                    c, u, first = un["c"], un["u"], un["first"]
                    csl = slice(c * 128, (c + 1) * 128)
                    Eb, wT, qsA, qsB = un["Eb"], un["wT"], un["qsA"], un["qsB"]
                    vpc, eibrep, pcA, pcB = un["vpc"], un["eibrep"], un["pcA"], un["pcB"]
                    dm, rd = t_["dm"][u % 2], t_["rd"][u % 2]
                    bk2 = pbank()
                    pnA = bk2[:, 0:128]
                    pnB = bk2[:, 128:256]
                    pdn = bk2[:, 256:384]
                    mm(pnA, vpc[:, 0:128], wT, start=True, stop=first)
                    if not first:
                        mm(pnA, t_["CbA"][:, 0:128], qsA, start=False, stop=False)
                        mm(pnA, t_["CbB"][:, 0:128], qsB, start=False, stop=True)
                    mm(pnB[0:64, :], vpc[:, 128:192], wT, start=True, stop=first)
                    if not first:
                        mm(pnB[0:64, :], t_["CbA"][:, 128:192], qsA, start=False, stop=False)
                        mm(pnB[0:64, :], t_["CbB"][:, 128:192], qsB, start=False, stop=True)
                    mm(pdn, eibrep, wT, start=True, stop=first)
                    if not first:
                        mm(pdn, t_["nrA"], qsA, start=False, stop=False)
                        mm(pdn, t_["nrB"], qsB, start=False, stop=True)
                    ebl = Eb[:, 127:128] if dr == 0 else Eb[:, 0:1]
                    if first:
                        act(t_["CA"][:, 0:193], pcA[:, 0:193], AF.Copy, scale=ebl)
                        act(t_["CB"][:, 0:193], pcB[0:64, 0:193], AF.Copy, scale=ebl[0:64, :])
                        act(t_["CbA"][:, 0:193], pcA[:, 0:193], AF.Copy, scale=ebl)
                        act(t_["CbB"][:, 0:193], pcB[0:64, 0:193], AF.Copy, scale=ebl[0:64, :])
                    else:
                        tt(t_["CtA"][:, 0:193], pcA[:, 0:193], t_["CA"][:, 0:193], ALU.add)
                        tt(t_["CtB"][:, 0:193], pcB[0:64, 0:193], t_["CB"][:, 0:193], ALU.add)
                        act(t_["CA"][:, 0:193], t_["CtA"][:, 0:193], AF.Copy, scale=ebl)
                        act(t_["CB"][:, 0:193], t_["CtB"][:, 0:193], AF.Copy, scale=ebl[0:64, :])
                        act(t_["CbA"][:, 0:193], t_["CtA"][:, 0:193], AF.Copy, scale=ebl)
                        act(t_["CbB"][:, 0:193], t_["CtB"][:, 0:193], AF.Copy, scale=ebl[0:64, :])
                    act(t_["nrA"], ones_f, AF.Copy, scale=t_["CA"][:, 192:193])
                    act(t_["nrB"], ones_f[0:64, :], AF.Copy, scale=t_["CB"][:, 192:193])
                    ts(rd, pdn, -1.0, ALU.mult, 1.0, ALU.max)
                    stt(dm, pdn, 1.0, rd, ALU.max, ALU.max)
                    recip(rd, dm)
                    if dr == 0:
                        tt(hfA[:, csl], pnA, rd, ALU.mult)
                        tt(hfB[0:64, csl], pnB[0:64, :], rd[0:64, :], ALU.mult)
                    else:
                        o4 = (c % 4) * 128
                        tt(t_["hbA"][:, o4:o4 + 128], pnA, rd, ALU.mult)
                        tt(t_["hbB"][0:64, o4:o4 + 128], pnB[0:64, :], rd[0:64, :], ALU.mult)
                    if dr == 1 and c % 4 == 0:
                        g = c // 4
                        gs = slice(g * 512, (g + 1) * 512)
                        hbA, hbB = t_["hbA"], t_["hbB"]
                        tt(hbA, hbA, hfA[:, gs], ALU.add)
                        tt(hbB, hbB, hfB[0:64, gs], ALU.add)
                        so = t_["so"]
                        for (idx, nm, coff, kk, fn) in ((0, "om", 0, 128, AF.Sigmoid), (1, "om", 128, 64, AF.Sigmoid),
                                                        (2, "zm", 0, 128, AF.Silu), (3, "zm", 128, 64, AF.Silu)):
                            wz = load_win(l, OFF[nm] + j * 192 + coff, kk)
                            ps = pbank()
                            proj_fm(ps, wz, 0, kk, g)
                            act(so[idx][0:kk, :], ps[0:kk, :], fn)
                        tt(hbA, hbA, so[0], ALU.mult)
                        tt(hbB, hbB, so[1][0:64, :], ALU.mult)
                        rstd = tf()
                        sumsq_rstd(rstd, [(hbA, 128), (hbB, 64)], 192, 512)
                        tt(hbA, hbA, rstd, ALU.mult)
                        tt(hbB, hbB, rstd[0:64, :], ALU.mult)
                        skA = tf()
                        skB = tf()
                        act(skA, xcA[:, gs], AF.Copy, scale=mvA[:, l * 12 + 4 + j:l * 12 + 5 + j])
                        act(skB[0:64, :], xcB[0:64, gs], AF.Copy, scale=mvB[:, l * 12 + 4 + j:l * 12 + 5 + j])
                        stt(hbA, hbA, mgsA[:, l * 4 + j:l * 4 + j + 1], skA, ALU.mult, ALU.add)
                        stt(hbB, hbB, mgsB[:, l * 4 + j:l * 4 + j + 1], skB[0:64, :], ALU.mult, ALU.add)
                        tt(yA[:, gs], hbA, so[2], ALU.mult)
                        tt(yB[0:64, gs], hbB, so[3][0:64, :], ALU.mult)

                run_pipeline(units, [st0, st1], SKEW)
            if j == 0:
                dbg_out("ym", yA, [128, S])
            out_proj(l, [((lambda g: yA[:, g * 512:(g + 1) * 512]), 128, Y_OFF["m"] + j * 192),
                         ((lambda g: yB[0:64, g * 512:(g + 1) * 512]), 64, Y_OFF["m"] + j * 192 + 128)])

    cur = {"b": 0}
    for l in range(NL):
        lb_for_layer(l, l)

    for b in range(NSEQ):
        cur["b"] = b
        xsrc[b] = [d_x[b, t * 128:(t + 1) * 128, :] for t in range(NT)]
        for l in range(NL):
            for t in range(NT):
                g = t // 4
                o = (t % 4) * 128
                rrx["i"] = (rrx["i"] + 1) % 2
                xt = xt_tiles[rrx["i"]]
                P.op("sp", (lambda dst, s_: (lambda e: e.dma_start(out=dst.ap, in_=s_)))(xt, xsrc[b][t]),
                     reads=xdr[b][t], writes=[xt], is_dma=True, dkey=xt.buf.id)
                xi = xin[t % 2]
                P.op("act", (lambda o_, i_, a_: (lambda e: e.activation(out=o_.ap, in_=i_.ap, func=AF.Square, accum_out=a_.ap)))(xi, xt, ssq[:, 0:1]),
                     reads=[xt], writes=[xi, ssq])
                rstd_from(ssq[:, 1:2], ssq[:, 0:1], 1024)
                ts(xi, xt, ssq[:, 1:2], ALU.mult)
                for kq in range(2):
                    ps = pbank()
                    for k4 in range(4):
                        k = kq * 4 + k4
                        transp(ps[:, k4 * 128:(k4 + 1) * 128], xi[:, k * 128:(k + 1) * 128], ident_f)
                    for k4 in range(4):
                        k = kq * 4 + k4
                        ts(hTg[k][g][:, o:o + 128], ps[:, k4 * 128:(k4 + 1) * 128], ng32[:, l * 8 + k:l * 8 + k + 1], ALU.mult)
            if "a" in groups:
                ctab = W_f32[0]
                stab = W_f32[1]
                angt = W_f32[2]
                posi = T(angt.ap[:, 0:S].bitcast(I32), angt.buf)
                dma_in(posi, d_pos[b:b + 1, :].to_broadcast([128, S]))
                cp(angt[:, 0:S], posi)
                ts(angt[:, 0:S], angt[:, 0:S], invf, ALU.mult)
                ts(ctab[:, 0:S], angt[:, 0:S], float(1.0 / (2 * math.pi)), ALU.mult)
                ki = T(stab.ap[:, 0:S].bitcast(I32), stab.buf)
                cp(ki, ctab[:, 0:S])
                cp(ctab[:, 0:S], ki)
                stt(angt[:, 0:S], ctab[:, 0:S], float(-2 * math.pi), angt[:, 0:S], ALU.mult, ALU.add)
                act(stab[:, 0:S], angt[:, 0:S], AF.Sin, scale=0.25)
                tt(stab[:, 0:S], stab[:, 0:S], stab[:, 0:S], ALU.mult)
                ts(stab[:, 0:S], stab[:, 0:S], -2.0, ALU.mult, 1.0, ALU.add)
                act(angt[:, 0:S], angt[:, 0:S], AF.Sin, scale=0.5)
                stt(stab[:, 0:S], angt[:, 0:S], 2.0, stab[:, 0:S], ALU.mult, ALU.mult)
                ts(stab[:, 0:S], stab[:, 0:S], sgn, ALU.mult)
                tt(ctab[:, 0:S], angt[:, 0:S], angt[:, 0:S], ALU.mult)
                ts(ctab[:, 0:S], ctab[:, 0:S], -2.0, ALU.mult, 1.0, ALU.add)
                attention_group(l, ctab, stab)
            excl["on"] = True
            if "h" in groups:
                hgrn_group(l)
            if "m" in groups:
                mlstm_group(l)
            excl["on"] = False
        if final_norm:
            dma_in(fgb, d_fgrow[0:1, :].to_broadcast([128, 1024]))
            ts(fgb, fgb, 32.0, ALU.mult)
        for t in range(NT):
            rrx["i"] = (rrx["i"] + 1) % 2
            xt = xt_tiles[rrx["i"]]
            P.op("sp", (lambda dst, s_: (lambda e: e.dma_start(out=dst.ap, in_=s_)))(xt, xsrc[b][t]),
                 reads=xdr[b][t], writes=[xt], is_dma=True, dkey=xt.buf.id)
            if final_norm:
                xi = xin[t % 2]
                P.op("act", (lambda o_, i_, a_: (lambda e: e.activation(out=o_.ap, in_=i_.ap, func=AF.Square, accum_out=a_.ap)))(xi, xt, ssq[:, 2:3]),
                     reads=[xt], writes=[xi, ssq])
                rstd_from(ssq[:, 3:4], ssq[:, 2:3], 1024)
                stt(xt, xt, ssq[:, 3:4], fgb, ALU.mult, ALU.mult)
            tok = T(None)
            P.op("sp", (lambda src_, d_: (lambda e: e.dma_start(out=d_, in_=src_.ap)))(xt, d_out[b, t * 128:(t + 1) * 128, :]),
                 reads=[xt], writes=xdr[b][t] + [tok], is_dma=True, dkey="o%d" % xt.buf.id)
            fin.append(tok)
    P.op("sp", None, reads=fin)
    P.emit(nc, ctx)
    P.sbuf_left = nc.sbuf_bytes_remaining
    ctx.close()
    return nc, P, dbg_outs


def prep_params(inp, layers, depth_all):
    NL = len(layers)
    f = np.float32
    out = {}
    out["w_in"] = np.ascontiguousarray(inp["w_in"][layers], dtype=f)
    out["w_out"] = np.ascontiguousarray(inp["w_out"][layers], dtype=f)
    out["consts"] = make_consts()
    ng = inp["norm_g"][layers].reshape(NL, 8, 128)
    out["ng"] = np.ascontiguousarray(ng.transpose(2, 0, 1).reshape(128, NL * 8), dtype=f)
    out["fgrow"] = np.ascontiguousarray(inp["final_g"].reshape(1, 1024), dtype=f)
    out["alam"] = np.ascontiguousarray(inp["a_lambda"][layers].reshape(1, NL * 256), dtype=f)
    out["ang"] = np.ascontiguousarray(inp["a_norm_g"][layers].reshape(NL, 4, 128).transpose(2, 0, 1).reshape(128, NL * 4), dtype=f)
    lb = inp["h_lb_logits"].reshape(depth_all, 2, 6, 128)
    out["lb"] = np.ascontiguousarray(lb.transpose(3, 1, 2, 0).reshape(128, 12 * depth_all), dtype=f)
    out["hng"] = np.ascontiguousarray(inp["h_norm_g"][layers].reshape(NL, 6, 128).transpose(2, 0, 1).reshape(128, NL * 6), dtype=f)
    cw = inp["m_conv_w"][layers].reshape(NL, 5, 4, 192)
    out["cwA"] = np.ascontiguousarray(cw[:, :, :, 0:128].transpose(3, 0, 2, 1).reshape(128, NL * 20), dtype=f)
    out["cwB"] = np.ascontiguousarray(cw[:, :, :, 128:192].transpose(3, 0, 2, 1).reshape(64, NL * 20), dtype=f)
    mv = np.stack([inp["m_conv_b"][layers], inp["m_skip"][layers], inp["m_norm_g"][layers]], axis=1)
    mv = mv.reshape(NL, 3, 4, 192)
    out["mvA"] = np.ascontiguousarray(mv[:, :, :, 0:128].transpose(3, 0, 1, 2).reshape(128, NL * 12), dtype=f)
    out["mvB"] = np.ascontiguousarray(mv[:, :, :, 128:192].transpose(3, 0, 1, 2).reshape(64, NL * 12), dtype=f)
    bdA = np.zeros((NL, 4, 128, 6, 128), f)
    bdB = np.zeros((NL, 4, 64, 6, 64), f)
    for wi, nm in enumerate(("m_wq", "m_wk", "m_wv")):
        w = inp[nm][layers].reshape(NL, 4, 48, 4, 4)
        for g_ in range(32):
            bdA[:, :, 4 * g_:4 * g_ + 4, wi, 4 * g_:4 * g_ + 4] = w[:, :, g_]
            bdA[:, :, 4 * g_:4 * g_ + 4, 3 + wi, 4 * g_:4 * g_ + 4] = w[:, :, g_].transpose(0, 1, 3, 2)
        for g_ in range(16):
            bdB[:, :, 4 * g_:4 * g_ + 4, wi, 4 * g_:4 * g_ + 4] = w[:, :, 32 + g_]
            bdB[:, :, 4 * g_:4 * g_ + 4, 3 + wi, 4 * g_:4 * g_ + 4] = w[:, :, 32 + g_].transpose(0, 1, 3, 2)
    out["bdA"] = bdA.reshape(NL, 4, 128, 6 * 128)
    out["bdB"] = bdB.reshape(NL, 4, 64, 6 * 64)
    wg = inp["m_w_gates"][layers].reshape(NL, 3, 4, 192, 16)
    out["wgA"] = np.ascontiguousarray(wg[:, :, :, 0:128].transpose(0, 3, 2, 1, 4).reshape(NL, 128, 4 * 3 * 16), dtype=f)
    out["wgB"] = np.ascontiguousarray(wg[:, :, :, 128:192].transpose(0, 3, 2, 1, 4).reshape(NL, 64, 4 * 3 * 16), dtype=f)
    out["bg"] = np.ascontiguousarray(inp["m_b_gates"][layers].reshape(1, NL * 16), dtype=f)
    return out


_CACHE = {}


def _get_prog(S, NSEQ, NL, depth_all, lam_inits, final_norm):
    key = (S, NSEQ, NL, depth_all, tuple(lam_inits), final_norm)
    if key not in _CACHE:
        _CACHE[key] = build(S, NSEQ, NL, depth_all, lam_inits, final_norm=final_norm)[0]
    return _CACHE[key]


def kernel(**inputs):
    inp = {k: np.asarray(v) for k, v in inputs.items()}
    x = np.ascontiguousarray(inp["x"], dtype=np.float32)
    pos = np.ascontiguousarray(inp["positions"], dtype=np.int32)
    B, S, D = x.shape
    DEPTH = inp["w_in"].shape[0]
    NCORE = 8
    per = B // NCORE
    lam_all = [0.8 - 0.6 * math.exp(-0.3 * l) for l in range(DEPTH)]
    params = prep_params(inp, list(range(DEPTH)), DEPTH)
    nc = _get_prog(S, per, DEPTH, DEPTH, lam_all, True)
    in_maps = []
    for c in range(NCORE):
        m = dict(params)
        m["x"] = x[c * per:(c + 1) * per]
        m["pos"] = pos[c * per:(c + 1) * per]
        in_maps.append(m)
    res = run_bass_kernel_spmd(nc, in_maps, core_ids=list(range(NCORE)))
    out = np.concatenate([np.asarray(r["out"]) for r in res.results], axis=0)
    return out.astype(np.float32)
```
